# Optimizing a Trainium2 kernel written in Bass

```python
import math
import jax
import jax.numpy as jnp
from jax import lax
import numpy as np

D_MODEL = 1024
BATCH = 8
SEQ = 2048
DEPTH = 4

CTX_LEN = 256
GRID_W = 64
EPS = 1e-6
CHUNK = 64
CONV_W = 5

S5_WIDTH = D_MODEL // 2
S5_GROUP = 16
S5_GROUPS = S5_WIDTH // S5_GROUP
S5_STATE = 64
GDN_DK = 128
GDN_DV = 128
GDN_WIDTH = D_MODEL // 2
GDN_HEADS = GDN_WIDTH // GDN_DV
GDN_CONV_CH = 2 * GDN_HEADS * GDN_DK + GDN_WIDTH
M2_WIDTH = D_MODEL
M2_HEADDIM = 64
M2_HEADS = M2_WIDTH // M2_HEADDIM
M2_GROUPS = 2
M2_HPG = M2_HEADS // M2_GROUPS
M2_STATE = 128
M2_CONV_CH = M2_WIDTH + 2 * M2_GROUPS * M2_STATE

MIX_WIDTH = S5_WIDTH + GDN_WIDTH + M2_WIDTH
IN_SIZES = (S5_WIDTH, GDN_CONV_CH, GDN_WIDTH, 2 * GDN_HEADS, 2 * GDN_HEADS,
            M2_WIDTH, M2_CONV_CH, 2 * M2_HEADS)
D_IN = sum(IN_SIZES)

D_FF = 256 * ((8 * D_MODEL // 3 + 255) // 256)
N_EXPERTS = 8
TOP_K = 2

F32 = jnp.float32

kernel_name = "hybrid_s5_gdn_ssd_moe_prefix_dit"


def rmsnorm(x, g):
    xf = x.astype(F32)
    y = xf * lax.rsqrt(jnp.mean(xf * xf, axis=-1, keepdims=True) + EPS)
    return (y * g.astype(F32)).astype(x.dtype)


def modulate(x, g, shift, scale):
    return rmsnorm(x, g) * (1 + scale) + shift


def l2norm(t):
    return t * lax.rsqrt(jnp.sum(t * t, axis=-1, keepdims=True) + EPS)


def grid_transpose(u, rows, cols):
    b, l, ch = u.shape
    return u.reshape(b, rows, cols, ch).transpose(0, 2, 1, 3).reshape(b, l, ch)


def split_cols(u, sizes):
    parts, start = [], 0
    for s in sizes:
        parts.append(u[..., start:start + s])
        start += s
    return parts


def rev(t, flag):
    return jnp.flip(t, axis=1) if flag else t


def dwconv(u, w):
    pad = CONV_W // 2
    return lax.conv_general_dilated(
        u, w[:, None, :].astype(u.dtype), window_strides=(1,), padding=[(pad, pad)],
        dimension_numbers=("NWC", "WIO", "NWC"), feature_group_count=u.shape[-1])


def seg_decay(cs):
    t = cs.shape[-1]
    mask = jnp.tril(jnp.ones((t, t), dtype=bool))
    diff = cs[..., :, None] - cs[..., None, :]
    return jnp.where(mask, jnp.exp(jnp.where(mask, diff, 0.0)), 0.0)


def _affine_combine(left, right):
    a_l, b_l = left
    a_r, b_r = right
    return a_l * a_r, a_r * b_l + b_r


def linear_scan(a, b, h0, reverse):
    a_cum, h = lax.associative_scan(_affine_combine, (a, b), axis=1, reverse=reverse)
    if h0 is not None:
        h = h + a_cum * h0[:, None]
    return h


def s5_direction(ug, lam_re, lam_im, b_re, b_im, log_dt, h0, reverse):
    lam = lax.complex(jnp.minimum(lam_re.astype(F32), -1e-4), lam_im.astype(F32))
    dt = jnp.exp(log_dt.astype(F32))[:, None]
    lam_bar = jnp.exp(lam * dt)
    gamma = (lam_bar - 1.0) / lam
    bu = lax.complex(jnp.einsum("blgi,gpi->blgp", ug, b_re.astype(F32)),
                     jnp.einsum("blgi,gpi->blgp", ug, b_im.astype(F32))) * gamma
    return linear_scan(jnp.broadcast_to(lam_bar, bu.shape), bu, h0, reverse)


def s5_readout(states, c_re, c_im):
    return (jnp.einsum("blgp,gip->blgi", states.real, c_re.astype(F32))
            - jnp.einsum("blgp,gip->blgi", states.imag, c_im.astype(F32)))


def s5_mixer(u_ctx, u_lat, lam_re, lam_im, b_re, b_im, c_re, c_im, log_dt, d_skip, w_glu, ctx_out):
    uc = u_ctx.reshape(*u_ctx.shape[:2], S5_GROUPS, S5_GROUP)
    ul = u_lat.reshape(*u_lat.shape[:2], S5_GROUPS, S5_GROUP)
    y_ctx, y_lat = [], []
    for d in range(2):
        reverse = d == 1
        p = (lam_re[d], lam_im[d], b_re[d], b_im[d], log_dt[d])
        s_ctx = s5_direction(uc, *p, None, reverse)
        h_end = s_ctx[:, 0] if reverse else s_ctx[:, -1]
        s_lat = s5_direction(ul, *p, h_end, reverse)
        y_lat.append(s5_readout(s_lat, c_re[d], c_im[d]))
        if ctx_out:
            y_ctx.append(s5_readout(s_ctx, c_re[d], c_im[d]))

    def finish(ys, u):
        y = (ys[0] + ys[1]).reshape(u.shape) + d_skip.astype(F32) * u
        y = jax.nn.gelu(y)
        return y * jax.nn.sigmoid(y @ w_glu.astype(F32))

    return (finish(y_ctx, u_ctx) if ctx_out else None), finish(y_lat, u_lat)


def gdn_prep(qkv, a_raw, b_raw, conv_w, a_log, dt_bias):
    qkv = jax.nn.silu(dwconv(qkv, conv_w))
    bsz, l, _ = qkv.shape
    q, k, v = split_cols(qkv, (GDN_HEADS * GDN_DK, GDN_HEADS * GDN_DK, GDN_WIDTH))
    q = l2norm(q.reshape(bsz, l, GDN_HEADS, GDN_DK))
    k = l2norm(k.reshape(bsz, l, GDN_HEADS, GDN_DK))
    v = v.reshape(bsz, l, GDN_HEADS, GDN_DV)
    g = -jnp.exp(a_log.astype(F32)) * jax.nn.softplus(
        a_raw.reshape(bsz, l, 2, GDN_HEADS) + dt_bias.astype(F32))
    beta = jax.nn.sigmoid(b_raw.reshape(bsz, l, 2, GDN_HEADS))
    return q, k, v, g, beta


def gdn_chunked(q, k, v, g, beta, s0, with_output):
    b, l, h, dk = q.shape
    n = l // CHUNK

    def blk(t):
        return jnp.moveaxis(t.reshape(b, n, CHUNK, h, *t.shape[3:]), 3, 1)

    q, k, v, beta = blk(q) * dk ** -0.5, blk(k), blk(v), blk(beta)
    gc = jnp.cumsum(blk(g), axis=-1)
    decay = seg_decay(gc)
    strict = jnp.tril(jnp.ones((CHUNK, CHUNK), dtype=bool), -1)
    kb = k * beta[..., None]
    lower = jnp.where(strict, jnp.einsum("bhnid,bhnjd->bhnij", kb, k) * decay, 0.0)
    rhs = jnp.concatenate([v * beta[..., None], kb * jnp.exp(gc)[..., None]], axis=-1)
    sol = lax.linalg.triangular_solve(jnp.eye(CHUNK, dtype=F32) + lower, rhs,
                                      left_side=True, lower=True, unit_diagonal=True)
    dv = v.shape[-1]
    u_c, w_c = sol[..., :dv], sol[..., dv:]
    k_dec = k * jnp.exp(gc[..., -1:] - gc)[..., None]
    g_last = jnp.exp(gc[..., -1])
    xs = [u_c, w_c, k_dec, g_last]
    if with_output:
        xs += [q * jnp.exp(gc)[..., None], jnp.einsum("bhnid,bhnjd->bhnij", q, k) * decay]

    def step(s, inp):
        u_i, w_i, kd_i, gl_i = inp[:4]
        v_new = u_i - jnp.einsum("bhcd,bhde->bhce", w_i, s)
        s_next = s * gl_i[..., None, None] + jnp.einsum("bhcd,bhce->bhde", kd_i, v_new)
        if not with_output:
            return s_next, None
        qd_i, a_i = inp[4:]
        o = jnp.einsum("bhcd,bhde->bhce", qd_i, s) + jnp.einsum("bhij,bhje->bhie", a_i, v_new)
        return s_next, o

    s_fin, o = lax.scan(step, s0, [jnp.moveaxis(t, 2, 0) for t in xs])
    if with_output:
        o = jnp.moveaxis(jnp.moveaxis(o, 0, 2), 1, 3).reshape(b, l, h, dv)
    return o, s_fin


def gdn_direction(p, d, s_init, with_output):
    q, k, v, g, beta = p
    r = d == 1
    o, s_fin = gdn_chunked(rev(q, r), rev(k, r), rev(v, r), rev(g[:, :, d], r), rev(beta[:, :, d], r),
                           s_init, with_output)
    return (rev(o, r) if with_output else None), s_fin


def gdn_mixer(ctx_in, lat_in, conv_w, a_log, dt_bias, norm_g, ctx_out):
    pc = gdn_prep(ctx_in[0], ctx_in[2], ctx_in[3], conv_w, a_log, dt_bias)
    pl = gdn_prep(lat_in[0], lat_in[2], lat_in[3], conv_w, a_log, dt_bias)
    s0 = jnp.zeros((ctx_in[0].shape[0], GDN_HEADS, GDN_DK, GDN_DV), F32)
    o_ctx, o_lat = [], []
    for d in range(2):
        oc, s_ctx = gdn_direction(pc, d, s0, ctx_out)
        ol, _ = gdn_direction(pl, d, s_ctx, True)
        o_lat.append(ol)
        if ctx_out:
            o_ctx.append(oc)

    def finish(os, z):
        o = rmsnorm(os[0] + os[1], norm_g)
        return (o * jax.nn.silu(z).reshape(o.shape)).reshape(z.shape)

    return (finish(o_ctx, ctx_in[1]) if ctx_out else None), finish(o_lat, lat_in[1])


def m2_prep(xbc, dt_raw, conv_w, conv_b, dt_bias):
    xbc = jax.nn.silu(dwconv(xbc, conv_w) + conv_b.astype(F32))
    bsz, l, _ = xbc.shape
    xs, bm, cm = split_cols(xbc, (M2_WIDTH, M2_GROUPS * M2_STATE, M2_GROUPS * M2_STATE))
    xs = xs.reshape(bsz, l, M2_GROUPS, M2_HPG, M2_HEADDIM)
    bm = bm.reshape(bsz, l, M2_GROUPS, M2_STATE)
    cm = cm.reshape(bsz, l, M2_GROUPS, M2_STATE)
    dt = jax.nn.softplus(dt_raw.reshape(bsz, l, 2, M2_GROUPS, M2_HPG)
                         + dt_bias.astype(F32).reshape(2, M2_GROUPS, M2_HPG))
    return xs, bm, cm, dt


def ssd_chunked(xdt, log_a, bm, cm, h0, with_output):
    bsz, l = xdt.shape[:2]
    n = l // CHUNK
    xc = xdt.reshape(bsz, n, CHUNK, *xdt.shape[2:])
    bc = bm.reshape(bsz, n, CHUNK, *bm.shape[2:])
    cc = cm.reshape(bsz, n, CHUNK, *cm.shape[2:])
    a_cs = jnp.cumsum(jnp.moveaxis(log_a.reshape(bsz, n, CHUNK, M2_GROUPS, M2_HPG), (3, 4), (1, 2)),
                      axis=-1)
    decay_to_end = jnp.exp(a_cs[..., -1:] - a_cs)
    chunk_states = jnp.einsum("bncge,bgjnc,bncgjp->bngjpe", bc, decay_to_end, xc)
    if h0 is None:
        h0 = jnp.zeros_like(chunk_states[:, 0])
    chunk_states = jnp.concatenate([h0[:, None], chunk_states], axis=1)
    chunk_cs = jnp.cumsum(jnp.pad(a_cs[..., -1], [(0, 0)] * 3 + [(1, 0)]), axis=-1)
    states = jnp.einsum("bgjzy,bygjpe->bzgjpe", seg_decay(chunk_cs), chunk_states)
    if not with_output:
        return None, states[:, -1]
    y_diag = jnp.einsum("bncge,bnsge,bgjncs,bnsgjp->bncgjp", cc, bc, seg_decay(a_cs), xc)
    y_off = jnp.einsum("bncge,bngjpe,bgjnc->bncgjp", cc, states[:, :-1], jnp.exp(a_cs))
    return (y_diag + y_off).reshape(xdt.shape), states[:, -1]


def m2_direction(p, a, d, h0, with_output):
    xs, bm, cm, dt = p
    r = d == 1
    dtd = dt[:, :, d]
    y, h = ssd_chunked(rev(xs * dtd[..., None], r), rev(dtd * a[d], r), rev(bm, r), rev(cm, r),
                       h0, with_output)
    return (rev(y, r) if with_output else None), h


def m2_mixer(ctx_in, lat_in, conv_w, conv_b, a_log, dt_bias, d_skip, norm_g, ctx_out):
    pc = m2_prep(ctx_in[1], ctx_in[2], conv_w, conv_b, dt_bias)
    pl = m2_prep(lat_in[1], lat_in[2], conv_w, conv_b, dt_bias)
    a = -jnp.exp(a_log.astype(F32)).reshape(2, M2_GROUPS, M2_HPG)
    y_ctx, y_lat = [], []
    for d in range(2):
        yc, h_ctx = m2_direction(pc, a, d, None, ctx_out)
        yl, _ = m2_direction(pl, a, d, h_ctx, True)
        y_lat.append(yl)
        if ctx_out:
            y_ctx.append(yc)
    d_skip = d_skip.astype(F32).reshape(M2_GROUPS, M2_HPG, 1)

    def finish(ys, xs, z):
        bsz, l = z.shape[:2]
        y = (ys[0] + ys[1] + d_skip * xs).reshape(bsz, l, M2_GROUPS, -1)
        y = y * jax.nn.silu(z).reshape(bsz, l, M2_GROUPS, -1)
        return rmsnorm(y, norm_g.reshape(M2_GROUPS, -1)).reshape(bsz, l, M2_WIDTH)

    return (finish(y_ctx, pc[0], ctx_in[0]) if ctx_out else None), finish(y_lat, pl[0], lat_in[0])


def token_mixer(h_ctx, h_lat, w_in, s5_p, gdn_p, m2_p, ctx_out):
    pc = split_cols((h_ctx @ w_in).astype(F32), IN_SIZES)
    pl = split_cols((h_lat @ w_in).astype(F32), IN_SIZES)
    a_ctx, a_lat = s5_mixer(pc[0], pl[0], *s5_p, ctx_out)
    b_ctx, b_lat = gdn_mixer(pc[1:5], pl[1:5], *gdn_p, ctx_out)
    m_ctx, m_lat = m2_mixer(pc[5:8], pl[5:8], *m2_p, ctx_out)
    y_lat = jnp.concatenate([a_lat, b_lat, m_lat], axis=-1)
    y_ctx = jnp.concatenate([a_ctx, b_ctx, m_ctx], axis=-1) if ctx_out else None
    return y_ctx, y_lat


def swiglu(h, w_gate, w_up, w_down):
    return (jax.nn.silu(h @ w_gate) * (h @ w_up)) @ w_down


def moe_swiglu(h, router_w, w_gate, w_up, w_down):
    logits = (h @ router_w).astype(F32)
    top_val, top_idx = lax.top_k(logits, TOP_K)
    top_w = jax.nn.softmax(top_val, axis=-1)
    gates = jnp.einsum("...k,...ke->...e", top_w,
                       jax.nn.one_hot(top_idx, N_EXPERTS, dtype=F32)).astype(h.dtype)
    out = jnp.zeros_like(h)
    for e in range(N_EXPERTS):
        out = out + gates[..., e:e + 1] * swiglu(h, w_gate[e], w_up[e], w_down[e])
    return out


def setup_inputs(seed: int = 0) -> dict:
    key = jax.random.key(seed)
    keys = iter(jax.random.split(key, 48))

    def nrm(shape, scale):
        return scale * jax.random.normal(next(keys), shape, F32)

    def unif(shape, lo, hi):
        return jax.random.uniform(next(keys), shape, F32, lo, hi)

    def dt_bias(shape):
        dt = jnp.exp(unif(shape, math.log(1e-3), math.log(1e-1)))
        return dt + jnp.log(-jnp.expm1(-dt))

    n_dense, n_moe = (DEPTH + 1) // 2, DEPTH // 2
    s5_shape = (DEPTH, 2, S5_GROUPS, S5_STATE)
    return {
        "x": nrm((BATCH, SEQ, D_MODEL), 1.0),
        "c": nrm((BATCH, D_MODEL), 1.0),
        "ctx": nrm((BATCH, CTX_LEN, D_MODEL), 1.0),
        "c_ctx": nrm((D_MODEL,), 1.0),
        "ada_w": nrm((DEPTH, D_MODEL, 6 * D_MODEL), 0.5 * D_MODEL ** -0.5),
        "ada_b": nrm((DEPTH, 6 * D_MODEL), 0.01),
        "norm1_g": 1.0 + nrm((DEPTH, D_MODEL), 0.02),
        "norm2_g": 1.0 + nrm((DEPTH, D_MODEL), 0.02),
        "w_in": nrm((DEPTH, D_MODEL, D_IN), D_MODEL ** -0.5),
        "w_out": nrm((DEPTH, MIX_WIDTH, D_MODEL), MIX_WIDTH ** -0.5),
        "s5_lam_re": -0.5 + nrm(s5_shape, 0.01),
        "s5_lam_im": math.pi * jnp.arange(S5_STATE, dtype=F32) + nrm(s5_shape, 0.01),
        "s5_b_re": nrm((DEPTH, 2, S5_GROUPS, S5_STATE, S5_GROUP), (2 * S5_GROUP) ** -0.5),
        "s5_b_im": nrm((DEPTH, 2, S5_GROUPS, S5_STATE, S5_GROUP), (2 * S5_GROUP) ** -0.5),
        "s5_c_re": nrm((DEPTH, 2, S5_GROUPS, S5_GROUP, S5_STATE), S5_STATE ** -0.5),
        "s5_c_im": nrm((DEPTH, 2, S5_GROUPS, S5_GROUP, S5_STATE), S5_STATE ** -0.5),
        "s5_log_dt": unif((DEPTH, 2, S5_GROUPS), math.log(1e-3), math.log(1e-1)),
        "s5_d": nrm((DEPTH, S5_WIDTH), 1.0),
        "s5_w_glu": nrm((DEPTH, S5_WIDTH, S5_WIDTH), S5_WIDTH ** -0.5),
        "gdn_conv_w": nrm((DEPTH, CONV_W, GDN_CONV_CH), CONV_W ** -0.5),
        "gdn_a_log": jnp.log(unif((DEPTH, 2, GDN_HEADS), 1.0, 16.0)),
        "gdn_dt_bias": dt_bias((DEPTH, 2, GDN_HEADS)),
        "gdn_norm_g": 1.0 + nrm((DEPTH, GDN_DV), 0.02),
        "m2_conv_w": nrm((DEPTH, CONV_W, M2_CONV_CH), CONV_W ** -0.5),
        "m2_conv_b": nrm((DEPTH, M2_CONV_CH), 0.02),
        "m2_a_log": jnp.log(unif((DEPTH, 2, M2_HEADS), 1.0, 16.0)),
        "m2_dt_bias": dt_bias((DEPTH, 2, M2_HEADS)),
        "m2_d": 1.0 + nrm((DEPTH, M2_HEADS), 0.02),
        "m2_norm_g": 1.0 + nrm((DEPTH, M2_WIDTH), 0.02),
        "ffn_w_gate": nrm((n_dense, D_MODEL, D_FF), D_MODEL ** -0.5),
        "ffn_w_up": nrm((n_dense, D_MODEL, D_FF), D_MODEL ** -0.5),
        "ffn_w_down": nrm((n_dense, D_FF, D_MODEL), D_FF ** -0.5),
        "moe_router": nrm((n_moe, D_MODEL, N_EXPERTS), D_MODEL ** -0.5),
        "moe_w_gate": nrm((n_moe, N_EXPERTS, D_MODEL, D_FF), D_MODEL ** -0.5),
        "moe_w_up": nrm((n_moe, N_EXPERTS, D_MODEL, D_FF), D_MODEL ** -0.5),
        "moe_w_down": nrm((n_moe, N_EXPERTS, D_FF, D_MODEL), D_FF ** -0.5),
        "final_norm_g": 1.0 + nrm((D_MODEL,), 0.02),
    }


def reference(x, c, ctx, c_ctx, ada_w, ada_b, norm1_g, norm2_g, w_in, w_out,
              s5_lam_re, s5_lam_im, s5_b_re, s5_b_im, s5_c_re, s5_c_im, s5_log_dt, s5_d, s5_w_glu,
              gdn_conv_w, gdn_a_log, gdn_dt_bias, gdn_norm_g,
              m2_conv_w, m2_conv_b, m2_a_log, m2_dt_bias, m2_d, m2_norm_g,
              ffn_w_gate, ffn_w_up, ffn_w_down,
              moe_router, moe_w_gate, moe_w_up, moe_w_down, final_norm_g):
    rows = x.shape[1] // GRID_W
    sc = jax.nn.silu(c)
    sx = jax.nn.silu(c_ctx)
    x_lat, x_ctx = x, ctx
    for i in range(DEPTH):
        last = i == DEPTH - 1
        col_major = i % 2 == 1
        ml = jnp.split((sc @ ada_w[i] + ada_b[i])[:, None, :], 6, axis=-1)
        mc = jnp.split(sx @ ada_w[i] + ada_b[i], 6, axis=-1)
        s5_p = (s5_lam_re[i], s5_lam_im[i], s5_b_re[i], s5_b_im[i], s5_c_re[i], s5_c_im[i],
                s5_log_dt[i], s5_d[i], s5_w_glu[i])
        gdn_p = (gdn_conv_w[i], gdn_a_log[i], gdn_dt_bias[i], gdn_norm_g[i])
        m2_p = (m2_conv_w[i], m2_conv_b[i], m2_a_log[i], m2_dt_bias[i], m2_d[i], m2_norm_g[i])

        h_lat = modulate(x_lat, norm1_g[i], ml[0], ml[1])
        h_ctx = modulate(x_ctx, norm1_g[i], mc[0], mc[1])
        if col_major:
            h_lat = grid_transpose(h_lat, rows, GRID_W)
        y_ctx, y_lat = token_mixer(h_ctx, h_lat, w_in[i], s5_p, gdn_p, m2_p, not last)
        if col_major:
            y_lat = grid_transpose(y_lat, GRID_W, rows)
        x_lat = x_lat + ml[2] * (y_lat.astype(x_lat.dtype) @ w_out[i])

        j = i // 2
        if i % 2 == 0:
            ffn = lambda h: swiglu(h, ffn_w_gate[j], ffn_w_up[j], ffn_w_down[j])
        else:
            ffn = lambda h: moe_swiglu(h, moe_router[j], moe_w_gate[j], moe_w_up[j], moe_w_down[j])
        x_lat = x_lat + ml[5] * ffn(modulate(x_lat, norm2_g[i], ml[3], ml[4]))
        if not last:
            x_ctx = x_ctx + mc[2] * (y_ctx.astype(x_ctx.dtype) @ w_out[i])
            x_ctx = x_ctx + mc[5] * ffn(modulate(x_ctx, norm2_g[i], mc[3], mc[4]))
    return rmsnorm(x_lat, final_norm_g)
```

```python
import math
import os
from contextlib import ExitStack
import numpy as np
import concourse.bass as bass
import concourse.mybir as mybir
from concourse.bass_utils import run_bass_kernel_spmd

F32 = mybir.dt.float32
BF16 = mybir.dt.bfloat16
I32 = mybir.dt.int32
AF = mybir.ActivationFunctionType
ALU = mybir.AluOpType
AX = mybir.AxisListType

COMPUTE = ["tensor", "vector", "scalar", "gpsimd"]
ENGS = ["sync", "tensor", "vector", "scalar", "gpsimd"]
DMA_RING = 6
SAME_ENGINE_SYNC = True

D = 1024
NT = 2304
NCTX = 256
DEPTH = 4
DIN = 5168
DFF = 2816
NE = 8
TT = [(0, 256), (256, 512), (768, 512), (1280, 512), (1792, 512)]
NCH = 18
EPS = 1e-6
TWO_PI = 2.0 * math.pi


def _key(k):
    if isinstance(k, (str, tuple)):
        return k
    t = getattr(k, "tensor", k)
    return t.name


class Sched:
    def __init__(self, nc):
        self.nc = nc
        self.ops = {e: [] for e in ENGS}
        self.cnt = {e: 0 for e in COMPUTE}
        self.seen = {e: {} for e in ENGS}
        self.last_w = {}
        self.readers = {}
        self.ring_tot = {}
        self.ring_pos = {e: 0 for e in ENGS}
        self.ring_know = {}
        self.sem_names = ["c_" + e for e in COMPUTE]
        for e in ("sync", "scalar", "gpsimd"):
            for i in range(DMA_RING):
                n = "d_%s_%d" % (e, i)
                self.sem_names.append(n)
                self.ring_tot[n] = 0
        self.sems = {}
        self.n_ops = 0

    def _need(self, eng, tok, waits, is_dma=False):
        s, v, know = tok
        if self.seen[eng].get(s, 0) >= v:
            return
        if (not is_dma) and s == "c_" + eng and (eng == "tensor" or not SAME_ENGINE_SYNC):
            return
        waits[s] = max(waits.get(s, 0), v)
        sn = self.seen[eng]
        for ks, kv in know.items():
            if sn.get(ks, 0) < kv:
                sn[ks] = kv
        sn[s] = max(sn.get(s, 0), v)

    def _deps(self, eng, reads, writes, waits, is_dma=False):
        for k in reads:
            k = _key(k)
            t = self.last_w.get(k)
            if t is not None:
                self._need(eng, t, waits, is_dma)
            if isinstance(k, str) and k.startswith("ps"):
                for t in list(self.readers.get(k, {}).values()):
                    if t[0] != "c_" + eng:
                        self._need(eng, t, waits, is_dma)
        for k in writes:
            k = _key(k)
            t = self.last_w.get(k)
            if t is not None:
                self._need(eng, t, waits, is_dma)
            for t in list(self.readers.get(k, {}).values()):
                self._need(eng, t, waits, is_dma)

    def _publish(self, tok, reads, writes):
        for k in reads:
            self.readers.setdefault(_key(k), {})[tok[0]] = tok
        for k in writes:
            k = _key(k)
            self.last_w[k] = tok
            self.readers[k] = {}

    def op(self, eng, fn, reads=(), writes=()):
        waits = {}
        self._deps(eng, reads, writes, waits)
        self.cnt[eng] += 1
        s = "c_" + eng
        know = dict(self.seen[eng])
        know[s] = self.cnt[eng]
        tok = (s, self.cnt[eng], know)
        self.ops[eng].append((waits, fn, s, 1))
        self._publish(tok, reads, writes)
        self.n_ops += 1
        return tok

    def dma(self, eng, fn, reads=(), writes=()):
        waits = {}
        self._deps(eng, reads, writes, waits, True)
        i = self.ring_pos[eng]
        self.ring_pos[eng] = (i + 1) % DMA_RING
        s = "d_%s_%d" % (eng, i)
        if self.ring_tot[s] > 0 and self.seen[eng].get(s, 0) < self.ring_tot[s]:
            waits[s] = self.ring_tot[s]
            self.seen[eng][s] = self.ring_tot[s]
            for ks, kv in self.ring_know.get(s, {}).items():
                if self.seen[eng].get(ks, 0) < kv:
                    self.seen[eng][ks] = kv
        self.ring_tot[s] += 16
        know = dict(self.seen[eng])
        self.ring_know[s] = know
        tok = (s, self.ring_tot[s], know)
        self.ops[eng].append((waits, fn, s, 16))
        self._publish(tok, reads, writes)
        self.n_ops += 1
        return tok

    def barrier(self):
        toks = []
        for e in COMPUTE:
            if self.cnt[e] > 0:
                toks.append(("c_" + e, self.cnt[e], {}))
        for s, v in self.ring_tot.items():
            if v > 0:
                toks.append((s, v, {}))
        for e in ENGS:
            waits = {}
            for t in toks:
                self._need(e, t, waits)
            if waits:
                self.ops[e].append((waits, None, None, 0))

    def emit(self):
        nc = self.nc
        self.barrier()
        with ExitStack() as st:
            for n in self.sem_names:
                self.sems[n] = st.enter_context(nc.semaphore(n))
            block = st.enter_context(nc.Block())
            sems = self.sems

            def run(eng_name):
                def body(eng):
                    for waits, fn, s, inc in self.ops[eng_name]:
                        for ws, wv in waits.items():
                            eng.wait_ge(sems[ws], wv)
                        if fn is not None:
                            ins = fn(eng)
                            ins.then_inc(sems[s], inc)
                return body

            block.sync(run("sync"))
            block.tensor(run("tensor"))
            block.vector(run("vector"))
            block.scalar(run("scalar"))
            block.gpsimd(run("gpsimd"))


def _aps(*xs):
    return [x for x in xs if x is not None and not isinstance(x, (int, float))]


class K:
    def __init__(self, nc):
        self.nc = nc
        self.S = Sched(nc)
        self.rr = 0

    def mm(self, ps, lhsT, rhs, start=True, stop=True):
        rd = [lhsT, rhs] + ([] if start else [ps])
        return self.S.op("tensor", lambda e: e.matmul(ps, lhsT=lhsT, rhs=rhs, start=start, stop=stop), rd, [ps])

    def tr(self, ps, in_, ident):
        return self.S.op("tensor", lambda e: e.transpose(ps, in_, ident), [in_, ident], [ps])

    def act(self, out, in_, func, bias=None, scale=1.0, accum=None, eng="scalar"):
        kw = {}
        if bias is not None:
            kw["bias"] = bias
        if accum is not None:
            kw["accum_out"] = accum
        return self.S.op("scalar", lambda e: e.activation(out=out, in_=in_, func=func, scale=scale, **kw),
                         _aps(in_, bias, scale), _aps(out, accum))

    def tt(self, out, a, b, op, eng="vector"):
        return self.S.op(eng, lambda e: e.tensor_tensor(out=out, in0=a, in1=b, op=op), [a, b], [out])

    def ts(self, out, a, s1, op0, s2=None, op1=None, eng="vector", accum=None):
        def f(e):
            kw = {}
            if op1 is not None:
                kw["op1"] = op1
            if accum is not None:
                kw["accum_out"] = accum
            return e.tensor_scalar(out=out, in0=a, scalar1=s1, scalar2=s2, op0=op0, **kw)
        return self.S.op(eng, f, _aps(a, s1, s2), _aps(out, accum))

    def stt(self, out, a, s, b, op0, op1, eng="vector"):
        eng = "vector"
        return self.S.op(eng, lambda e: e.scalar_tensor_tensor(out=out, in0=a, scalar=s, in1=b, op0=op0, op1=op1),
                         _aps(a, s, b), [out])

    def cp(self, out, in_, eng="vector"):
        if eng == "scalar":
            return self.S.op(eng, lambda e: e.copy(out=out, in_=in_), [in_], [out])
        return self.S.op(eng, lambda e: e.tensor_copy(out=out, in_=in_), [in_], [out])

    def memset(self, out, v, eng="gpsimd"):
        return self.S.op(eng, lambda e: e.memset(out, v), [], [out])

    def red(self, out, in_, op, eng="vector"):
        return self.S.op(eng, lambda e: e.tensor_reduce(out=out, in_=in_, axis=AX.X, op=op), [in_], [out])

    def recip(self, out, in_):
        return self.S.op("vector", lambda e: e.reciprocal(out=out, in_=in_), [in_], [out])

    def scan(self, out, d0, d1, init):
        return self.S.op("vector", lambda e: e.tensor_tensor_scan(out=out, data0=d0, data1=d1, initial=init,
                                                                  op0=ALU.mult, op1=ALU.add),
                         _aps(d0, d1, init), [out])

    def dma(self, out, in_, q="sync"):
        return self.S.dma(q, lambda e: e.dma_start(out=out, in_=in_), [in_], [out])

    def ev(self):
        self.rr ^= 1
        return "vector" if self.rr else "gpsimd"


def perm_ap(t3, kt, lo, n):
    c0 = (lo - NCTX) // 32
    ncol = n // 32
    base = t3[:, kt, NCTX:NT]
    v = base.rearrange("p (r c) -> p c r", c=64)
    return v[:, c0:c0 + ncol, :]


def build_program(nlayers=DEPTH, stop=None):
    nc = bass.Bass("TRN2", target_bir_lowering=False)
    k = K(nc)
    _cnt = [0]

    def sbt(name, shape, dt):
        _cnt[0] += 1
        return nc.sbuf_tensor("%s_%d" % (name, _cnt[0]), shape, dt)

    def din(name, shape, dt=F32):
        return nc.dram_tensor(name, list(shape), dt, kind="ExternalInput").ap()

    def dscr(name, shape, dt=F32):
        return nc.dram_tensor(name, list(shape), dt, kind="Internal").ap()

    xT_in = din("xT", [D, NT])
    cs_in = din("cs", [128, 8, 2])
    ada_w = din("ada_w", [DEPTH, D, 6 * D])
    ada_b = din("ada_b", [DEPTH, 128, 48])
    n1g = din("n1g", [DEPTH, 128, 8])
    n2g = din("n2g", [DEPTH, 128, 8])
    fng = din("fng", [128, 8])
    w_in = din("w_in", [DEPTH, D, DIN])
    w_out = din("w_out", [DEPTH, 2048, D])
    ffn_wg = din("ffn_wg", [2, D, DFF])
    ffn_wu = din("ffn_wu", [2, D, DFF])
    ffn_wd = din("ffn_wd", [2, DFF, D])
    moe_r = din("moe_r", [2, D, NE])
    moe_wg = din("moe_wg", [2, NE, D, DFF])
    moe_wu = din("moe_wu", [2, NE, D, DFF])
    moe_wd = din("moe_wd", [2, NE, DFF, D])
    s5_lam = din("s5_lam", [DEPTH, 128, 3, 32])
    s5_B = din("s5_B", [DEPTH, 32, 2, 128, 128])
    s5_C = din("s5_C", [DEPTH, 32, 2, 128, 128])
    s5_d = din("s5_d", [DEPTH, 128, 4])
    s5_glu = din("s5_glu", [DEPTH, 512, 512])
    gdn_cw = din("gdn_cw", [DEPTH, 128, 12, 5])
    gdn_ab = din("gdn_ab", [DEPTH, 128, 16])
    gdn_ng = din("gdn_ng", [DEPTH, 128, 128])
    m2_cw = din("m2_cw", [DEPTH, 128, 12, 6])
    m2_ab = din("m2_ab", [DEPTH, 128, 80])
    m2_ng = din("m2_ng", [DEPTH, 128, 1024])
    out_T = nc.dram_tensor("outT", [D, 2048], F32, kind="ExternalOutput").ap()

    if stop == "mix":
        ymix = nc.dram_tensor("ymix", [2048, NT], BF16, kind="ExternalOutput").ap()
    else:
        ymix = dscr("ymix", [2048, NT], BF16)
    dbgx = nc.dram_tensor("dbgx", [D, NT], F32, kind="ExternalOutput").ap() if stop in ("oproj", "ffn", "norm1") else None
    yfwd_g = dscr("yfwd_g", [4, NT, 128])
    yfwd_m = dscr("yfwd_m", [NT, 1024])

    with ExitStack() as st:
        def sb(name, shape, dt=F32):
            return st.enter_context(sbt(name, list(shape), dt))

        def psum(name, shape=(128, 512), dt=F32):
            return st.enter_context(nc.psum_tensor(name, list(shape), dt))

        xT = sb("xTs", [128, 8, NT])
        hT = sb("hTs", [128, 8, NT], BF16)
        mod = sb("mod", [128, 2, 48])
        gs = sb("gs", [128, 2, 8])
        ident = sb("ident", [128, 128])
        identb = sb("identb", [128, 128], BF16)
        ones = sb("ones", [128, 128])
        epsc = sb("epsc", [128, 1])
        PS = [psum("ps%d" % i) for i in range(8)]

        DBG = os.environ.get("DBGDUMP", "").split(",")

        def dbg_dump(name, ap, shape, dt=F32):
            if name not in DBG:
                return
            o = nc.dram_tensor("dbg_" + name, list(shape), dt, kind="ExternalOutput").ap()
            k.dma(o, ap, "sync")

        k.memset(ident[:], 0.0)
        k.S.op("gpsimd", lambda e: e.affine_select(out=ident[:], in_=ident[:], pattern=[[-1, 128]],
                                                   compare_op=ALU.not_equal, fill=1.0, base=0,
                                                   channel_multiplier=1), [ident], [ident])
        k.cp(identb[:], ident[:])
        k.memset(ones[:], 1.0)
        k.memset(epsc[:], EPS)

        xv = xT_in.rearrange("(kt p) t -> p kt t", p=128)
        for kt in range(8):
            k.dma(xT[:, kt, :], xv[:, kt, :], "sync")

        csr = sb("csr", [128, 8, 2])
        k.dma(csr[:], cs_in, "sync")
        cs = sb("css", [128, 8, 2])
        k.act(cs[:], csr[:], AF.Silu)

        def adaln(l):
            with ExitStack() as s2:
                wb = [s2.enter_context(sbt("adaw%d" % i, [128, 8, 512], F32)) for i in range(2)]
                bb = s2.enter_context(sbt("adab", [128, 48], F32))
                k.dma(bb[:], ada_b[l], "sync")
                wv = ada_w[l].rearrange("(kt p) n -> p kt n", p=128)
                for blk in range(12):
                    w = wb[blk % 2]
                    k.dma(w[:], wv[:, :, blk * 512:(blk + 1) * 512], "sync" if blk % 2 == 0 else "scalar")
                    for j in range(4):
                        col = blk * 4 + j
                        ps = PS[col % 2]
                        for kt in range(8):
                            k.mm(ps[:, 0:2], w[:, kt, j * 128:(j + 1) * 128], cs[:, kt, :], kt == 0, kt == 7)
                        for jj in range(2):
                            k.ts(mod[:, jj, col:col + 1], ps[:, jj:jj + 1], bb[:, col:col + 1], ALU.add)
                k.S.barrier()

        def rmsnorm_mod(l, gsrc, shift_idx, scale_idx, permute, final=False):
            with ExitStack() as s2:
                g = s2.enter_context(sbt("ng", [128, 8], F32))
                sq = [s2.enter_context(sbt("sq%d" % i, [128, 512], F32)) for i in range(2)]
                rstd = s2.enter_context(sbt("rstd", [128, 512], F32))
                tmp = [s2.enter_context(sbt("nt%d" % i, [128, 512], F32)) for i in range(2)]
                k.dma(g[:], gsrc, "sync")
                if not final:
                    for j in range(2):
                        k.ts(gs[:, j, :], mod[:, j, scale_idx * 8:(scale_idx + 1) * 8], 1.0, ALU.add)
                        k.tt(gs[:, j, :], gs[:, j, :], g[:], ALU.mult)
                for ti, (lo, n) in enumerate(TT):
                    if final and ti == 0:
                        continue
                    ps = PS[2 + ti % 2]
                    for kt in range(8):
                        s = sq[kt % 2]
                        k.act(s[:, :n], xT[:, kt, lo:lo + n], AF.Square)
                        k.mm(ps[:, :n], ones[:], s[:, :n], kt == 0, kt == 7)
                    k.act(rstd[:, :n], ps[:, :n], AF.Sqrt, bias=epsc[:, 0:1], scale=1.0 / D)
                    k.recip(rstd[:, :n], rstd[:, :n])
                    j = 1 if ti == 0 else 0
                    for kt in range(8):
                        t = tmp[kt % 2]
                        k.tt(t[:, :n], xT[:, kt, lo:lo + n], rstd[:, :n], ALU.mult)
                        if final:
                            k.ts(t[:, :n], t[:, :n], g[:, kt:kt + 1], ALU.mult)
                            k.dma(out_T[kt * 128:(kt + 1) * 128, lo - NCTX:lo - NCTX + n], t[:, :n], "sync")
                        else:
                            if permute and ti > 0:
                                r0 = (lo - NCTX) // 64
                                dst = hT[:, kt, NCTX:NT].rearrange("p (c r) -> p r c", r=32)[:, r0:r0 + n // 64, :]
                                src = t[:, :n].rearrange("p (r c) -> p r c", c=64)
                            else:
                                dst = hT[:, kt, lo:lo + n]
                                src = t[:, :n]
                            k.ts(dst, src, gs[:, j, kt:kt + 1], ALU.mult,
                                 mod[:, j, shift_idx * 8 + kt:shift_idx * 8 + kt + 1], ALU.add)
                k.S.barrier()

        def out_proj(l, permute):
            with ExitStack() as s2:
                wo = s2.enter_context(sbt("wo", [128, 16, D], BF16))
                yb = [s2.enter_context(sbt("yb%d" % i, [128, 16, 512], BF16)) for i in range(2)]
                wv = w_out[l].rearrange("(kt p) n -> p kt n", p=128)
                for q4 in range(4):
                    k.dma(wo[:, q4 * 4:(q4 + 1) * 4, :], wv[:, q4 * 4:(q4 + 1) * 4, :], "gpsimd")
                yv = ymix.rearrange("(kt p) t -> p kt t", p=128)
                for ti, (lo, n) in enumerate(TT):
                    y = yb[ti % 2]
                    k.dma(y[:, :, :n], yv[:, :, lo:lo + n], "sync")
                    j = 1 if ti == 0 else 0
                    for nt in range(8):
                        ps = PS[nt % 4]
                        for kt in range(16):
                            k.mm(ps[:, :n], wo[:, kt, nt * 128:(nt + 1) * 128], y[:, kt, :n], kt == 0, kt == 15)
                        if permute and ti > 0:
                            dst = perm_ap(xT, nt, lo, n)
                            src = ps[:, :n].rearrange("p (c r) -> p c r", r=32)
                        else:
                            dst = xT[:, nt, lo:lo + n]
                            src = ps[:, :n]
                        k.stt(dst, src, mod[:, j, 16 + nt:17 + nt], dst, ALU.mult, ALU.add)
                k.S.barrier()

        def ffn_expert(wg, wu, wd, gbc, bufs):
            wgb, wub, wdb, hid, sgs = bufs
            wgv = wg.rearrange("(kt p) f -> p kt f", p=128)
            wuv = wu.rearrange("(kt p) f -> p kt f", p=128)
            wdv = wd.rearrange("(ft p) n -> p ft n", p=128)
            for fb in range(11):
                b = fb % 2
                k.dma(wgb[b][:], wgv[:, :, fb * 256:(fb + 1) * 256], "gpsimd")
                k.dma(wub[b][:], wuv[:, :, fb * 256:(fb + 1) * 256], "gpsimd")
                k.dma(wdb[b][:], wdv[:, fb * 2:(fb + 1) * 2, :], "gpsimd")
                for ti, (lo, n) in enumerate(TT):
                    j = 1 if ti == 0 else 0
                    hb = hid[ti % 2]
                    for f in range(2):
                        pg = PS[0 + f]
                        pu = PS[2 + f]
                        for kt in range(8):
                            k.mm(pg[:, :n], wgb[b][:, kt, f * 128:(f + 1) * 128], hT[:, kt, lo:lo + n], kt == 0, kt == 7)
                        for kt in range(8):
                            k.mm(pu[:, :n], wub[b][:, kt, f * 128:(f + 1) * 128], hT[:, kt, lo:lo + n], kt == 0, kt == 7)
                        sg = sgs[f]
                        k.act(sg[:, :n], pg[:, :n], AF.Silu)
                        if gbc is not None:
                            k.tt(sg[:, :n], sg[:, :n], gbc[:, lo:lo + n], ALU.mult, eng="gpsimd")
                        k.tt(hb[:, f, :n], sg[:, :n], pu[:, :n], ALU.mult)
                    for nt in range(8):
                        ps = PS[4 + nt % 4]
                        for f in range(2):
                            k.mm(ps[:, :n], wdb[b][:, f, nt * 128:(nt + 1) * 128], hb[:, f, :n], f == 0, f == 1)
                        k.stt(xT[:, nt, lo:lo + n], ps[:, :n], mod[:, j, 40 + nt:41 + nt], xT[:, nt, lo:lo + n],
                              ALU.mult, ALU.add)

        def ffn_bufs(s2):
            wgb = [s2.enter_context(sbt("wgb%d" % i, [128, 8, 256], BF16)) for i in range(2)]
            wub = [s2.enter_context(sbt("wub%d" % i, [128, 8, 256], BF16)) for i in range(2)]
            wdb = [s2.enter_context(sbt("wdb%d" % i, [128, 2, D], BF16)) for i in range(2)]
            hid = [s2.enter_context(sbt("hid%d" % i, [128, 2, 512], BF16)) for i in range(2)]
            sg = [s2.enter_context(sbt("sg%d" % i, [128, 512], F32)) for i in range(2)]
            return (wgb, wub, wdb, hid, sg)

        def ffn_dense(j):
            with ExitStack() as s2:
                bufs = ffn_bufs(s2)
                ffn_expert(ffn_wg[j], ffn_wu[j], ffn_wd[j], None, bufs)
                k.S.barrier()

        def ffn_moe(j):
            with ExitStack() as s2:
                bufs = ffn_bufs(s2)
                rw = s2.enter_context(sbt("rw", [128, 8, NE], BF16))
                gT = s2.enter_context(sbt("gT", [NE, NT], F32))
                sel = s2.enter_context(sbt("sel", [NE, NE, 128], F32))
                gbc = s2.enter_context(sbt("gbc", [128, NT], F32))
                lg = s2.enter_context(sbt("lg", [128, NE], F32))
                sm = s2.enter_context(sbt("sm", [128, 8], F32))
                m1 = s2.enter_context(sbt("m1", [128, NE], F32))
                m2 = s2.enter_context(sbt("m2", [128, NE], F32))
                l2 = s2.enter_context(sbt("l2", [128, NE], F32))
                gt = s2.enter_context(sbt("gt", [128, NE], F32))
                k.dma(rw[:], moe_r[j].rearrange("(kt p) e -> p kt e", p=128), "gpsimd")
                for e in range(NE):
                    k.cp(sel[:, e, :], ident[0:NE, e:e + 1].to_broadcast([NE, 128]))
                for c in range(NCH):
                    lo = c * 128
                    ps = PS[c % 2]
                    for kt in range(8):
                        k.mm(ps[:, 0:NE], hT[:, kt, lo:lo + 128], rw[:, kt, :], kt == 0, kt == 7)
                    k.cp(lg[:], ps[:, 0:NE])
                    k.red(sm[:, 0:1], lg[:], ALU.max)
                    k.ts(m1[:], lg[:], sm[:, 0:1], ALU.is_equal)
                    k.stt(l2[:], m1[:], -1e30, lg[:], ALU.mult, ALU.add)
                    k.red(sm[:, 1:2], l2[:], ALU.max)
                    k.ts(m2[:], l2[:], sm[:, 1:2], ALU.is_equal)
                    k.tt(sm[:, 2:3], sm[:, 0:1], sm[:, 1:2], ALU.subtract)
                    k.act(sm[:, 3:4], sm[:, 2:3], AF.Sigmoid)
                    k.ts(sm[:, 4:5], sm[:, 3:4], -1.0, ALU.mult, 1.0, ALU.add)
                    k.ts(gt[:], m1[:], sm[:, 3:4], ALU.mult)
                    k.stt(gt[:], m2[:], sm[:, 4:5], gt[:], ALU.mult, ALU.add)
                    pt = PS[2 + c % 2]
                    k.tr(pt[0:NE, 0:128], gt[:], ident[:])
                    k.cp(gT[:, lo:lo + 128], pt[0:NE, 0:128])
                for e in range(NE):
                    for ti, (lo, n) in enumerate(TT):
                        ps = PS[6 + ti % 2]
                        k.mm(ps[:, :n], sel[:, e, :], gT[:, lo:lo + n])
                        k.cp(gbc[:, lo:lo + n], ps[:, :n], eng="scalar")
                    ffn_expert(moe_wg[j, e], moe_wu[j, e], moe_wd[j, e], gbc, bufs)
                k.S.barrier()

        def load_win(dst, l, c0, ncols, q="gpsimd"):
            wv = w_in[l].rearrange("(kt p) c -> p kt c", p=128)
            k.dma(dst[:, :, :ncols], wv[:, :, c0:c0 + ncols], q)

        def proj_fm(wt, ncols, ti, ps):
            lo, n = TT[ti]
            for kt in range(8):
                k.mm(ps[:ncols, :n], wt[:, kt, :ncols], hT[:, kt, lo:lo + n], kt == 0, kt == 7)

        def proj_tm(wt, ncols, c, ps):
            for kt in range(8):
                k.mm(ps[:, :ncols], hT[:, kt, c * 128:(c + 1) * 128], wt[:, kt, :ncols], kt == 0, kt == 7)

        def sincos(s_out, c_out, ang, ki, tf, shape_slc):
            sl = shape_slc
            k.ts(ki[sl], ang, 1.0 / TWO_PI, ALU.mult)
            k.ts(tf[sl], ki[sl], -TWO_PI, ALU.mult)
            k.tt(tf[sl], tf[sl], ang, ALU.add)
            k.ts(tf[sl], tf[sl], math.pi, ALU.min, -math.pi, ALU.max)
            k.act(s_out, tf[sl], AF.Sin)
            k.ts(ki[sl], ang, 1.0 / TWO_PI, ALU.mult, 0.25, ALU.add)
            k.ts(tf[sl], ki[sl], -TWO_PI, ALU.mult)
            k.stt(tf[sl], ang, math.pi / 2, tf[sl], ALU.add, ALU.add)
            k.ts(tf[sl], tf[sl], math.pi, ALU.min, -math.pi, ALU.max)
            k.act(c_out, tf[sl], AF.Sin)

        s5yg = dscr("s5yg", [512, NT], BF16)

        def s5_mixer(l):
            with ExitStack() as s2:
                def T(name, shape, dt=F32):
                    return s2.enter_context(sbt("s5" + name, list(shape), dt))
                lam = T("lam", [128, 3, 32])
                pa = T("pa", [128, 32]); pdt = T("pdt", [128, 32]); par = T("par", [128, 32])
                pth = T("pth", [128, 32]); pr = T("pr", [128, 32]); psn = T("psn", [128, 32])
                pcs = T("pcs", [128, 32]); pki = T("pki", [128, 32], I32); ptf = T("ptf", [128, 32])
                lbr = T("lbr", [128, 32]); lbi = T("lbi", [128, 32]); den = T("den", [128, 32])
                gre = T("gre", [128, 32]); gim = T("gim", [128, 32]); ngim = T("ngim", [128, 32])
                t32 = T("t32", [128, 32])
                t96i = T("t96i", [128, 96], I32); t96 = T("t96", [128, 96])
                a96 = T("a96", [128, 96]); k96 = T("k96", [128, 96], I32); f96 = T("f96", [128, 96])
                s96 = T("s96", [128, 96]); c96 = T("c96", [128, 96])
                ttmp = [T("ttmp%d" % i, [128, 512]) for i in range(2)]
                ctabL = [T("ctab%d" % i, [128, NT]) for i in range(2)]
                stabL = [T("stab%d" % i, [128, NT]) for i in range(2)]
                SG = []
                for i in range(2):
                    b = {nm: T("%s%d" % (nm, i), [128, 512]) for nm in ("A", "Bb", "T1", "G1", "G2")}
                    b["HR"] = T("HR%d" % i, [128, 512], BF16); b["HI"] = T("HI%d" % i, [128, 512], BF16)
                    SG.append(b)
                car = T("car", [128, 2])
                ubf = T("ubf", [128, NT], BF16)
                wt = T("wt", [128, 8, 128], BF16)
                UB = []
                for i in range(2):
                    b = {nm: T("%s%d" % (nm, i), [128, 128]) for nm in ("Bre", "Bim", "Cre", "Cim", "Btr", "Bti")}
                    for nm in ("BtR", "BtI", "CbR", "CbI"):
                        b[nm] = T("%s%d" % (nm, i), [128, 128], BF16)
                    UB.append(b)
                dsk = T("dsk", [128, 4])
                yy = T("yy", [128, 512]); y2 = T("y2", [128, 512]); ygb = T("ygb", [128, 512], BF16)

                k.dma(lam[:], s5_lam[l], "sync")
                k.dma(dsk[:], s5_d[l], "sync")
                k.S.op("gpsimd", lambda e: e.iota(t96i[:, 0:48], pattern=[[48, 48]], base=0, channel_multiplier=0), [], [t96i])
                k.S.op("gpsimd", lambda e: e.iota(t96i[:, 48:96], pattern=[[1, 48]], base=0, channel_multiplier=0), [t96i], [t96i])
                k.cp(t96[:], t96i[:])
                k.ts(pa[:], lam[:, 0, :], -1e-4, ALU.min)
                k.act(pdt[:], lam[:, 2, :], AF.Exp)
                k.tt(par[:], pa[:], pdt[:], ALU.mult)
                k.tt(pth[:], lam[:, 1, :], pdt[:], ALU.mult)
                k.act(pr[:], par[:], AF.Exp)
                sincos(psn[:], pcs[:], pth[:], pki, ptf, (slice(None), slice(None)))
                k.tt(lbr[:], pr[:], pcs[:], ALU.mult)
                k.tt(lbi[:], pr[:], psn[:], ALU.mult)
                k.ts(lbr[:], lbr[:], -1.0, ALU.add)
                k.tt(den[:], pa[:], pa[:], ALU.mult)
                k.tt(t32[:], lam[:, 1, :], lam[:, 1, :], ALU.mult)
                k.tt(den[:], den[:], t32[:], ALU.add)
                k.recip(den[:], den[:])
                k.tt(gre[:], lbr[:], pa[:], ALU.mult)
                k.tt(t32[:], lbi[:], lam[:, 1, :], ALU.mult)
                k.tt(gre[:], gre[:], t32[:], ALU.add)
                k.tt(gre[:], gre[:], den[:], ALU.mult)
                k.tt(gim[:], lbi[:], pa[:], ALU.mult)
                k.tt(t32[:], lbr[:], lam[:, 1, :], ALU.mult)
                k.tt(gim[:], gim[:], t32[:], ALU.subtract)
                k.tt(gim[:], gim[:], den[:], ALU.mult)
                k.ts(ngim[:], gim[:], -1.0, ALU.mult)

                PSy = PS[0:5]
                UORD = [(0, 0), (1, 0), (2, 0), (3, 0), (0, 1), (1, 1), (2, 1), (3, 1)]

                def setup(ub, ui):
                    stl, d = UORD[ui]
                    u = (ub * 4 + stl) * 2 + d
                    B = UB[ui % 2]
                    ctab = ctabL[ui % 2]; stab = stabL[ui % 2]
                    k.dma(B["Bre"][:], s5_B[l, u, 0], "sync")
                    k.dma(B["Bim"][:], s5_B[l, u, 1], "sync")
                    k.dma(B["Cre"][:], s5_C[l, u, 0], "sync")
                    k.dma(B["Cim"][:], s5_C[l, u, 1], "sync")
                    k.ts(B["Btr"][:], B["Bre"][:], gre[:, u:u + 1], ALU.mult)
                    k.ts(B["Bti"][:], B["Bim"][:], gre[:, u:u + 1], ALU.mult)
                    k.stt(B["Btr"][:], B["Bim"][:], ngim[:, u:u + 1], B["Btr"][:], ALU.mult, ALU.add)
                    k.stt(B["Bti"][:], B["Bre"][:], gim[:, u:u + 1], B["Bti"][:], ALU.mult, ALU.add)
                    k.tr(PS[7][:, 0:128], B["Btr"][:], ident[:])
                    k.tr(PS[7][:, 128:256], B["Bti"][:], ident[:])
                    k.cp(B["BtR"][:], PS[7][:, 0:128], eng="scalar")
                    k.cp(B["BtI"][:], PS[7][:, 128:256], eng="scalar")
                    k.cp(B["CbR"][:], B["Cre"][:], eng="scalar")
                    k.ts(B["CbI"][:], B["Cim"][:], -1.0, ALU.mult)
                    k.ts(a96[:], t96[:], pth[:, u:u + 1], ALU.mult)
                    sincos(s96[:], c96[:], a96[:], k96, f96, (slice(None), slice(None)))
                    for pc in range(5):
                        a0 = pc * 10; na = min(10, 48 - a0)
                        eng = "gpsimd" if pc == 1 else "vector"
                        tm = ttmp[pc % 2]
                        def bc_a(src):
                            return src[:, a0:a0 + na].unsqueeze(2).to_broadcast([128, na, 48])
                        def bc_b(src):
                            return src[:, 48:96].unsqueeze(1).to_broadcast([128, na, 48])
                        cv = ctab[:, a0 * 48:(a0 + na) * 48].rearrange("p (a b) -> p a b", b=48)
                        sv = stab[:, a0 * 48:(a0 + na) * 48].rearrange("p (a b) -> p a b", b=48)
                        tv = tm[:, 0:na * 48].rearrange("p (a b) -> p a b", b=48)
                        k.tt(cv, bc_a(c96), bc_b(c96), ALU.mult, eng=eng)
                        k.tt(tv, bc_a(s96), bc_b(s96), ALU.mult, eng=eng)
                        k.tt(cv, cv, tv, ALU.subtract, eng=eng)
                        k.tt(sv, bc_a(s96), bc_b(c96), ALU.mult, eng=eng)
                        k.tt(tv, bc_a(c96), bc_b(s96), ALU.mult, eng=eng)
                        k.tt(sv, sv, tv, ALU.add, eng=eng)

                segctr = [0]

                def seg_info(ui, si):
                    stl, d = UORD[ui]
                    order = [0, 1, 2, 3, 4] if d == 0 else [0, 4, 3, 2, 1]
                    ti = order[si]
                    lo, n = TT[ti]
                    if d == 0:
                        tsl = slice(lo, lo + n); fw = slice(0, n); last = n - 1
                    else:
                        tsl = slice(255, None, -1) if ti == 0 else slice(2559 - lo, 2559 - lo - n, -1)
                        fw = slice(n - 1, None, -1); last = 0
                    return d, ti, lo, n, tsl, fw, last

                def stageA(ub, ui, si, buf):
                    d, ti, lo, n, tsl, fw, last = seg_info(ui, si)
                    B = UB[ui % 2]; ct = ctabL[ui % 2][:, tsl]; sn = stabL[ui % 2][:, tsl]
                    A, Bb, T1 = buf["A"], buf["Bb"], buf["T1"]
                    k.mm(PS[5][:, :n], B["BtR"][:], ubf[:, lo:lo + n])
                    k.mm(PS[6][:, :n], B["BtI"][:], ubf[:, lo:lo + n])
                    k.tt(A[:, :n], PS[5][:, :n], ct, ALU.mult)
                    k.tt(T1[:, :n], PS[6][:, :n], sn, ALU.mult)
                    k.tt(Bb[:, :n], PS[6][:, :n], ct, ALU.mult)
                    k.tt(A[:, :n], A[:, :n], T1[:, :n], ALU.add, eng="gpsimd")
                    k.tt(T1[:, :n], PS[5][:, :n], sn, ALU.mult)
                    k.tt(Bb[:, :n], Bb[:, :n], T1[:, :n], ALU.subtract, eng="gpsimd")

                def stageB(ub, ui, si, buf):
                    d, ti, lo, n, tsl, fw, last = seg_info(ui, si)
                    stl = UORD[ui][0]
                    u = (ub * 4 + stl) * 2 + d
                    B = UB[ui % 2]; ct = ctabL[ui % 2][:, tsl]; sn = stabL[ui % 2][:, tsl]
                    A, Bb, T1, G1, G2, HR, HI = buf["A"], buf["Bb"], buf["T1"], buf["G1"], buf["G2"], buf["HR"], buf["HI"]
                    rb = pr[:, u:u + 1].to_broadcast([128, n])
                    i1 = 0.0 if si == 0 else car[:, 0:1]
                    i2 = 0.0 if si == 0 else car[:, 1:2]
                    k.scan(G1[:, fw], rb, A[:, fw], i1)
                    k.scan(G2[:, fw], rb, Bb[:, fw], i2)
                    if si < 4:
                        k.cp(car[:, 0:1], G1[:, last:last + 1])
                        k.cp(car[:, 1:2], G2[:, last:last + 1])
                    k.tt(T1[:, :n], G1[:, :n], ct, ALU.mult, eng="gpsimd")
                    k.tt(A[:, :n], G2[:, :n], sn, ALU.mult, eng="gpsimd")
                    k.tt(HR[:, :n], T1[:, :n], A[:, :n], ALU.subtract)
                    k.tt(T1[:, :n], G1[:, :n], sn, ALU.mult, eng="gpsimd")
                    k.tt(A[:, :n], G2[:, :n], ct, ALU.mult, eng="gpsimd")
                    k.tt(HI[:, :n], T1[:, :n], A[:, :n], ALU.add)
                    k.mm(PSy[ti][:, :n], B["CbR"][:], HR[:, :n], ui == 0, False)
                    k.mm(PSy[ti][:, :n], B["CbI"][:], HI[:, :n], False, ui == 7)

                for ub in range(4):
                    load_win(wt, l, ub * 128, 128)
                    for ti, (lo, n) in enumerate(TT):
                        proj_fm(wt, 128, ti, PS[5 + ti % 2])
                        k.cp(ubf[:, lo:lo + n], PS[5 + ti % 2][:, :n], eng="scalar")
                    setup(ub, 0)
                    bufs = {}
                    bufs[(0, 0)] = SG[segctr[0] % 2]; segctr[0] += 1
                    stageA(ub, 0, 0, bufs[(0, 0)])
                    for ui in range(8):
                        for si in range(5):
                            if si < 4:
                                nb = SG[segctr[0] % 2]; segctr[0] += 1
                                bufs[(ui, si + 1)] = nb
                                stageA(ub, ui, si + 1, nb)
                            elif ui < 7:
                                setup(ub, ui + 1)
                                nb = SG[segctr[0] % 2]; segctr[0] += 1
                                bufs[(ui + 1, 0)] = nb
                                stageA(ub, ui + 1, 0, nb)
                            stageB(ub, ui, si, bufs[(ui, si)])
                    for ti, (lo, n) in enumerate(TT):
                        k.stt(yy[:, :n], ubf[:, lo:lo + n], dsk[:, ub:ub + 1], PSy[ti][:, :n], ALU.mult, ALU.add)
                        k.tt(y2[:, :n], yy[:, :n], yy[:, :n], ALU.mult, eng="gpsimd")
                        k.ts(y2[:, :n], y2[:, :n], 0.044715, ALU.mult, 1.0, ALU.add, eng="gpsimd")
                        k.tt(y2[:, :n], y2[:, :n], yy[:, :n], ALU.mult, eng="gpsimd")
                        k.act(y2[:, :n], y2[:, :n], AF.Sigmoid, scale=2.0 * math.sqrt(2.0 / math.pi))
                        k.tt(ygb[:, :n], yy[:, :n], y2[:, :n], ALU.mult)
                        k.dma(s5yg[ub * 128:(ub + 1) * 128, lo:lo + n], ygb[:, :n], "sync")
                k.S.barrier()
            with ExitStack() as s2:
                def T(name, shape, dt=F32):
                    return s2.enter_context(sbt("s5g" + name, list(shape), dt))
                wglu = T("wglu", [128, 4, 512], BF16)
                ygl = T("ygl", [128, 4, 512], BF16)
                yo = T("yo", [128, 512], BF16)
                yy = T("yy", [128, 512])
                k.dma(wglu[:], s5_glu[l].rearrange("(kt p) n -> p kt n", p=128), "gpsimd")
                ygv = s5yg.rearrange("(kt p) t -> p kt t", p=128)
                for ti, (lo, n) in enumerate(TT):
                    k.dma(ygl[:, :, :n], ygv[:, :, lo:lo + n], "sync")
                    for nt in range(4):
                        ps = PS[nt % 4]
                        for kt in range(4):
                            k.mm(ps[:, :n], wglu[:, kt, nt * 128:(nt + 1) * 128], ygl[:, kt, :n], kt == 0, kt == 3)
                        k.act(yy[:, :n], ps[:, :n], AF.Sigmoid)
                        k.tt(yo[:, :n], yy[:, :n], ygl[:, nt, :n], ALU.mult)
                        k.dma(ymix[nt * 128:(nt + 1) * 128, lo:lo + n], yo[:, :n], "sync")
                k.S.barrier()

        maskF = sb("maskF", [128, 128]); maskB = sb("maskB", [128, 128])
        nstrF = sb("nstrF", [128, 128]); nstrB = sb("nstrB", [128, 128])
        selF = sb("selF", [128, 128]); selB = sb("selB", [128, 128])
        k.memset(maskF[:], 1.0); k.memset(maskB[:], 1.0); k.memset(selF[:], 0.0); k.memset(selB[:], 0.0)
        k.S.op("gpsimd", lambda e: e.affine_select(out=maskF[:], in_=maskF[:], pattern=[[1, 128]], compare_op=ALU.is_ge,
                                                   fill=0.0, base=0, channel_multiplier=-1), [maskF], [maskF])
        k.S.op("gpsimd", lambda e: e.affine_select(out=maskB[:], in_=maskB[:], pattern=[[-1, 128]], compare_op=ALU.is_ge,
                                                   fill=0.0, base=0, channel_multiplier=1), [maskB], [maskB])
        k.S.op("gpsimd", lambda e: e.affine_select(out=selF[:], in_=selF[:], pattern=[[0, 128]], compare_op=ALU.not_equal,
                                                   fill=1.0, base=-127, channel_multiplier=1), [selF], [selF])
        k.S.op("gpsimd", lambda e: e.affine_select(out=selB[:], in_=selB[:], pattern=[[0, 128]], compare_op=ALU.not_equal,
                                                   fill=1.0, base=0, channel_multiplier=1), [selB], [selB])
        k.tt(nstrF[:], ident[:], maskF[:], ALU.subtract)
        k.tt(nstrB[:], ident[:], maskB[:], ALU.subtract)
        MASK = [maskF, maskB]; NSTR = [nstrF, nstrB]; SEL = [selF, selB]; ENDI = [127, 0]
        CH_ORDER = [list(range(NCH)), [1, 0] + list(range(NCH - 1, 1, -1))]

        def conv_block(raw, acc, cw, cb, ntap_bias):
            if ntap_bias:
                k.ts(acc[:], raw[:], cw[:, cb, 2:3], ALU.mult, cw[:, cb, 5:6], ALU.add)
            else:
                k.ts(acc[:], raw[:], cw[:, cb, 2:3], ALU.mult)
            for kk in (0, 1, 3, 4):
                dd = kk - 2
                for (s0, L) in ((0, NCTX), (NCTX, NT - NCTX)):
                    o0 = s0 + max(0, -dd); o1 = s0 + L - max(0, dd)
                    i0 = s0 + max(0, dd); i1 = s0 + L - max(0, -dd)
                    k.stt(acc[:, o0:o1], raw[:, i0:i1], cw[:, cb, kk:kk + 1], acc[:, o0:o1], ALU.mult, ALU.add,
                          eng=("vector" if kk < 2 else "gpsimd"))


        PADL = 2 + NCTX + 2 + 2 + (NT - NCTX) + 2

        def pad_off(lo):
            return 2 + lo if lo < NCTX else 2 + NCTX + 2 + 2 + (lo - NCTX)

        def conv_pe(rawp, dg, cw, cb, acc, bias_col):
            for kk in range(5):
                k.ts(dg[:, kk, :], identb[:], cw[:, cb, kk:kk + 1], ALU.mult)
            for ti, (lo, n) in enumerate(TT):
                ps = PS[4 + ti % 4]
                o = pad_off(lo)
                for kk in range(5):
                    k.mm(ps[:, :n], dg[:, kk, :], rawp[:, o + kk - 2:o + kk - 2 + n], kk == 0, kk == 4)
                if bias_col is not None:
                    k.act(acc[:, lo:lo + n], ps[:, :n], AF.Silu, bias=bias_col)
                else:
                    k.act(acc[:, lo:lo + n], ps[:, :n], AF.Silu)

        m2tm = dscr("m2tm", [NT, 1280])
        m2fm = dscr("m2fm", [512, NT])
        M2X = 3600

        def m2_mixer(l):
            with ExitStack() as s2:
                def T(name, shape, dt=F32):
                    return s2.enter_context(sbt("ma" + name, list(shape), dt))
                cw = T("cw", [128, 12, 6])
                wt = [T("wt%d" % i, [128, 8, 128], BF16) for i in range(2)]
                rawp = T("rawp", [128, PADL], BF16); acc = T("acc", [128, NT])
                dg = T("dg", [128, 5, 128], BF16)
                tmall = T("tmall", [128, NCH, 128])
                k.dma(cw[:], m2_cw[l], "sync")
                k.memset(rawp[:], 0.0)
                for cb in range(12):
                    w = wt[cb % 2]
                    load_win(w, l, M2X + cb * 128, 128)
                    for ti, (lo, n) in enumerate(TT):
                        ps = PS[ti % 4]
                        proj_fm(w, 128, ti, ps)
                        o = pad_off(lo)
                        k.cp(rawp[:, o:o + n], ps[:, :n], eng=("scalar" if ti % 2 else "vector"))
                    conv_pe(rawp, dg, cw, cb, acc, cw[:, cb, 5:6])
                    if cb >= 8:
                        k.dma(m2fm[(cb - 8) * 128:(cb - 7) * 128, :], acc[:], "sync")
                    if cb < 10:
                        for c in range(NCH):
                            ps = PS[c % 4]
                            k.tr(ps[:, 0:128], acc[:, c * 128:(c + 1) * 128], ident[:])
                            k.cp(tmall[:, c, :], ps[:, 0:128], eng=("vector" if c % 2 else "scalar"))
                        k.dma(m2tm[:, cb * 128:(cb + 1) * 128].rearrange("(c p) f -> p c f", p=128), tmall[:], "sync")
                k.S.barrier()
            with ExitStack() as s2:
                def T(name, shape, dt=F32):
                    return s2.enter_context(sbt("mb" + name, list(shape), dt))
                wz = T("wz", [128, 8, 1024], BF16)
                wdt = T("wdt", [128, 8, 32], BF16)
                ab = T("ab", [128, 80]); ng = T("ng", [128, 1024])
                nega = T("nega", [128, 32])
                Sst = T("S", [128, 16, 64])
                xtm = T("xtm", [128, 16, 64]); btm = T("btm", [128, 256])
                BT = T("BT", [128, 2, 128]); CT = T("CT", [128, 2, 128])
                dtv = T("dtv", [128, 16]); la = T("la", [128, 16]); cum = T("cum", [128, 16]); cend = T("cend", [128, 16])
                ecum = T("ecum", [128, 16]); edd = T("edd", [128, 16]); ecend = T("ecend", [128, 16])
                DEM = T("DEM", [128, 16, 128])
                Gm = T("Gm", [128, 2, 128])
                xdt = T("xdt", [128, 16, 64]); xdec = T("xdec", [128, 16, 64])
                yacc = T("yacc", [128, 16, 64]); yf = T("yf", [128, 16, 64])
                zs = T("zs", [128, 1024]); sq = T("sq", [128, 512]); ss = T("ss", [128, 2])
                ytb = T("ytb", [128, 8, 128], BF16)
                k.dma(ab[:], m2_ab[l], "sync")
                k.dma(ng[:], m2_ng[l], "sync")
                load_win(wz, l, 2576, 1024)
                load_win(wdt, l, 5136, 32)
                k.act(nega[:], ab[:, 0:32], AF.Exp)
                k.ts(nega[:], nega[:], -1.0, ALU.mult)
                for d in range(2):
                    mk = MASK[d]
                    k.memset(Sst[:], 0.0)
                    for c in CH_ORDER[d]:
                        r0 = c * 128
                        k.dma(xtm[:].rearrange("p h e -> p (h e)"), m2tm[r0:r0 + 128, 0:1024], "sync")
                        k.dma(btm[:], m2tm[r0:r0 + 128, 1024:1280], "sync")
                        k.dma(BT[:], m2fm[0:256, r0:r0 + 128].rearrange("(g p) t -> p g t", p=128), "sync")
                        k.dma(CT[:], m2fm[256:512, r0:r0 + 128].rearrange("(g p) t -> p g t", p=128), "sync")
                        proj_tm(wdt, 32, c, PS[6])
                        k.tt(dtv[:], PS[6][:, d * 16:(d + 1) * 16], ab[:, 32 + d * 16:48 + d * 16], ALU.add)
                        k.act(dtv[:], dtv[:], AF.Exp)
                        k.act(dtv[:], dtv[:], AF.Ln, bias=ones[:, 0:1])
                        k.tt(la[:], dtv[:], nega[:, d * 16:(d + 1) * 16], ALU.mult)
                        k.mm(PS[6][:, 64:80], mk[:], la[:])
                        k.cp(cum[:], PS[6][:, 64:80])
                        k.mm(PS[6][:, 96:112], SEL[d][:], cum[:])
                        k.cp(cend[:], PS[6][:, 96:112])
                        k.act(ecum[:], cum[:], AF.Exp)
                        k.act(ecend[:], cend[:], AF.Exp)
                        k.tt(edd[:], cend[:], cum[:], ALU.subtract)
                        k.act(edd[:], edd[:], AF.Exp)
                        k.tt(xdt[:], xtm[:], dtv[:].unsqueeze(2).to_broadcast([128, 16, 64]), ALU.mult)
                        k.tt(DEM[:], ident[:].unsqueeze(1).to_broadcast([128, 16, 128]),
                             cum[:].unsqueeze(2).to_broadcast([128, 16, 128]), ALU.mult, eng="gpsimd")
                        for q4 in range(4):
                            k.mm(PS[q4][:], ones[:], DEM[:, q4 * 4:(q4 + 1) * 4, :].rearrange("p h t -> p (h t)"))
                        for q4 in range(4):
                            k.tt(DEM[:, q4 * 4:(q4 + 1) * 4, :], PS[q4][:].rearrange("p (h t) -> p h t", t=128),
                                 cum[:, q4 * 4:(q4 + 1) * 4].unsqueeze(2).to_broadcast([128, 4, 128]), ALU.subtract)
                        k.tt(DEM[:], DEM[:], mk[:].unsqueeze(1).to_broadcast([128, 16, 128]), ALU.mult, eng="gpsimd")
                        k.act(DEM[:], DEM[:], AF.Exp)
                        for g in range(2):
                            k.mm(PS[6][:, 128 + g * 128:256 + g * 128], BT[:, g, :], CT[:, g, :])
                        k.tt(Gm[:], PS[6][:, 128:384].rearrange("p (g t) -> p g t", t=128),
                             mk[:].unsqueeze(1).to_broadcast([128, 2, 128]), ALU.mult)
                        for g in range(2):
                            k.tt(DEM[:, g * 8:(g + 1) * 8, :], DEM[:, g * 8:(g + 1) * 8, :],
                                 Gm[:, g, :].unsqueeze(1).to_broadcast([128, 8, 128]), ALU.mult,
                                 eng=("vector" if g == 0 else "gpsimd"))
                        for h in range(16):
                            ps = PS[4 + h // 8]
                            k.mm(ps[:, (h % 8) * 64:(h % 8 + 1) * 64], DEM[:, h, :], xdt[:, h, :])
                        for g in range(2):
                            k.mm(PS[g][:], CT[:, g, :], Sst[:, g * 8:(g + 1) * 8, :].rearrange("p h e -> p (h e)"))
                        for g in range(2):
                            hs = slice(g * 8, (g + 1) * 8)
                            k.tt(yacc[:, hs, :], PS[g][:].rearrange("p (h e) -> p h e", e=64),
                                 ecum[:, hs].unsqueeze(2).to_broadcast([128, 8, 64]), ALU.mult)
                            k.tt(yacc[:, hs, :], yacc[:, hs, :], PS[4 + g][:].rearrange("p (h e) -> p h e", e=64), ALU.add)
                        k.tt(xdec[:], xdt[:], edd[:].unsqueeze(2).to_broadcast([128, 16, 64]), ALU.mult, eng="gpsimd")
                        for g in range(2):
                            k.mm(PS[2 + g][:], btm[:, g * 128:(g + 1) * 128],
                                 xdec[:, g * 8:(g + 1) * 8, :].rearrange("p h e -> p (h e)"))
                        k.tt(Sst[:], Sst[:], ecend[:].unsqueeze(2).to_broadcast([128, 16, 64]), ALU.mult, eng="gpsimd")
                        for g in range(2):
                            hs = slice(g * 8, (g + 1) * 8)
                            k.tt(Sst[:, hs, :], Sst[:, hs, :], PS[2 + g][:].rearrange("p (h e) -> p h e", e=64), ALU.add)
                        if d == 0:
                            k.dma(yfwd_m[r0:r0 + 128, :], yacc[:].rearrange("p h e -> p (h e)"), "sync")
                        else:
                            k.dma(yf[:].rearrange("p h e -> p (h e)"), yfwd_m[r0:r0 + 128, :], "sync")
                            k.tt(yacc[:], yacc[:], yf[:], ALU.add, eng="gpsimd")
                            k.tt(yf[:], xtm[:], ab[:, 64:80].unsqueeze(2).to_broadcast([128, 16, 64]), ALU.mult, eng="gpsimd")
                            k.tt(yacc[:], yacc[:], yf[:], ALU.add, eng="gpsimd")
                            for hf in range(2):
                                proj_tm(wz[:, :, hf * 512:(hf + 1) * 512], 512, c, PS[7])
                                k.act(zs[:, hf * 512:(hf + 1) * 512], PS[7][:], AF.Silu)
                            yv = yacc[:].rearrange("p h e -> p (h e)")
                            k.tt(yv, yv, zs[:], ALU.mult)
                            k.memset(ss[:], 0.0)
                            for g in range(2):
                                k.act(sq[:], yv[:, g * 512:(g + 1) * 512], AF.Square, accum=ss[:, g:g + 1])
                            k.act(ss[:], ss[:], AF.Sqrt, bias=epsc[:, 0:1], scale=1.0 / 512)
                            k.recip(ss[:], ss[:])
                            for g in range(2):
                                k.stt(yv[:, g * 512:(g + 1) * 512], yv[:, g * 512:(g + 1) * 512], ss[:, g:g + 1],
                                      ng[:, g * 512:(g + 1) * 512], ALU.mult, ALU.mult)
                            for cb in range(8):
                                pst = PS[6 + cb % 2]
                                k.tr(pst[:, 0:128], yv[:, cb * 128:(cb + 1) * 128], ident[:])
                                k.cp(ytb[:, cb, :], pst[:, 0:128], eng=("scalar" if cb % 2 else "vector"))
                            k.dma(ymix[1024:2048, r0:r0 + 128].rearrange("(cb p) t -> p cb t", p=128), ytb[:], "sync")
                k.S.barrier()

        gtm = dscr("gtm", [NT, 1024])
        gfm = dscr("gfm", [1024, NT])
        GQ = 512

        GC = os.environ.get("GDNCUT", "")

        def gdn_mixer(l):
            with ExitStack() as s2:
                def T(name, shape, dt=F32):
                    return s2.enter_context(sbt("ga" + name, list(shape), dt))
                cw = T("cw", [128, 12, 5])
                wt = [T("wt%d" % i, [128, 8, 128], BF16) for i in range(2)]
                rawp = T("rawp", [128, PADL], BF16); acc = T("acc", [128, NT])
                dg = T("dg", [128, 5, 128], BF16)
                sq = T("sq", [128, 512]); rs = T("rs", [128, 512])
                tmall = T("tmall", [128, NCH, 128])
                k.dma(cw[:], gdn_cw[l], "sync")
                k.memset(rawp[:], 0.0)
                for cb in range(12):
                    w = wt[cb % 2]
                    load_win(w, l, GQ + cb * 128, 128)
                    for ti, (lo, n) in enumerate(TT):
                        ps = PS[ti % 4]
                        proj_fm(w, 128, ti, ps)
                        o = pad_off(lo)
                        k.cp(rawp[:, o:o + n], ps[:, :n], eng=("scalar" if ti % 2 else "vector"))
                    conv_pe(rawp, dg, cw, cb, acc, None)
                    if cb < 8:
                        for ti, (lo, n) in enumerate(TT):
                            ps = PS[4 + ti % 2]
                            k.tt(sq[:, :n], acc[:, lo:lo + n], acc[:, lo:lo + n], ALU.mult, eng="gpsimd")
                            k.mm(ps[:, :n], ones[:], sq[:, :n])
                            k.act(rs[:, :n], ps[:, :n], AF.Sqrt, bias=epsc[:, 0:1], scale=1.0)
                            k.recip(rs[:, :n], rs[:, :n])
                            if cb < 4:
                                k.stt(acc[:, lo:lo + n], acc[:, lo:lo + n], 128.0 ** -0.5, rs[:, :n], ALU.mult, ALU.mult)
                            else:
                                k.tt(acc[:, lo:lo + n], acc[:, lo:lo + n], rs[:, :n], ALU.mult)
                        k.dma(gfm[cb * 128:(cb + 1) * 128, :], acc[:], "sync")
                    if cb >= 4:
                        for c in range(NCH):
                            ps = PS[6 + c % 2]
                            k.tr(ps[:, 0:128], acc[:, c * 128:(c + 1) * 128], ident[:])
                            k.cp(tmall[:, c, :], ps[:, 0:128], eng=("vector" if c % 2 else "scalar"))
                        k.dma(gtm[:, (cb - 4) * 128:(cb - 3) * 128].rearrange("(c p) f -> p c f", p=128), tmall[:], "sync")
                k.S.barrier()
            if GC == "s1":
                return
            with ExitStack() as s2:
                def T(name, shape, dt=F32):
                    return s2.enter_context(sbt("gb" + name, list(shape), dt))
                wz = T("wz", [128, 8, 512], BF16)
                wab = T("wab", [128, 8, 16], BF16)
                abp = T("abp", [128, 16]); ng = T("ng", [128, 128]); nega = T("nega", [128, 8])
                Sst = T("S", [128, 4, 128])
                ggL = [T("gg%d" % i, [128, 4]) for i in range(2)]
                betaL = [T("beta%d" % i, [128, 4]) for i in range(2)]
                gcL = [T("gc%d" % i, [128, 4]) for i in range(2)]
                egcL = [T("egc%d" % i, [128, 4]) for i in range(2)]
                zsL = [T("zs%d" % i, [128, 512]) for i in range(2)]
                HB = []
                for h in range(4):
                    b = {}
                    for nm in ("knT", "qnT", "ktm", "vtm", "EdT", "AT", "P", "PT", "wT", "vn", "qdT", "eRg", "kdec",
                               "oo", "of", "sq"):
                        b[nm] = T("%s%d" % (nm, h), [128, 128])
                    b["D2"] = T("D2%d" % h, [128, 256]); b["R"] = T("R%d" % h, [128, 256])
                    b["sc1"] = T("sc1%d" % h, [128, 8]); b["otb"] = T("otb%d" % h, [128, 128], BF16)
                    HB.append(b)
                k.dma(abp[:], gdn_ab[l], "sync")
                k.dma(ng[:], gdn_ng[l], "sync")
                load_win(wz, l, 2048, 512)
                load_win(wab, l, 2560, 16)
                k.act(nega[:], abp[:, 0:8], AF.Exp)
                k.ts(nega[:], nega[:], -1.0, ALU.mult)

                def prep(d, c, par):
                    gg, beta, gc, egc, zs = ggL[par], betaL[par], gcL[par], egcL[par], zsL[par]
                    proj_tm(wab, 16, c, PS[7])
                    k.tt(gg[:], PS[7][:, d * 4:(d + 1) * 4], abp[:, 8 + d * 4:12 + d * 4], ALU.add)
                    k.act(gg[:], gg[:], AF.Exp)
                    k.act(gg[:], gg[:], AF.Ln, bias=ones[:, 0:1])
                    k.tt(gg[:], gg[:], nega[:, d * 4:(d + 1) * 4], ALU.mult)
                    k.act(beta[:], PS[7][:, 8 + d * 4:12 + d * 4], AF.Sigmoid)
                    k.mm(PS[7][:, 32:36], MASK[d][:], gg[:])
                    k.cp(gc[:], PS[7][:, 32:36])
                    k.act(egc[:], gc[:], AF.Exp)
                    if d == 1:
                        proj_tm(wz, 512, c, PS[6])
                        k.act(zs[:], PS[6][:], AF.Silu)

                def unit(d, c, par, h):
                    beta, gc, egc, zs = betaL[par], gcL[par], egcL[par], zsL[par]
                    mk = MASK[d]; e_i = ENDI[d]
                    r0 = c * 128
                    B = HB[h]
                    knT, qnT, ktm, vtm, D2, EdT, AT = B["knT"], B["qnT"], B["ktm"], B["vtm"], B["D2"], B["EdT"], B["AT"]
                    P, PT, R, wT, vn, qdT, eRg, kdec = B["P"], B["PT"], B["R"], B["wT"], B["vn"], B["qdT"], B["eRg"], B["kdec"]
                    sc1, oo, of, sq, otb = B["sc1"], B["oo"], B["of"], B["sq"], B["otb"]
                    pA = PS[h]; pB = PS[4 + h]
                    k.dma(qnT[:], gfm[h * 128:(h + 1) * 128, r0:r0 + 128], "sync")
                    k.dma(knT[:], gfm[512 + h * 128:640 + h * 128, r0:r0 + 128], "sync")
                    k.dma(ktm[:], gtm[r0:r0 + 128, h * 128:(h + 1) * 128], "scalar")
                    k.dma(vtm[:], gtm[r0:r0 + 128, 512 + h * 128:640 + h * 128], "scalar")
                    yield
                    k.ts(D2[:, 0:128], ident[:], gc[:, h:h + 1], ALU.mult)
                    k.ts(D2[:, 128:256], ident[:], beta[:, h:h + 1], ALU.mult)
                    yield
                    k.mm(pA[:, 0:256], ones[:], D2[:])
                    Rg = pA[:, 0:128]; Rb = pA[:, 128:256]
                    k.mm(pB[:, 0:128], knT[:], knT[:])
                    k.mm(pB[:, 128:256], knT[:], qnT[:])
                    yield
                    k.ts(EdT[:], Rg, gc[:, h:h + 1], ALU.subtract)
                    yield
                    k.tt(EdT[:], EdT[:], mk[:], ALU.mult)
                    yield
                    k.act(EdT[:], EdT[:], AF.Exp)
                    k.cp(sc1[:, 4:5], pA[:, e_i:e_i + 1])
                    yield
                    k.tt(EdT[:], EdT[:], mk[:], ALU.mult)
                    yield
                    k.tt(AT[:], pB[:, 128:256], EdT[:], ALU.mult)
                    k.tt(PT[:], pB[:, 0:128], EdT[:], ALU.mult)
                    yield
                    k.tt(PT[:], PT[:], Rb, ALU.mult)
                    k.act(eRg[:], Rg, AF.Exp)
                    yield
                    k.tt(PT[:], PT[:], NSTR[d][:], ALU.mult)
                    k.ts(R[:, 0:128], vtm[:], beta[:, h:h + 1], ALU.mult)
                    k.ts(R[:, 128:256], ktm[:], beta[:, h:h + 1], ALU.mult, egc[:, h:h + 1], ALU.mult)
                    yield
                    k.tr(pB[:, 256:384], PT[:], ident[:])
                    yield
                    k.cp(P[:], pB[:, 256:384])
                    k.tt(qdT[:], qnT[:], eRg[:], ALU.mult)
                    yield
                    for lev in range(7):
                        k.mm(pA[:, 256:512], PT[:], R[:])
                        if lev < 6:
                            k.mm(pB[:, 0:128], PT[:], P[:])
                            k.mm(pB[:, 128:256], P[:], PT[:])
                        yield
                        k.tt(R[:], R[:], pA[:, 256:512], ALU.add)
                        if lev < 6:
                            k.cp(P[:], pB[:, 0:128])
                            k.cp(PT[:], pB[:, 128:256])
                        yield
                    k.tr(pB[:, 256:384], R[:, 128:256], ident[:])
                    k.ts(sc1[:, 0:1], gc[:, h:h + 1], -1.0, ALU.mult, sc1[:, 4:5], ALU.add)
                    yield
                    k.cp(wT[:], pB[:, 256:384])
                    k.act(sc1[:, 1:2], sc1[:, 0:1], AF.Exp)
                    k.act(sc1[:, 2:3], sc1[:, 4:5], AF.Exp)
                    yield
                    k.mm(pB[:, 384:512], wT[:], Sst[:, h, :])
                    k.ts(kdec[:], ktm[:], sc1[:, 1:2], ALU.mult)
                    yield
                    k.tt(vn[:], R[:, 0:128], pB[:, 384:512], ALU.subtract)
                    yield
                    k.mm(pB[:, 384:512], qdT[:], Sst[:, h, :], True, False)
                    k.mm(pB[:, 384:512], AT[:], vn[:], False, True)
                    k.mm(pB[:, 256:384], kdec[:], vn[:])
                    yield
                    if d == 0:
                        k.cp(oo[:], pB[:, 384:512])
                    else:
                        k.dma(of[:], yfwd_g[h, r0:r0 + 128, :], "sync")
                        k.tt(oo[:], of[:], pB[:, 384:512], ALU.add)
                    k.stt(Sst[:, h, :], Sst[:, h, :], sc1[:, 2:3], pB[:, 256:384], ALU.mult, ALU.add)
                    yield
                    if d == 0:
                        k.dma(yfwd_g[h, r0:r0 + 128, :], oo[:], "sync")
                    else:
                        k.memset(sc1[:, 3:4], 0.0)
                        yield
                        k.act(sq[:], oo[:], AF.Square, accum=sc1[:, 3:4])
                        yield
                        k.act(sc1[:, 3:4], sc1[:, 3:4], AF.Sqrt, bias=epsc[:, 0:1], scale=1.0 / 128)
                        yield
                        k.recip(sc1[:, 3:4], sc1[:, 3:4])
                        yield
                        k.stt(oo[:], oo[:], sc1[:, 3:4], ng[:], ALU.mult, ALU.mult)
                        yield
                        k.tt(oo[:], oo[:], zs[:, h * 128:(h + 1) * 128], ALU.mult)
                        yield
                        k.tr(pB[:, 256:384], oo[:], ident[:])
                        yield
                        k.cp(otb[:], pB[:, 256:384], eng="scalar")
                        yield
                        k.dma(ymix[512 + h * 128:640 + h * 128, r0:r0 + 128], otb[:], "sync")

                for d in range(2):
                    k.memset(Sst[:], 0.0)
                    order = CH_ORDER[d]
                    prep(d, order[0], 0)
                    for ci, c in enumerate(order):
                        par = ci % 2
                        gens = [unit(d, c, par, h) for h in range(4)]
                        first = True
                        while gens:
                            nxt = []
                            for g in gens:
                                try:
                                    next(g)
                                    nxt.append(g)
                                except StopIteration:
                                    pass
                            gens = nxt
                            if first and ci + 1 < len(order):
                                prep(d, order[ci + 1], 1 - par)
                                first = False
                k.S.barrier()

        MIX = {"s5": s5_mixer, "gdn": gdn_mixer, "m2": m2_mixer}

        DBG = os.environ.get("DBGDUMP", "").split(",")

        def dbg_dump(name, ap, shape, dt=F32):
            if name not in DBG:
                return
            o = nc.dram_tensor("dbg_" + name, list(shape), dt, kind="ExternalOutput").ap()
            k.dma(o, ap, "sync")

        def dump_x():
            xo = dbgx.rearrange("(kt p) t -> p kt t", p=128)
            for kt in range(8):
                k.dma(xo[:, kt, :], xT[:, kt, :], "sync")

        skip = os.environ.get("SKIPMIX", "").split(",")
        for l in range(nlayers):
            odd = (l % 2 == 1)
            lastl = (l == nlayers - 1)
            adaln(l)
            rmsnorm_mod(l, n1g[l], 0, 1, odd)
            dbg_dump("h%d" % l, hT[:], [128, 8, NT], BF16)
            dbg_dump("mod%d" % l, mod[:], [128, 2, 48])
            dbg_dump("gs%d" % l, gs[:], [128, 2, 8])
            if "s5" not in skip:
                MIX["s5"](l)
            if "gdn" not in skip:
                MIX["gdn"](l)
            if "m2" not in skip:
                MIX["m2"](l)
            if lastl and stop == "mix":
                break
            out_proj(l, odd)
            if lastl and stop == "oproj":
                dump_x()
                break
            rmsnorm_mod(l, n2g[l], 3, 4, False)
            if l % 2 == 0:
                ffn_dense(l // 2)
            else:
                ffn_moe(l // 2)
            if lastl and stop == "ffn":
                dump_x()
                break
        if stop is None:
            rmsnorm_mod(0, fng, 0, 0, False, final=True)
        k.S.emit()
    return nc


_NC_CACHE = {}


def _prep_shared(inp):
    f = lambda a: np.ascontiguousarray(np.asarray(a, dtype=np.float32))
    g = {}
    g["ada_w"] = f(inp["ada_w"])
    g["ada_b"] = f(np.asarray(inp["ada_b"]).reshape(DEPTH, 48, 128).transpose(0, 2, 1))
    g["n1g"] = f(np.asarray(inp["norm1_g"]).reshape(DEPTH, 8, 128).transpose(0, 2, 1))
    g["n2g"] = f(np.asarray(inp["norm2_g"]).reshape(DEPTH, 8, 128).transpose(0, 2, 1))
    g["fng"] = f(np.asarray(inp["final_norm_g"]).reshape(8, 128).T)
    g["w_in"] = f(inp["w_in"])
    g["w_out"] = f(inp["w_out"])
    g["ffn_wg"] = f(inp["ffn_w_gate"]); g["ffn_wu"] = f(inp["ffn_w_up"]); g["ffn_wd"] = f(inp["ffn_w_down"])
    g["moe_r"] = f(inp["moe_router"])
    g["moe_wg"] = f(inp["moe_w_gate"]); g["moe_wu"] = f(inp["moe_w_up"]); g["moe_wd"] = f(inp["moe_w_down"])
    lam_re = np.asarray(inp["s5_lam_re"]); lam_im = np.asarray(inp["s5_lam_im"]); log_dt = np.asarray(inp["s5_log_dt"])
    b_re = np.asarray(inp["s5_b_re"]); b_im = np.asarray(inp["s5_b_im"])
    c_re = np.asarray(inp["s5_c_re"]); c_im = np.asarray(inp["s5_c_im"])
    s5_lam = np.zeros((DEPTH, 128, 3, 32), np.float32)
    s5_B = np.zeros((DEPTH, 32, 2, 128, 128), np.float32)
    s5_C = np.zeros((DEPTH, 32, 2, 128, 128), np.float32)
    for st in range(16):
        for d in range(2):
            u = st * 2 + d
            for g2 in range(2):
                gi = 2 * st + g2
                ps = slice(g2 * 64, (g2 + 1) * 64)
                s5_lam[:, ps, 0, u] = lam_re[:, d, gi, :]
                s5_lam[:, ps, 1, u] = lam_im[:, d, gi, :]
                s5_lam[:, ps, 2, u] = log_dt[:, d, gi][:, None]
                ch0 = 16 * (2 * (st % 4) + g2)
                s5_B[:, u, 0, ps, ch0:ch0 + 16] = b_re[:, d, gi]
                s5_B[:, u, 1, ps, ch0:ch0 + 16] = b_im[:, d, gi]
                s5_C[:, u, 0, ps, ch0:ch0 + 16] = c_re[:, d, gi].transpose(0, 2, 1)
                s5_C[:, u, 1, ps, ch0:ch0 + 16] = c_im[:, d, gi].transpose(0, 2, 1)
    g["s5_lam"] = s5_lam; g["s5_B"] = s5_B; g["s5_C"] = s5_C
    g["s5_d"] = f(np.asarray(inp["s5_d"]).reshape(DEPTH, 4, 128).transpose(0, 2, 1))
    g["s5_glu"] = f(inp["s5_w_glu"])
    g["gdn_cw"] = f(np.asarray(inp["gdn_conv_w"]).reshape(DEPTH, 5, 12, 128).transpose(0, 3, 2, 1))
    ab = np.concatenate([np.asarray(inp["gdn_a_log"]).reshape(DEPTH, 8), np.asarray(inp["gdn_dt_bias"]).reshape(DEPTH, 8)], 1)
    g["gdn_ab"] = f(np.broadcast_to(ab[:, None, :], (DEPTH, 128, 16)))
    g["gdn_ng"] = f(np.broadcast_to(np.asarray(inp["gdn_norm_g"])[:, None, :], (DEPTH, 128, 128)))
    cw = np.asarray(inp["m2_conv_w"]).reshape(DEPTH, 5, 12, 128).transpose(0, 3, 2, 1)
    cb = np.asarray(inp["m2_conv_b"]).reshape(DEPTH, 12, 128).transpose(0, 2, 1)[..., None]
    g["m2_cw"] = f(np.concatenate([cw, cb], axis=3))
    ab = np.concatenate([np.asarray(inp["m2_a_log"]).reshape(DEPTH, 32), np.asarray(inp["m2_dt_bias"]).reshape(DEPTH, 32),
                         np.asarray(inp["m2_d"]).reshape(DEPTH, 16)], 1)
    g["m2_ab"] = f(np.broadcast_to(ab[:, None, :], (DEPTH, 128, 80)))
    g["m2_ng"] = f(np.broadcast_to(np.asarray(inp["m2_norm_g"])[:, None, :], (DEPTH, 128, 1024)))
    return g


def kernel(**inp):
    n = 8
    x = np.asarray(inp["x"], dtype=np.float32)
    ctx = np.asarray(inp["ctx"], dtype=np.float32)
    c = np.asarray(inp["c"], dtype=np.float32)
    c_ctx = np.asarray(inp["c_ctx"], dtype=np.float32)
    shared = _prep_shared(inp)
    if "nc" not in _NC_CACHE:
        _NC_CACHE["nc"] = build_program()
    nc = _NC_CACHE["nc"]
    in_maps = []
    for b in range(n):
        m = dict(shared)
        m["xT"] = np.ascontiguousarray(np.concatenate([ctx[b], x[b]], axis=0).T)
        cs = np.stack([c[b].reshape(8, 128).T, c_ctx.reshape(8, 128).T], axis=2)
        m["cs"] = np.ascontiguousarray(cs.astype(np.float32))
        in_maps.append(m)
    res = run_bass_kernel_spmd(nc, in_maps, core_ids=list(range(n)))
    out = np.stack([np.asarray(r["outT"], dtype=np.float32).T for r in res.results], axis=0)
    return np.ascontiguousarray(out)
```

```python
import math
import os
from contextlib import ExitStack
import numpy as np
import concourse.bass as bass
import concourse.mybir as mybir
from concourse.bass_utils import run_bass_kernel_spmd

F32 = mybir.dt.float32
BF16 = mybir.dt.bfloat16
I32 = mybir.dt.int32
AF = mybir.ActivationFunctionType
ALU = mybir.AluOpType
AX = mybir.AxisListType

COMPUTE = ["tensor", "vector", "scalar", "gpsimd"]
ENGS = ["sync", "tensor", "vector", "scalar", "gpsimd"]
DMA_RING = 6
SAME_ENGINE_SYNC = True

D = 1024
NT = 2304
NCTX = 256
DEPTH = 4
DIN = 5168
DFF = 2816
NE = 8
TT = [(0, 256), (256, 512), (768, 512), (1280, 512), (1792, 512)]
NCH = 18
EPS = 1e-6
TWO_PI = 2.0 * math.pi


def _key(k):
    if isinstance(k, (str, tuple)):
        return k
    t = getattr(k, "tensor", k)
    return t.name


class Sched:
    def __init__(self, nc):
        self.nc = nc
        self.ops = {e: [] for e in ENGS}
        self.cnt = {e: 0 for e in COMPUTE}
        self.seen = {e: {} for e in ENGS}
        self.last_w = {}
        self.readers = {}
        self.ring_tot = {}
        self.ring_pos = {e: 0 for e in ENGS}
        self.ring_know = {}
        self.sem_names = ["c_" + e for e in COMPUTE]
        for e in ("sync", "scalar", "gpsimd"):
            for i in range(DMA_RING):
                n = "d_%s_%d" % (e, i)
                self.sem_names.append(n)
                self.ring_tot[n] = 0
        self.sems = {}
        self.n_ops = 0

    def _need(self, eng, tok, waits, is_dma=False):
        s, v, know = tok
        if self.seen[eng].get(s, 0) >= v:
            return
        if (not is_dma) and s == "c_" + eng and (eng == "tensor" or not SAME_ENGINE_SYNC):
            return
        waits[s] = max(waits.get(s, 0), v)
        sn = self.seen[eng]
        for ks, kv in know.items():
            if sn.get(ks, 0) < kv:
                sn[ks] = kv
        sn[s] = max(sn.get(s, 0), v)

    def _deps(self, eng, reads, writes, waits, is_dma=False):
        for k in reads:
            k = _key(k)
            t = self.last_w.get(k)
            if t is not None:
                self._need(eng, t, waits, is_dma)
            if isinstance(k, str) and k.startswith("ps"):
                for t in list(self.readers.get(k, {}).values()):
                    if t[0] != "c_" + eng:
                        self._need(eng, t, waits, is_dma)
        for k in writes:
            k = _key(k)
            t = self.last_w.get(k)
            if t is not None:
                self._need(eng, t, waits, is_dma)
            for t in list(self.readers.get(k, {}).values()):
                self._need(eng, t, waits, is_dma)

    def _publish(self, tok, reads, writes):
        for k in reads:
            self.readers.setdefault(_key(k), {})[tok[0]] = tok
        for k in writes:
            k = _key(k)
            self.last_w[k] = tok
            self.readers[k] = {}

    def op(self, eng, fn, reads=(), writes=()):
        waits = {}
        self._deps(eng, reads, writes, waits)
        self.cnt[eng] += 1
        s = "c_" + eng
        know = dict(self.seen[eng])
        know[s] = self.cnt[eng]
        tok = (s, self.cnt[eng], know)
        self.ops[eng].append((waits, fn, s, 1))
        self._publish(tok, reads, writes)
        self.n_ops += 1
        return tok

    def dma(self, eng, fn, reads=(), writes=()):
        waits = {}
        self._deps(eng, reads, writes, waits, True)
        i = self.ring_pos[eng]
        self.ring_pos[eng] = (i + 1) % DMA_RING
        s = "d_%s_%d" % (eng, i)
        if self.ring_tot[s] > 0 and self.seen[eng].get(s, 0) < self.ring_tot[s]:
            waits[s] = self.ring_tot[s]
            self.seen[eng][s] = self.ring_tot[s]
            for ks, kv in self.ring_know.get(s, {}).items():
                if self.seen[eng].get(ks, 0) < kv:
                    self.seen[eng][ks] = kv
        self.ring_tot[s] += 16
        know = dict(self.seen[eng])
        self.ring_know[s] = know
        tok = (s, self.ring_tot[s], know)
        self.ops[eng].append((waits, fn, s, 16))
        self._publish(tok, reads, writes)
        self.n_ops += 1
        return tok

    def barrier(self):
        toks = []
        for e in COMPUTE:
            if self.cnt[e] > 0:
                toks.append(("c_" + e, self.cnt[e], {}))
        for s, v in self.ring_tot.items():
            if v > 0:
                toks.append((s, v, {}))
        for e in ENGS:
            waits = {}
            for t in toks:
                self._need(e, t, waits)
            if waits:
                self.ops[e].append((waits, None, None, 0))

    def emit(self):
        nc = self.nc
        self.barrier()
        with ExitStack() as st:
            for n in self.sem_names:
                self.sems[n] = st.enter_context(nc.semaphore(n))
            block = st.enter_context(nc.Block())
            sems = self.sems

            def run(eng_name):
                def body(eng):
                    for waits, fn, s, inc in self.ops[eng_name]:
                        for ws, wv in waits.items():
                            eng.wait_ge(sems[ws], wv)
                        if fn is not None:
                            ins = fn(eng)
                            ins.then_inc(sems[s], inc)
                return body

            block.sync(run("sync"))
            block.tensor(run("tensor"))
            block.vector(run("vector"))
            block.scalar(run("scalar"))
            block.gpsimd(run("gpsimd"))


def _aps(*xs):
    return [x for x in xs if x is not None and not isinstance(x, (int, float))]


class K:
    def __init__(self, nc):
        self.nc = nc
        self.S = Sched(nc)
        self.rr = 0

    def mm(self, ps, lhsT, rhs, start=True, stop=True):
        rd = [lhsT, rhs] + ([] if start else [ps])
        return self.S.op("tensor", lambda e: e.matmul(ps, lhsT=lhsT, rhs=rhs, start=start, stop=stop), rd, [ps])

    def tr(self, ps, in_, ident):
        return self.S.op("tensor", lambda e: e.transpose(ps, in_, ident), [in_, ident], [ps])

    def act(self, out, in_, func, bias=None, scale=1.0, accum=None, eng="scalar"):
        kw = {}
        if bias is not None:
            kw["bias"] = bias
        if accum is not None:
            kw["accum_out"] = accum
        return self.S.op("scalar", lambda e: e.activation(out=out, in_=in_, func=func, scale=scale, **kw),
                         _aps(in_, bias, scale), _aps(out, accum))

    def tt(self, out, a, b, op, eng="vector"):
        return self.S.op(eng, lambda e: e.tensor_tensor(out=out, in0=a, in1=b, op=op), [a, b], [out])

    def ts(self, out, a, s1, op0, s2=None, op1=None, eng="vector", accum=None):
        def f(e):
            kw = {}
            if op1 is not None:
                kw["op1"] = op1
            if accum is not None:
                kw["accum_out"] = accum
            return e.tensor_scalar(out=out, in0=a, scalar1=s1, scalar2=s2, op0=op0, **kw)
        return self.S.op(eng, f, _aps(a, s1, s2), _aps(out, accum))

    def stt(self, out, a, s, b, op0, op1, eng="vector"):
        eng = "vector"
        return self.S.op(eng, lambda e: e.scalar_tensor_tensor(out=out, in0=a, scalar=s, in1=b, op0=op0, op1=op1),
                         _aps(a, s, b), [out])

    def cp(self, out, in_, eng="vector"):
        if eng == "scalar":
            return self.S.op(eng, lambda e: e.copy(out=out, in_=in_), [in_], [out])
        return self.S.op(eng, lambda e: e.tensor_copy(out=out, in_=in_), [in_], [out])

    def memset(self, out, v, eng="gpsimd"):
        return self.S.op(eng, lambda e: e.memset(out, v), [], [out])

    def red(self, out, in_, op, eng="vector"):
        return self.S.op(eng, lambda e: e.tensor_reduce(out=out, in_=in_, axis=AX.X, op=op), [in_], [out])

    def recip(self, out, in_):
        return self.S.op("vector", lambda e: e.reciprocal(out=out, in_=in_), [in_], [out])

    def scan(self, out, d0, d1, init):
        return self.S.op("vector", lambda e: e.tensor_tensor_scan(out=out, data0=d0, data1=d1, initial=init,
                                                                  op0=ALU.mult, op1=ALU.add),
                         _aps(d0, d1, init), [out])

    def dma(self, out, in_, q="sync"):
        return self.S.dma(q, lambda e: e.dma_start(out=out, in_=in_), [in_], [out])

    def ev(self):
        self.rr ^= 1
        return "vector" if self.rr else "gpsimd"


def perm_ap(t3, kt, lo, n):
    c0 = (lo - NCTX) // 32
    ncol = n // 32
    base = t3[:, kt, NCTX:NT]
    v = base.rearrange("p (r c) -> p c r", c=64)
    return v[:, c0:c0 + ncol, :]


def build_program(nlayers=DEPTH, stop=None):
    nc = bass.Bass("TRN2", target_bir_lowering=False)
    k = K(nc)
    _cnt = [0]

    def sbt(name, shape, dt):
        _cnt[0] += 1
        return nc.sbuf_tensor("%s_%d" % (name, _cnt[0]), shape, dt)

    def din(name, shape, dt=F32):
        return nc.dram_tensor(name, list(shape), dt, kind="ExternalInput").ap()

    def dscr(name, shape, dt=F32):
        return nc.dram_tensor(name, list(shape), dt, kind="Internal").ap()

    xT_in = din("xT", [D, NT])
    cs_in = din("cs", [128, 8, 2])
    ada_w = din("ada_w", [DEPTH, D, 6 * D])
    ada_b = din("ada_b", [DEPTH, 128, 48])
    n1g = din("n1g", [DEPTH, 128, 8])
    n2g = din("n2g", [DEPTH, 128, 8])
    fng = din("fng", [128, 8])
    w_in = din("w_in", [DEPTH, D, DIN])
    w_out = din("w_out", [DEPTH, 2048, D])
    ffn_wg = din("ffn_wg", [2, D, DFF])
    ffn_wu = din("ffn_wu", [2, D, DFF])
    ffn_wd = din("ffn_wd", [2, DFF, D])
    moe_r = din("moe_r", [2, D, NE])
    moe_wg = din("moe_wg", [2, NE, D, DFF])
    moe_wu = din("moe_wu", [2, NE, D, DFF])
    moe_wd = din("moe_wd", [2, NE, DFF, D])
    s5_lam = din("s5_lam", [DEPTH, 128, 3, 32])
    s5_B = din("s5_B", [DEPTH, 32, 2, 128, 128])
    s5_C = din("s5_C", [DEPTH, 32, 2, 128, 128])
    s5_d = din("s5_d", [DEPTH, 128, 4])
    s5_glu = din("s5_glu", [DEPTH, 512, 512])
    gdn_cw = din("gdn_cw", [DEPTH, 128, 12, 5])
    gdn_ab = din("gdn_ab", [DEPTH, 128, 16])
    gdn_ng = din("gdn_ng", [DEPTH, 128, 128])
    m2_cw = din("m2_cw", [DEPTH, 128, 12, 6])
    m2_ab = din("m2_ab", [DEPTH, 128, 80])
    m2_ng = din("m2_ng", [DEPTH, 128, 1024])
    out_T = nc.dram_tensor("outT", [D, 2048], F32, kind="ExternalOutput").ap()

    if stop == "mix":
        ymix = nc.dram_tensor("ymix", [2048, NT], BF16, kind="ExternalOutput").ap()
    else:
        ymix = dscr("ymix", [2048, NT], BF16)
    dbgx = nc.dram_tensor("dbgx", [D, NT], F32, kind="ExternalOutput").ap() if stop in ("oproj", "ffn", "norm1") else None
    yfwd_g = dscr("yfwd_g", [4, NT, 128])
    yfwd_m = dscr("yfwd_m", [NT, 1024])

    with ExitStack() as st:
        def sb(name, shape, dt=F32):
            return st.enter_context(sbt(name, list(shape), dt))

        def psum(name, shape=(128, 512), dt=F32):
            return st.enter_context(nc.psum_tensor(name, list(shape), dt))

        xT = sb("xTs", [128, 8, NT])
        hT = sb("hTs", [128, 8, NT], BF16)
        mod = sb("mod", [128, 2, 48])
        gs = sb("gs", [128, 2, 8])
        ident = sb("ident", [128, 128])
        identb = sb("identb", [128, 128], BF16)
        ones = sb("ones", [128, 128])
        epsc = sb("epsc", [128, 1])
        PS = [psum("ps%d" % i) for i in range(8)]

        DBG = os.environ.get("DBGDUMP", "").split(",")

        def dbg_dump(name, ap, shape, dt=F32):
            if name not in DBG:
                return
            o = nc.dram_tensor("dbg_" + name, list(shape), dt, kind="ExternalOutput").ap()
            k.dma(o, ap, "sync")

        k.memset(ident[:], 0.0)
        k.S.op("gpsimd", lambda e: e.affine_select(out=ident[:], in_=ident[:], pattern=[[-1, 128]],
                                                   compare_op=ALU.not_equal, fill=1.0, base=0,
                                                   channel_multiplier=1), [ident], [ident])
        k.cp(identb[:], ident[:])
        k.memset(ones[:], 1.0)
        k.memset(epsc[:], EPS)

        xv = xT_in.rearrange("(kt p) t -> p kt t", p=128)
        for kt in range(8):
            k.dma(xT[:, kt, :], xv[:, kt, :], "sync")

        csr = sb("csr", [128, 8, 2])
        k.dma(csr[:], cs_in, "sync")
        cs = sb("css", [128, 8, 2])
        k.act(cs[:], csr[:], AF.Silu)

        def adaln(l):
            with ExitStack() as s2:
                wb = [s2.enter_context(sbt("adaw%d" % i, [128, 8, 512], F32)) for i in range(2)]
                bb = s2.enter_context(sbt("adab", [128, 48], F32))
                k.dma(bb[:], ada_b[l], "sync")
                wv = ada_w[l].rearrange("(kt p) n -> p kt n", p=128)
                for blk in range(12):
                    w = wb[blk % 2]
                    k.dma(w[:], wv[:, :, blk * 512:(blk + 1) * 512], "sync" if blk % 2 == 0 else "scalar")
                    for j in range(4):
                        col = blk * 4 + j
                        ps = PS[col % 2]
                        for kt in range(8):
                            k.mm(ps[:, 0:2], w[:, kt, j * 128:(j + 1) * 128], cs[:, kt, :], kt == 0, kt == 7)
                        for jj in range(2):
                            k.ts(mod[:, jj, col:col + 1], ps[:, jj:jj + 1], bb[:, col:col + 1], ALU.add)
                k.S.barrier()

        def rmsnorm_mod(l, gsrc, shift_idx, scale_idx, permute, final=False):
            with ExitStack() as s2:
                g = s2.enter_context(sbt("ng", [128, 8], F32))
                sq = [s2.enter_context(sbt("sq%d" % i, [128, 512], F32)) for i in range(2)]
                rstd = s2.enter_context(sbt("rstd", [128, 512], F32))
                tmp = [s2.enter_context(sbt("nt%d" % i, [128, 512], F32)) for i in range(2)]
                k.dma(g[:], gsrc, "sync")
                if not final:
                    for j in range(2):
                        k.ts(gs[:, j, :], mod[:, j, scale_idx * 8:(scale_idx + 1) * 8], 1.0, ALU.add)
                        k.tt(gs[:, j, :], gs[:, j, :], g[:], ALU.mult)
                for ti, (lo, n) in enumerate(TT):
                    if final and ti == 0:
                        continue
                    ps = PS[2 + ti % 2]
                    for kt in range(8):
                        s = sq[kt % 2]
                        k.act(s[:, :n], xT[:, kt, lo:lo + n], AF.Square)
                        k.mm(ps[:, :n], ones[:], s[:, :n], kt == 0, kt == 7)
                    k.act(rstd[:, :n], ps[:, :n], AF.Sqrt, bias=epsc[:, 0:1], scale=1.0 / D)
                    k.recip(rstd[:, :n], rstd[:, :n])
                    j = 1 if ti == 0 else 0
                    for kt in range(8):
                        t = tmp[kt % 2]
                        k.tt(t[:, :n], xT[:, kt, lo:lo + n], rstd[:, :n], ALU.mult)
                        if final:
                            k.ts(t[:, :n], t[:, :n], g[:, kt:kt + 1], ALU.mult)
                            k.dma(out_T[kt * 128:(kt + 1) * 128, lo - NCTX:lo - NCTX + n], t[:, :n], "sync")
                        else:
                            if permute and ti > 0:
                                r0 = (lo - NCTX) // 64
                                dst = hT[:, kt, NCTX:NT].rearrange("p (c r) -> p r c", r=32)[:, r0:r0 + n // 64, :]
                                src = t[:, :n].rearrange("p (r c) -> p r c", c=64)
                            else:
                                dst = hT[:, kt, lo:lo + n]
                                src = t[:, :n]
                            k.ts(dst, src, gs[:, j, kt:kt + 1], ALU.mult,
                                 mod[:, j, shift_idx * 8 + kt:shift_idx * 8 + kt + 1], ALU.add)
                k.S.barrier()

        def out_proj(l, permute):
            with ExitStack() as s2:
                wo = s2.enter_context(sbt("wo", [128, 16, D], BF16))
                yb = [s2.enter_context(sbt("yb%d" % i, [128, 16, 512], BF16)) for i in range(2)]
                wv = w_out[l].rearrange("(kt p) n -> p kt n", p=128)
                for q4 in range(4):
                    k.dma(wo[:, q4 * 4:(q4 + 1) * 4, :], wv[:, q4 * 4:(q4 + 1) * 4, :], "gpsimd")
                yv = ymix.rearrange("(kt p) t -> p kt t", p=128)
                for ti, (lo, n) in enumerate(TT):
                    y = yb[ti % 2]
                    k.dma(y[:, :, :n], yv[:, :, lo:lo + n], "sync")
                    j = 1 if ti == 0 else 0
                    for nt in range(8):
                        ps = PS[nt % 4]
                        for kt in range(16):
                            k.mm(ps[:, :n], wo[:, kt, nt * 128:(nt + 1) * 128], y[:, kt, :n], kt == 0, kt == 15)
                        if permute and ti > 0:
                            dst = perm_ap(xT, nt, lo, n)
                            src = ps[:, :n].rearrange("p (c r) -> p c r", r=32)
                        else:
                            dst = xT[:, nt, lo:lo + n]
                            src = ps[:, :n]
                        k.stt(dst, src, mod[:, j, 16 + nt:17 + nt], dst, ALU.mult, ALU.add)
                k.S.barrier()

        def ffn_expert(wg, wu, wd, gbc, bufs):
            wgb, wub, wdb, hid, sgs = bufs
            wgv = wg.rearrange("(kt p) f -> p kt f", p=128)
            wuv = wu.rearrange("(kt p) f -> p kt f", p=128)
            wdv = wd.rearrange("(ft p) n -> p ft n", p=128)
            for fb in range(11):
                b = fb % 2
                k.dma(wgb[b][:], wgv[:, :, fb * 256:(fb + 1) * 256], "gpsimd")
                k.dma(wub[b][:], wuv[:, :, fb * 256:(fb + 1) * 256], "gpsimd")
                k.dma(wdb[b][:], wdv[:, fb * 2:(fb + 1) * 2, :], "gpsimd")
                for ti, (lo, n) in enumerate(TT):
                    j = 1 if ti == 0 else 0
                    hb = hid[ti % 2]
                    for f in range(2):
                        pg = PS[0 + f]
                        pu = PS[2 + f]
                        for kt in range(8):
                            k.mm(pg[:, :n], wgb[b][:, kt, f * 128:(f + 1) * 128], hT[:, kt, lo:lo + n], kt == 0, kt == 7)
                        for kt in range(8):
                            k.mm(pu[:, :n], wub[b][:, kt, f * 128:(f + 1) * 128], hT[:, kt, lo:lo + n], kt == 0, kt == 7)
                        sg = sgs[f]
                        k.act(sg[:, :n], pg[:, :n], AF.Silu)
                        if gbc is not None:
                            k.tt(sg[:, :n], sg[:, :n], gbc[:, lo:lo + n], ALU.mult, eng="gpsimd")
                        k.tt(hb[:, f, :n], sg[:, :n], pu[:, :n], ALU.mult)
                    for nt in range(8):
                        ps = PS[4 + nt % 4]
                        for f in range(2):
                            k.mm(ps[:, :n], wdb[b][:, f, nt * 128:(nt + 1) * 128], hb[:, f, :n], f == 0, f == 1)
                        k.stt(xT[:, nt, lo:lo + n], ps[:, :n], mod[:, j, 40 + nt:41 + nt], xT[:, nt, lo:lo + n],
                              ALU.mult, ALU.add)

        def ffn_bufs(s2):
            wgb = [s2.enter_context(sbt("wgb%d" % i, [128, 8, 256], BF16)) for i in range(2)]
            wub = [s2.enter_context(sbt("wub%d" % i, [128, 8, 256], BF16)) for i in range(2)]
            wdb = [s2.enter_context(sbt("wdb%d" % i, [128, 2, D], BF16)) for i in range(2)]
            hid = [s2.enter_context(sbt("hid%d" % i, [128, 2, 512], BF16)) for i in range(2)]
            sg = [s2.enter_context(sbt("sg%d" % i, [128, 512], F32)) for i in range(2)]
            return (wgb, wub, wdb, hid, sg)

        def ffn_dense(j):
            with ExitStack() as s2:
                bufs = ffn_bufs(s2)
                ffn_expert(ffn_wg[j], ffn_wu[j], ffn_wd[j], None, bufs)
                k.S.barrier()

        def ffn_moe(j):
            with ExitStack() as s2:
                bufs = ffn_bufs(s2)
                rw = s2.enter_context(sbt("rw", [128, 8, NE], BF16))
                gT = s2.enter_context(sbt("gT", [NE, NT], F32))
                sel = s2.enter_context(sbt("sel", [NE, NE, 128], F32))
                gbc = s2.enter_context(sbt("gbc", [128, NT], F32))
                lg = s2.enter_context(sbt("lg", [128, NE], F32))
                sm = s2.enter_context(sbt("sm", [128, 8], F32))
                m1 = s2.enter_context(sbt("m1", [128, NE], F32))
                m2 = s2.enter_context(sbt("m2", [128, NE], F32))
                l2 = s2.enter_context(sbt("l2", [128, NE], F32))
                gt = s2.enter_context(sbt("gt", [128, NE], F32))
                k.dma(rw[:], moe_r[j].rearrange("(kt p) e -> p kt e", p=128), "gpsimd")
                for e in range(NE):
                    k.cp(sel[:, e, :], ident[0:NE, e:e + 1].to_broadcast([NE, 128]))
                for c in range(NCH):
                    lo = c * 128
                    ps = PS[c % 2]
                    for kt in range(8):
                        k.mm(ps[:, 0:NE], hT[:, kt, lo:lo + 128], rw[:, kt, :], kt == 0, kt == 7)
                    k.cp(lg[:], ps[:, 0:NE])
                    k.red(sm[:, 0:1], lg[:], ALU.max)
                    k.ts(m1[:], lg[:], sm[:, 0:1], ALU.is_equal)
                    k.stt(l2[:], m1[:], -1e30, lg[:], ALU.mult, ALU.add)
                    k.red(sm[:, 1:2], l2[:], ALU.max)
                    k.ts(m2[:], l2[:], sm[:, 1:2], ALU.is_equal)
                    k.tt(sm[:, 2:3], sm[:, 0:1], sm[:, 1:2], ALU.subtract)
                    k.act(sm[:, 3:4], sm[:, 2:3], AF.Sigmoid)
                    k.ts(sm[:, 4:5], sm[:, 3:4], -1.0, ALU.mult, 1.0, ALU.add)
                    k.ts(gt[:], m1[:], sm[:, 3:4], ALU.mult)
                    k.stt(gt[:], m2[:], sm[:, 4:5], gt[:], ALU.mult, ALU.add)
                    pt = PS[2 + c % 2]
                    k.tr(pt[0:NE, 0:128], gt[:], ident[:])
                    k.cp(gT[:, lo:lo + 128], pt[0:NE, 0:128])
                for e in range(NE):
                    for ti, (lo, n) in enumerate(TT):
                        ps = PS[6 + ti % 2]
                        k.mm(ps[:, :n], sel[:, e, :], gT[:, lo:lo + n])
                        k.cp(gbc[:, lo:lo + n], ps[:, :n], eng="scalar")
                    ffn_expert(moe_wg[j, e], moe_wu[j, e], moe_wd[j, e], gbc, bufs)
                k.S.barrier()

        def load_win(dst, l, c0, ncols, q="gpsimd"):
            wv = w_in[l].rearrange("(kt p) c -> p kt c", p=128)
            k.dma(dst[:, :, :ncols], wv[:, :, c0:c0 + ncols], q)

        def proj_fm(wt, ncols, ti, ps):
            lo, n = TT[ti]
            for kt in range(8):
                k.mm(ps[:ncols, :n], wt[:, kt, :ncols], hT[:, kt, lo:lo + n], kt == 0, kt == 7)

        def proj_tm(wt, ncols, c, ps):
            for kt in range(8):
                k.mm(ps[:, :ncols], hT[:, kt, c * 128:(c + 1) * 128], wt[:, kt, :ncols], kt == 0, kt == 7)

        def sincos(s_out, c_out, ang, ki, tf, shape_slc):
            sl = shape_slc
            k.ts(ki[sl], ang, 1.0 / TWO_PI, ALU.mult)
            k.ts(tf[sl], ki[sl], -TWO_PI, ALU.mult)
            k.tt(tf[sl], tf[sl], ang, ALU.add)
            k.ts(tf[sl], tf[sl], math.pi, ALU.min, -math.pi, ALU.max)
            k.act(s_out, tf[sl], AF.Sin)
            k.ts(ki[sl], ang, 1.0 / TWO_PI, ALU.mult, 0.25, ALU.add)
            k.ts(tf[sl], ki[sl], -TWO_PI, ALU.mult)
            k.stt(tf[sl], ang, math.pi / 2, tf[sl], ALU.add, ALU.add)
            k.ts(tf[sl], tf[sl], math.pi, ALU.min, -math.pi, ALU.max)
            k.act(c_out, tf[sl], AF.Sin)

        s5yg = dscr("s5yg", [512, NT], BF16)

        def s5_mixer(l):
            with ExitStack() as s2:
                def T(name, shape, dt=F32):
                    return s2.enter_context(sbt("s5" + name, list(shape), dt))
                lam = T("lam", [128, 3, 32])
                pa = T("pa", [128, 32]); pdt = T("pdt", [128, 32]); par = T("par", [128, 32])
                pth = T("pth", [128, 32]); pr = T("pr", [128, 32]); psn = T("psn", [128, 32])
                pcs = T("pcs", [128, 32]); pki = T("pki", [128, 32], I32); ptf = T("ptf", [128, 32])
                lbr = T("lbr", [128, 32]); lbi = T("lbi", [128, 32]); den = T("den", [128, 32])
                gre = T("gre", [128, 32]); gim = T("gim", [128, 32]); ngim = T("ngim", [128, 32])
                t32 = T("t32", [128, 32])
                t96i = T("t96i", [128, 96], I32); t96 = T("t96", [128, 96])
                a96 = T("a96", [128, 96]); k96 = T("k96", [128, 96], I32); f96 = T("f96", [128, 96])
                s96 = T("s96", [128, 96]); c96 = T("c96", [128, 96])
                ttmp = [T("ttmp%d" % i, [128, 512]) for i in range(2)]
                ctabL = [T("ctab%d" % i, [128, NT]) for i in range(2)]
                stabL = [T("stab%d" % i, [128, NT]) for i in range(2)]
                SG = []
                for i in range(2):
                    b = {nm: T("%s%d" % (nm, i), [128, 512]) for nm in ("A", "Bb", "T1", "G1", "G2")}
                    b["HR"] = T("HR%d" % i, [128, 512], BF16); b["HI"] = T("HI%d" % i, [128, 512], BF16)
                    SG.append(b)
                car = T("car", [128, 2])
                ubf = T("ubf", [128, NT], BF16)
                wt = T("wt", [128, 8, 128], BF16)
                UB = []
                for i in range(2):
                    b = {nm: T("%s%d" % (nm, i), [128, 128]) for nm in ("Bre", "Bim", "Cre", "Cim", "Btr", "Bti")}
                    for nm in ("BtR", "BtI", "CbR", "CbI"):
                        b[nm] = T("%s%d" % (nm, i), [128, 128], BF16)
                    UB.append(b)
                dsk = T("dsk", [128, 4])
                XT = [T("X%d" % i, [128, 512]) for i in range(4)]
                yy = XT[0]; y2 = XT[1]; ygb = T("ygb", [128, 512], BF16)

                k.dma(lam[:], s5_lam[l], "sync")
                k.dma(dsk[:], s5_d[l], "sync")
                k.S.op("gpsimd", lambda e: e.iota(t96i[:, 0:48], pattern=[[48, 48]], base=0, channel_multiplier=0), [], [t96i])
                k.S.op("gpsimd", lambda e: e.iota(t96i[:, 48:96], pattern=[[1, 48]], base=0, channel_multiplier=0), [t96i], [t96i])
                k.cp(t96[:], t96i[:])
                k.ts(pa[:], lam[:, 0, :], -1e-4, ALU.min)
                k.act(pdt[:], lam[:, 2, :], AF.Exp)
                k.tt(par[:], pa[:], pdt[:], ALU.mult)
                k.tt(pth[:], lam[:, 1, :], pdt[:], ALU.mult)
                k.act(pr[:], par[:], AF.Exp)
                sincos(psn[:], pcs[:], pth[:], pki, ptf, (slice(None), slice(None)))
                k.tt(lbr[:], pr[:], pcs[:], ALU.mult)
                k.tt(lbi[:], pr[:], psn[:], ALU.mult)
                k.ts(lbr[:], lbr[:], -1.0, ALU.add)
                k.tt(den[:], pa[:], pa[:], ALU.mult)
                k.tt(t32[:], lam[:, 1, :], lam[:, 1, :], ALU.mult)
                k.tt(den[:], den[:], t32[:], ALU.add)
                k.recip(den[:], den[:])
                k.tt(gre[:], lbr[:], pa[:], ALU.mult)
                k.tt(t32[:], lbi[:], lam[:, 1, :], ALU.mult)
                k.tt(gre[:], gre[:], t32[:], ALU.add)
                k.tt(gre[:], gre[:], den[:], ALU.mult)
                k.tt(gim[:], lbi[:], pa[:], ALU.mult)
                k.tt(t32[:], lbr[:], lam[:, 1, :], ALU.mult)
                k.tt(gim[:], gim[:], t32[:], ALU.subtract)
                k.tt(gim[:], gim[:], den[:], ALU.mult)
                k.ts(ngim[:], gim[:], -1.0, ALU.mult)

                PSy = PS[0:5]
                UORD = [(0, 0), (1, 0), (2, 0), (3, 0), (0, 1), (1, 1), (2, 1), (3, 1)]

                def setup(ub, ui):
                    stl, d = UORD[ui]
                    u = (ub * 4 + stl) * 2 + d
                    B = UB[ui % 2]
                    ctab = ctabL[ui % 2]; stab = stabL[ui % 2]
                    k.dma(B["Bre"][:], s5_B[l, u, 0], "sync")
                    k.dma(B["Bim"][:], s5_B[l, u, 1], "sync")
                    k.dma(B["Cre"][:], s5_C[l, u, 0], "sync")
                    k.dma(B["Cim"][:], s5_C[l, u, 1], "sync")
                    k.ts(B["Btr"][:], B["Bre"][:], gre[:, u:u + 1], ALU.mult)
                    k.ts(B["Bti"][:], B["Bim"][:], gre[:, u:u + 1], ALU.mult)
                    k.stt(B["Btr"][:], B["Bim"][:], ngim[:, u:u + 1], B["Btr"][:], ALU.mult, ALU.add)
                    k.stt(B["Bti"][:], B["Bre"][:], gim[:, u:u + 1], B["Bti"][:], ALU.mult, ALU.add)
                    k.tr(PS[7][:, 0:128], B["Btr"][:], ident[:])
                    k.tr(PS[7][:, 128:256], B["Bti"][:], ident[:])
                    k.cp(B["BtR"][:], PS[7][:, 0:128], eng="scalar")
                    k.cp(B["BtI"][:], PS[7][:, 128:256], eng="scalar")
                    k.cp(B["CbR"][:], B["Cre"][:], eng="scalar")
                    k.ts(B["CbI"][:], B["Cim"][:], -1.0, ALU.mult)
                    k.ts(a96[:], t96[:], pth[:, u:u + 1], ALU.mult)
                    sincos(s96[:], c96[:], a96[:], k96, f96, (slice(None), slice(None)))
                    for pc in range(5):
                        a0 = pc * 10; na = min(10, 48 - a0)
                        eng = "gpsimd" if pc == 1 else "vector"
                        tm = ttmp[pc % 2]
                        def bc_a(src):
                            return src[:, a0:a0 + na].unsqueeze(2).to_broadcast([128, na, 48])
                        def bc_b(src):
                            return src[:, 48:96].unsqueeze(1).to_broadcast([128, na, 48])
                        cv = ctab[:, a0 * 48:(a0 + na) * 48].rearrange("p (a b) -> p a b", b=48)
                        sv = stab[:, a0 * 48:(a0 + na) * 48].rearrange("p (a b) -> p a b", b=48)
                        tv = tm[:, 0:na * 48].rearrange("p (a b) -> p a b", b=48)
                        k.tt(cv, bc_a(c96), bc_b(c96), ALU.mult, eng=eng)
                        k.tt(tv, bc_a(s96), bc_b(s96), ALU.mult, eng=eng)
                        k.tt(cv, cv, tv, ALU.subtract, eng=eng)
                        k.tt(sv, bc_a(s96), bc_b(c96), ALU.mult, eng=eng)
                        k.tt(tv, bc_a(c96), bc_b(s96), ALU.mult, eng=eng)
                        k.tt(sv, sv, tv, ALU.add, eng=eng)

                segctr = [0]

                def seg_info(ui, si):
                    stl, d = UORD[ui]
                    order = [0, 1, 2, 3, 4] if d == 0 else [0, 4, 3, 2, 1]
                    ti = order[si]
                    lo, n = TT[ti]
                    if d == 0:
                        tsl = slice(lo, lo + n); fw = slice(0, n); last = n - 1
                    else:
                        tsl = slice(255, None, -1) if ti == 0 else slice(2559 - lo, 2559 - lo - n, -1)
                        fw = slice(n - 1, None, -1); last = 0
                    return d, ti, lo, n, tsl, fw, last

                def stageA(ub, ui, si, buf):
                    d, ti, lo, n, tsl, fw, last = seg_info(ui, si)
                    B = UB[ui % 2]; ct = ctabL[ui % 2][:, tsl]; sn = stabL[ui % 2][:, tsl]
                    A, Bb, T1 = buf["A"], buf["Bb"], buf["T1"]
                    k.mm(PS[5][:, :n], B["BtR"][:], ubf[:, lo:lo + n])
                    k.mm(PS[6][:, :n], B["BtI"][:], ubf[:, lo:lo + n])
                    k.tt(A[:, :n], PS[5][:, :n], ct, ALU.mult)
                    k.tt(T1[:, :n], PS[6][:, :n], sn, ALU.mult)
                    k.tt(Bb[:, :n], PS[6][:, :n], ct, ALU.mult)
                    k.tt(A[:, :n], A[:, :n], T1[:, :n], ALU.add, eng="gpsimd")
                    k.tt(T1[:, :n], PS[5][:, :n], sn, ALU.mult)
                    k.tt(Bb[:, :n], Bb[:, :n], T1[:, :n], ALU.subtract, eng="gpsimd")

                def stageB1(ub, ui, si, buf):
                    d, ti, lo, n, tsl, fw, last = seg_info(ui, si)
                    stl = UORD[ui][0]
                    u = (ub * 4 + stl) * 2 + d
                    ct = ctabL[ui % 2][:, tsl]; sn = stabL[ui % 2][:, tsl]
                    A, Bb, G1, G2 = buf["A"], buf["Bb"], buf["G1"], buf["G2"]
                    rb = pr[:, u:u + 1].to_broadcast([128, n])
                    i1 = 0.0 if si == 0 else car[:, 0:1]
                    i2 = 0.0 if si == 0 else car[:, 1:2]
                    k.scan(G1[:, fw], rb, A[:, fw], i1)
                    k.scan(G2[:, fw], rb, Bb[:, fw], i2)
                    if si < 4:
                        k.cp(car[:, 0:1], G1[:, last:last + 1])
                        k.cp(car[:, 1:2], G2[:, last:last + 1])
                    k.tt(XT[0][:, :n], G1[:, :n], ct, ALU.mult, eng="gpsimd")
                    k.tt(XT[1][:, :n], G2[:, :n], sn, ALU.mult, eng="gpsimd")
                    k.tt(XT[2][:, :n], G1[:, :n], sn, ALU.mult, eng="gpsimd")
                    k.tt(XT[3][:, :n], G2[:, :n], ct, ALU.mult, eng="gpsimd")

                def stageB2(ub, ui, si, buf):
                    d, ti, lo, n, tsl, fw, last = seg_info(ui, si)
                    B = UB[ui % 2]
                    HR, HI = buf["HR"], buf["HI"]
                    k.tt(HR[:, :n], XT[0][:, :n], XT[1][:, :n], ALU.subtract)
                    k.tt(HI[:, :n], XT[2][:, :n], XT[3][:, :n], ALU.add)
                    k.mm(PSy[ti][:, :n], B["CbR"][:], HR[:, :n], ui == 0, False)
                    k.mm(PSy[ti][:, :n], B["CbI"][:], HI[:, :n], False, ui == 7)

                for ub in range(4):
                    load_win(wt, l, ub * 128, 128)
                    for ti, (lo, n) in enumerate(TT):
                        proj_fm(wt, 128, ti, PS[5 + ti % 2])
                        k.cp(ubf[:, lo:lo + n], PS[5 + ti % 2][:, :n], eng="scalar")
                    setup(ub, 0)
                    bufs = {}
                    bufs[(0, 0)] = SG[segctr[0] % 2]; segctr[0] += 1
                    stageA(ub, 0, 0, bufs[(0, 0)])
                    pending = None
                    for ui in range(8):
                        for si in range(5):
                            stageB1(ub, ui, si, bufs[(ui, si)]) if pending is None else None
                            if pending is not None:
                                pass
                            if si < 4:
                                nb = SG[segctr[0] % 2]; segctr[0] += 1
                                bufs[(ui, si + 1)] = nb
                                stageA(ub, ui, si + 1, nb)
                            elif ui < 7:
                                setup(ub, ui + 1)
                                nb = SG[segctr[0] % 2]; segctr[0] += 1
                                bufs[(ui + 1, 0)] = nb
                                stageA(ub, ui + 1, 0, nb)
                            stageB2(ub, ui, si, bufs[(ui, si)])
                    for ti, (lo, n) in enumerate(TT):
                        k.stt(yy[:, :n], ubf[:, lo:lo + n], dsk[:, ub:ub + 1], PSy[ti][:, :n], ALU.mult, ALU.add)
                        k.tt(y2[:, :n], yy[:, :n], yy[:, :n], ALU.mult, eng="gpsimd")
                        k.ts(y2[:, :n], y2[:, :n], 0.044715, ALU.mult, 1.0, ALU.add, eng="gpsimd")
                        k.tt(y2[:, :n], y2[:, :n], yy[:, :n], ALU.mult, eng="gpsimd")
                        k.act(y2[:, :n], y2[:, :n], AF.Sigmoid, scale=2.0 * math.sqrt(2.0 / math.pi))
                        k.tt(ygb[:, :n], yy[:, :n], y2[:, :n], ALU.mult)
                        k.dma(s5yg[ub * 128:(ub + 1) * 128, lo:lo + n], ygb[:, :n], "sync")
                k.S.barrier()
            with ExitStack() as s2:
                def T(name, shape, dt=F32):
                    return s2.enter_context(sbt("s5g" + name, list(shape), dt))
                wglu = T("wglu", [128, 4, 512], BF16)
                ygl = T("ygl", [128, 4, 512], BF16)
                yo = T("yo", [128, 512], BF16)
                yy = T("yy", [128, 512])
                k.dma(wglu[:], s5_glu[l].rearrange("(kt p) n -> p kt n", p=128), "gpsimd")
                ygv = s5yg.rearrange("(kt p) t -> p kt t", p=128)
                for ti, (lo, n) in enumerate(TT):
                    k.dma(ygl[:, :, :n], ygv[:, :, lo:lo + n], "sync")
                    for nt in range(4):
                        ps = PS[nt % 4]
                        for kt in range(4):
                            k.mm(ps[:, :n], wglu[:, kt, nt * 128:(nt + 1) * 128], ygl[:, kt, :n], kt == 0, kt == 3)
                        k.act(yy[:, :n], ps[:, :n], AF.Sigmoid)
                        k.tt(yo[:, :n], yy[:, :n], ygl[:, nt, :n], ALU.mult)
                        k.dma(ymix[nt * 128:(nt + 1) * 128, lo:lo + n], yo[:, :n], "sync")
                k.S.barrier()

        maskF = sb("maskF", [128, 128]); maskB = sb("maskB", [128, 128])
        nstrF = sb("nstrF", [128, 128]); nstrB = sb("nstrB", [128, 128])
        selF = sb("selF", [128, 128]); selB = sb("selB", [128, 128])
        k.memset(maskF[:], 1.0); k.memset(maskB[:], 1.0); k.memset(selF[:], 0.0); k.memset(selB[:], 0.0)
        k.S.op("gpsimd", lambda e: e.affine_select(out=maskF[:], in_=maskF[:], pattern=[[1, 128]], compare_op=ALU.is_ge,
                                                   fill=0.0, base=0, channel_multiplier=-1), [maskF], [maskF])
        k.S.op("gpsimd", lambda e: e.affine_select(out=maskB[:], in_=maskB[:], pattern=[[-1, 128]], compare_op=ALU.is_ge,
                                                   fill=0.0, base=0, channel_multiplier=1), [maskB], [maskB])
        k.S.op("gpsimd", lambda e: e.affine_select(out=selF[:], in_=selF[:], pattern=[[0, 128]], compare_op=ALU.not_equal,
                                                   fill=1.0, base=-127, channel_multiplier=1), [selF], [selF])
        k.S.op("gpsimd", lambda e: e.affine_select(out=selB[:], in_=selB[:], pattern=[[0, 128]], compare_op=ALU.not_equal,
                                                   fill=1.0, base=0, channel_multiplier=1), [selB], [selB])
        k.tt(nstrF[:], ident[:], maskF[:], ALU.subtract)
        k.tt(nstrB[:], ident[:], maskB[:], ALU.subtract)
        MASK = [maskF, maskB]; NSTR = [nstrF, nstrB]; SEL = [selF, selB]; ENDI = [127, 0]
        CH_ORDER = [list(range(NCH)), [1, 0] + list(range(NCH - 1, 1, -1))]

        def conv_block(raw, acc, cw, cb, ntap_bias):
            if ntap_bias:
                k.ts(acc[:], raw[:], cw[:, cb, 2:3], ALU.mult, cw[:, cb, 5:6], ALU.add)
            else:
                k.ts(acc[:], raw[:], cw[:, cb, 2:3], ALU.mult)
            for kk in (0, 1, 3, 4):
                dd = kk - 2
                for (s0, L) in ((0, NCTX), (NCTX, NT - NCTX)):
                    o0 = s0 + max(0, -dd); o1 = s0 + L - max(0, dd)
                    i0 = s0 + max(0, dd); i1 = s0 + L - max(0, -dd)
                    k.stt(acc[:, o0:o1], raw[:, i0:i1], cw[:, cb, kk:kk + 1], acc[:, o0:o1], ALU.mult, ALU.add,
                          eng=("vector" if kk < 2 else "gpsimd"))


        PADL = 2 + NCTX + 2 + 2 + (NT - NCTX) + 2

        def pad_off(lo):
            return 2 + lo if lo < NCTX else 2 + NCTX + 2 + 2 + (lo - NCTX)

        def conv_pe(rawp, dg, cw, cb, acc, bias_col):
            for kk in range(5):
                k.ts(dg[:, kk, :], identb[:], cw[:, cb, kk:kk + 1], ALU.mult)
            for ti, (lo, n) in enumerate(TT):
                ps = PS[4 + ti % 4]
                o = pad_off(lo)
                for kk in range(5):
                    k.mm(ps[:, :n], dg[:, kk, :], rawp[:, o + kk - 2:o + kk - 2 + n], kk == 0, kk == 4)
                if bias_col is not None:
                    k.act(acc[:, lo:lo + n], ps[:, :n], AF.Silu, bias=bias_col)
                else:
                    k.act(acc[:, lo:lo + n], ps[:, :n], AF.Silu)

        m2tm = dscr("m2tm", [NT, 1280])
        m2fm = dscr("m2fm", [512, NT])
        M2X = 3600

        def m2_mixer(l):
            with ExitStack() as s2:
                def T(name, shape, dt=F32):
                    return s2.enter_context(sbt("ma" + name, list(shape), dt))
                cw = T("cw", [128, 12, 6])
                wt = [T("wt%d" % i, [128, 8, 128], BF16) for i in range(2)]
                rawp = T("rawp", [128, PADL], BF16); acc = T("acc", [128, NT])
                dg = T("dg", [128, 5, 128], BF16)
                tmall = T("tmall", [128, NCH, 128])
                k.dma(cw[:], m2_cw[l], "sync")
                k.memset(rawp[:], 0.0)
                for cb in range(12):
                    w = wt[cb % 2]
                    load_win(w, l, M2X + cb * 128, 128)
                    for ti, (lo, n) in enumerate(TT):
                        ps = PS[ti % 4]
                        proj_fm(w, 128, ti, ps)
                        o = pad_off(lo)
                        k.cp(rawp[:, o:o + n], ps[:, :n], eng=("scalar" if ti % 2 else "vector"))
                    conv_pe(rawp, dg, cw, cb, acc, cw[:, cb, 5:6])
                    if cb >= 8:
                        k.dma(m2fm[(cb - 8) * 128:(cb - 7) * 128, :], acc[:], "sync")
                    if cb < 10:
                        for c in range(NCH):
                            ps = PS[c % 4]
                            k.tr(ps[:, 0:128], acc[:, c * 128:(c + 1) * 128], ident[:])
                            k.cp(tmall[:, c, :], ps[:, 0:128], eng=("vector" if c % 2 else "scalar"))
                        k.dma(m2tm[:, cb * 128:(cb + 1) * 128].rearrange("(c p) f -> p c f", p=128), tmall[:], "sync")
                k.S.barrier()
            with ExitStack() as s2:
                def T(name, shape, dt=F32):
                    return s2.enter_context(sbt("mb" + name, list(shape), dt))
                wz = T("wz", [128, 8, 1024], BF16)
                wdt = T("wdt", [128, 8, 32], BF16)
                ab = T("ab", [128, 80]); ng = T("ng", [128, 1024])
                nega = T("nega", [128, 32])
                Sst = T("S", [128, 16, 64])
                xtm = T("xtm", [128, 16, 64]); btm = T("btm", [128, 256])
                BT = T("BT", [128, 2, 128]); CT = T("CT", [128, 2, 128])
                dtv = T("dtv", [128, 16]); la = T("la", [128, 16]); cum = T("cum", [128, 16]); cend = T("cend", [128, 16])
                ecum = T("ecum", [128, 16]); edd = T("edd", [128, 16]); ecend = T("ecend", [128, 16])
                DEM = T("DEM", [128, 16, 128])
                Gm = T("Gm", [128, 2, 128])
                xdt = T("xdt", [128, 16, 64]); xdec = T("xdec", [128, 16, 64])
                yacc = T("yacc", [128, 16, 64]); yf = T("yf", [128, 16, 64])
                zs = T("zs", [128, 1024]); sq = T("sq", [128, 512]); ss = T("ss", [128, 2])
                ytb = T("ytb", [128, 8, 128], BF16)
                k.dma(ab[:], m2_ab[l], "sync")
                k.dma(ng[:], m2_ng[l], "sync")
                load_win(wz, l, 2576, 1024)
                load_win(wdt, l, 5136, 32)
                k.act(nega[:], ab[:, 0:32], AF.Exp)
                k.ts(nega[:], nega[:], -1.0, ALU.mult)
                for d in range(2):
                    mk = MASK[d]
                    k.memset(Sst[:], 0.0)
                    for c in CH_ORDER[d]:
                        r0 = c * 128
                        k.dma(xtm[:].rearrange("p h e -> p (h e)"), m2tm[r0:r0 + 128, 0:1024], "sync")
                        k.dma(btm[:], m2tm[r0:r0 + 128, 1024:1280], "sync")
                        k.dma(BT[:], m2fm[0:256, r0:r0 + 128].rearrange("(g p) t -> p g t", p=128), "sync")
                        k.dma(CT[:], m2fm[256:512, r0:r0 + 128].rearrange("(g p) t -> p g t", p=128), "sync")
                        proj_tm(wdt, 32, c, PS[6])
                        k.tt(dtv[:], PS[6][:, d * 16:(d + 1) * 16], ab[:, 32 + d * 16:48 + d * 16], ALU.add)
                        k.act(dtv[:], dtv[:], AF.Exp)
                        k.act(dtv[:], dtv[:], AF.Ln, bias=ones[:, 0:1])
                        k.tt(la[:], dtv[:], nega[:, d * 16:(d + 1) * 16], ALU.mult)
                        k.mm(PS[6][:, 64:80], mk[:], la[:])
                        k.cp(cum[:], PS[6][:, 64:80])
                        k.mm(PS[6][:, 96:112], SEL[d][:], cum[:])
                        k.cp(cend[:], PS[6][:, 96:112])
                        k.act(ecum[:], cum[:], AF.Exp)
                        k.act(ecend[:], cend[:], AF.Exp)
                        k.tt(edd[:], cend[:], cum[:], ALU.subtract)
                        k.act(edd[:], edd[:], AF.Exp)
                        k.tt(xdt[:], xtm[:], dtv[:].unsqueeze(2).to_broadcast([128, 16, 64]), ALU.mult)
                        k.tt(DEM[:], ident[:].unsqueeze(1).to_broadcast([128, 16, 128]),
                             cum[:].unsqueeze(2).to_broadcast([128, 16, 128]), ALU.mult, eng="gpsimd")
                        for q4 in range(4):
                            k.mm(PS[q4][:], ones[:], DEM[:, q4 * 4:(q4 + 1) * 4, :].rearrange("p h t -> p (h t)"))
                        for q4 in range(4):
                            k.tt(DEM[:, q4 * 4:(q4 + 1) * 4, :], PS[q4][:].rearrange("p (h t) -> p h t", t=128),
                                 cum[:, q4 * 4:(q4 + 1) * 4].unsqueeze(2).to_broadcast([128, 4, 128]), ALU.subtract)
                        k.tt(DEM[:], DEM[:], mk[:].unsqueeze(1).to_broadcast([128, 16, 128]), ALU.mult, eng="gpsimd")
                        k.act(DEM[:], DEM[:], AF.Exp)
                        for g in range(2):
                            k.mm(PS[6][:, 128 + g * 128:256 + g * 128], BT[:, g, :], CT[:, g, :])
                        k.tt(Gm[:], PS[6][:, 128:384].rearrange("p (g t) -> p g t", t=128),
                             mk[:].unsqueeze(1).to_broadcast([128, 2, 128]), ALU.mult)
                        for g in range(2):
                            k.tt(DEM[:, g * 8:(g + 1) * 8, :], DEM[:, g * 8:(g + 1) * 8, :],
                                 Gm[:, g, :].unsqueeze(1).to_broadcast([128, 8, 128]), ALU.mult,
                                 eng=("vector" if g == 0 else "gpsimd"))
                        for h in range(16):
                            ps = PS[4 + h // 8]
                            k.mm(ps[:, (h % 8) * 64:(h % 8 + 1) * 64], DEM[:, h, :], xdt[:, h, :])
                        for g in range(2):
                            k.mm(PS[g][:], CT[:, g, :], Sst[:, g * 8:(g + 1) * 8, :].rearrange("p h e -> p (h e)"))
                        for g in range(2):
                            hs = slice(g * 8, (g + 1) * 8)
                            k.tt(yacc[:, hs, :], PS[g][:].rearrange("p (h e) -> p h e", e=64),
                                 ecum[:, hs].unsqueeze(2).to_broadcast([128, 8, 64]), ALU.mult)
                            k.tt(yacc[:, hs, :], yacc[:, hs, :], PS[4 + g][:].rearrange("p (h e) -> p h e", e=64), ALU.add)
                        k.tt(xdec[:], xdt[:], edd[:].unsqueeze(2).to_broadcast([128, 16, 64]), ALU.mult, eng="gpsimd")
                        for g in range(2):
                            k.mm(PS[2 + g][:], btm[:, g * 128:(g + 1) * 128],
                                 xdec[:, g * 8:(g + 1) * 8, :].rearrange("p h e -> p (h e)"))
                        k.tt(Sst[:], Sst[:], ecend[:].unsqueeze(2).to_broadcast([128, 16, 64]), ALU.mult, eng="gpsimd")
                        for g in range(2):
                            hs = slice(g * 8, (g + 1) * 8)
                            k.tt(Sst[:, hs, :], Sst[:, hs, :], PS[2 + g][:].rearrange("p (h e) -> p h e", e=64), ALU.add)
                        if d == 0:
                            k.dma(yfwd_m[r0:r0 + 128, :], yacc[:].rearrange("p h e -> p (h e)"), "sync")
                        else:
                            k.dma(yf[:].rearrange("p h e -> p (h e)"), yfwd_m[r0:r0 + 128, :], "sync")
                            k.tt(yacc[:], yacc[:], yf[:], ALU.add, eng="gpsimd")
                            k.tt(yf[:], xtm[:], ab[:, 64:80].unsqueeze(2).to_broadcast([128, 16, 64]), ALU.mult, eng="gpsimd")
                            k.tt(yacc[:], yacc[:], yf[:], ALU.add, eng="gpsimd")
                            for hf in range(2):
                                proj_tm(wz[:, :, hf * 512:(hf + 1) * 512], 512, c, PS[7])
                                k.act(zs[:, hf * 512:(hf + 1) * 512], PS[7][:], AF.Silu)
                            yv = yacc[:].rearrange("p h e -> p (h e)")
                            k.tt(yv, yv, zs[:], ALU.mult)
                            k.memset(ss[:], 0.0)
                            for g in range(2):
                                k.act(sq[:], yv[:, g * 512:(g + 1) * 512], AF.Square, accum=ss[:, g:g + 1])
                            k.act(ss[:], ss[:], AF.Sqrt, bias=epsc[:, 0:1], scale=1.0 / 512)
                            k.recip(ss[:], ss[:])
                            for g in range(2):
                                k.stt(yv[:, g * 512:(g + 1) * 512], yv[:, g * 512:(g + 1) * 512], ss[:, g:g + 1],
                                      ng[:, g * 512:(g + 1) * 512], ALU.mult, ALU.mult)
                            for cb in range(8):
                                pst = PS[6 + cb % 2]
                                k.tr(pst[:, 0:128], yv[:, cb * 128:(cb + 1) * 128], ident[:])
                                k.cp(ytb[:, cb, :], pst[:, 0:128], eng=("scalar" if cb % 2 else "vector"))
                            k.dma(ymix[1024:2048, r0:r0 + 128].rearrange("(cb p) t -> p cb t", p=128), ytb[:], "sync")
                k.S.barrier()

        gtm = dscr("gtm", [NT, 1024])
        gfm = dscr("gfm", [1024, NT])
        GQ = 512

        GC = os.environ.get("GDNCUT", "")

        def gdn_mixer(l):
            with ExitStack() as s2:
                def T(name, shape, dt=F32):
                    return s2.enter_context(sbt("ga" + name, list(shape), dt))
                cw = T("cw", [128, 12, 5])
                wt = [T("wt%d" % i, [128, 8, 128], BF16) for i in range(2)]
                rawp = T("rawp", [128, PADL], BF16); acc = T("acc", [128, NT])
                dg = T("dg", [128, 5, 128], BF16)
                sq = T("sq", [128, 512]); rs = T("rs", [128, 512])
                tmall = T("tmall", [128, NCH, 128])
                k.dma(cw[:], gdn_cw[l], "sync")
                k.memset(rawp[:], 0.0)
                for cb in range(12):
                    w = wt[cb % 2]
                    load_win(w, l, GQ + cb * 128, 128)
                    for ti, (lo, n) in enumerate(TT):
                        ps = PS[ti % 4]
                        proj_fm(w, 128, ti, ps)
                        o = pad_off(lo)
                        k.cp(rawp[:, o:o + n], ps[:, :n], eng=("scalar" if ti % 2 else "vector"))
                    conv_pe(rawp, dg, cw, cb, acc, None)
                    if cb < 8:
                        for ti, (lo, n) in enumerate(TT):
                            ps = PS[4 + ti % 2]
                            k.tt(sq[:, :n], acc[:, lo:lo + n], acc[:, lo:lo + n], ALU.mult, eng="gpsimd")
                            k.mm(ps[:, :n], ones[:], sq[:, :n])
                            k.act(rs[:, :n], ps[:, :n], AF.Sqrt, bias=epsc[:, 0:1], scale=1.0)
                            k.recip(rs[:, :n], rs[:, :n])
                            if cb < 4:
                                k.stt(acc[:, lo:lo + n], acc[:, lo:lo + n], 128.0 ** -0.5, rs[:, :n], ALU.mult, ALU.mult)
                            else:
                                k.tt(acc[:, lo:lo + n], acc[:, lo:lo + n], rs[:, :n], ALU.mult)
                        k.dma(gfm[cb * 128:(cb + 1) * 128, :], acc[:], "sync")
                    if cb >= 4:
                        for c in range(NCH):
                            ps = PS[6 + c % 2]
                            k.tr(ps[:, 0:128], acc[:, c * 128:(c + 1) * 128], ident[:])
                            k.cp(tmall[:, c, :], ps[:, 0:128], eng=("vector" if c % 2 else "scalar"))
                        k.dma(gtm[:, (cb - 4) * 128:(cb - 3) * 128].rearrange("(c p) f -> p c f", p=128), tmall[:], "sync")
                k.S.barrier()
            if GC == "s1":
                return
            with ExitStack() as s2:
                def T(name, shape, dt=F32):
                    return s2.enter_context(sbt("gb" + name, list(shape), dt))
                wz = T("wz", [128, 8, 512], BF16)
                wab = T("wab", [128, 8, 16], BF16)
                abp = T("abp", [128, 16]); ng = T("ng", [128, 128]); nega = T("nega", [128, 8])
                Sst = T("S", [128, 4, 128])
                ggL = [T("gg%d" % i, [128, 4]) for i in range(2)]
                betaL = [T("beta%d" % i, [128, 4]) for i in range(2)]
                gcL = [T("gc%d" % i, [128, 4]) for i in range(2)]
                egcL = [T("egc%d" % i, [128, 4]) for i in range(2)]
                zsL = [T("zs%d" % i, [128, 512]) for i in range(2)]
                HB = []
                for h in range(4):
                    b = {}
                    for nm in ("knT", "qnT", "ktm", "vtm", "EdT", "AT", "P", "PT", "wT", "vn", "qdT", "eRg", "kdec",
                               "oo", "of", "sq"):
                        b[nm] = T("%s%d" % (nm, h), [128, 128])
                    b["D2"] = T("D2%d" % h, [128, 256]); b["R"] = T("R%d" % h, [128, 256])
                    b["sc1"] = T("sc1%d" % h, [128, 8]); b["otb"] = T("otb%d" % h, [128, 128], BF16)
                    HB.append(b)
                k.dma(abp[:], gdn_ab[l], "sync")
                k.dma(ng[:], gdn_ng[l], "sync")
                load_win(wz, l, 2048, 512)
                load_win(wab, l, 2560, 16)
                k.act(nega[:], abp[:, 0:8], AF.Exp)
                k.ts(nega[:], nega[:], -1.0, ALU.mult)

                def prep(d, c, par):
                    gg, beta, gc, egc, zs = ggL[par], betaL[par], gcL[par], egcL[par], zsL[par]
                    proj_tm(wab, 16, c, PS[7])
                    k.tt(gg[:], PS[7][:, d * 4:(d + 1) * 4], abp[:, 8 + d * 4:12 + d * 4], ALU.add)
                    k.act(gg[:], gg[:], AF.Exp)
                    k.act(gg[:], gg[:], AF.Ln, bias=ones[:, 0:1])
                    k.tt(gg[:], gg[:], nega[:, d * 4:(d + 1) * 4], ALU.mult)
                    k.act(beta[:], PS[7][:, 8 + d * 4:12 + d * 4], AF.Sigmoid)
                    k.mm(PS[7][:, 32:36], MASK[d][:], gg[:])
                    k.cp(gc[:], PS[7][:, 32:36])
                    k.act(egc[:], gc[:], AF.Exp)
                    if d == 1:
                        proj_tm(wz, 512, c, PS[6])
                        k.act(zs[:], PS[6][:], AF.Silu)

                def unit(d, c, par, h):
                    beta, gc, egc, zs = betaL[par], gcL[par], egcL[par], zsL[par]
                    mk = MASK[d]; e_i = ENDI[d]
                    r0 = c * 128
                    B = HB[h]
                    knT, qnT, ktm, vtm, D2, EdT, AT = B["knT"], B["qnT"], B["ktm"], B["vtm"], B["D2"], B["EdT"], B["AT"]
                    P, PT, R, wT, vn, qdT, eRg, kdec = B["P"], B["PT"], B["R"], B["wT"], B["vn"], B["qdT"], B["eRg"], B["kdec"]
                    sc1, oo, of, sq, otb = B["sc1"], B["oo"], B["of"], B["sq"], B["otb"]
                    pA = PS[h]; pB = PS[4 + h]
                    k.dma(qnT[:], gfm[h * 128:(h + 1) * 128, r0:r0 + 128], "sync")
                    k.dma(knT[:], gfm[512 + h * 128:640 + h * 128, r0:r0 + 128], "sync")
                    k.dma(ktm[:], gtm[r0:r0 + 128, h * 128:(h + 1) * 128], "scalar")
                    k.dma(vtm[:], gtm[r0:r0 + 128, 512 + h * 128:640 + h * 128], "scalar")
                    yield
                    k.ts(D2[:, 0:128], ident[:], gc[:, h:h + 1], ALU.mult)
                    k.ts(D2[:, 128:256], ident[:], beta[:, h:h + 1], ALU.mult)
                    yield
                    k.mm(pA[:, 0:256], ones[:], D2[:])
                    Rg = pA[:, 0:128]; Rb = pA[:, 128:256]
                    k.mm(pB[:, 0:128], knT[:], knT[:])
                    k.mm(pB[:, 128:256], knT[:], qnT[:])
                    yield
                    k.ts(EdT[:], Rg, gc[:, h:h + 1], ALU.subtract)
                    yield
                    k.tt(EdT[:], EdT[:], mk[:], ALU.mult)
                    yield
                    k.act(EdT[:], EdT[:], AF.Exp)
                    k.cp(sc1[:, 4:5], pA[:, e_i:e_i + 1])
                    yield
                    k.tt(EdT[:], EdT[:], mk[:], ALU.mult)
                    yield
                    k.tt(AT[:], pB[:, 128:256], EdT[:], ALU.mult)
                    k.tt(PT[:], pB[:, 0:128], EdT[:], ALU.mult)
                    yield
                    k.tt(PT[:], PT[:], Rb, ALU.mult)
                    k.act(eRg[:], Rg, AF.Exp)
                    yield
                    k.tt(PT[:], PT[:], NSTR[d][:], ALU.mult)
                    k.ts(R[:, 0:128], vtm[:], beta[:, h:h + 1], ALU.mult)
                    k.ts(R[:, 128:256], ktm[:], beta[:, h:h + 1], ALU.mult, egc[:, h:h + 1], ALU.mult)
                    yield
                    k.tr(pB[:, 256:384], PT[:], ident[:])
                    yield
                    k.cp(P[:], pB[:, 256:384])
                    k.tt(qdT[:], qnT[:], eRg[:], ALU.mult)
                    yield
                    for lev in range(7):
                        k.mm(pA[:, 256:512], PT[:], R[:])
                        if lev < 6:
                            k.mm(pB[:, 0:128], PT[:], P[:])
                            k.mm(pB[:, 128:256], P[:], PT[:])
                        yield
                        k.tt(R[:], R[:], pA[:, 256:512], ALU.add)
                        if lev < 6:
                            k.cp(P[:], pB[:, 0:128])
                            k.cp(PT[:], pB[:, 128:256])
                        yield
                    k.tr(pB[:, 256:384], R[:, 128:256], ident[:])
                    k.ts(sc1[:, 0:1], gc[:, h:h + 1], -1.0, ALU.mult, sc1[:, 4:5], ALU.add)
                    yield
                    k.cp(wT[:], pB[:, 256:384])
                    k.act(sc1[:, 1:2], sc1[:, 0:1], AF.Exp)
                    k.act(sc1[:, 2:3], sc1[:, 4:5], AF.Exp)
                    yield
                    k.mm(pB[:, 384:512], wT[:], Sst[:, h, :])
                    k.ts(kdec[:], ktm[:], sc1[:, 1:2], ALU.mult)
                    yield
                    k.tt(vn[:], R[:, 0:128], pB[:, 384:512], ALU.subtract)
                    yield
                    k.mm(pB[:, 384:512], qdT[:], Sst[:, h, :], True, False)
                    k.mm(pB[:, 384:512], AT[:], vn[:], False, True)
                    k.mm(pB[:, 256:384], kdec[:], vn[:])
                    yield
                    if d == 0:
                        k.cp(oo[:], pB[:, 384:512])
                    else:
                        k.dma(of[:], yfwd_g[h, r0:r0 + 128, :], "sync")
                        k.tt(oo[:], of[:], pB[:, 384:512], ALU.add)
                    k.stt(Sst[:, h, :], Sst[:, h, :], sc1[:, 2:3], pB[:, 256:384], ALU.mult, ALU.add)
                    yield
                    if d == 0:
                        k.dma(yfwd_g[h, r0:r0 + 128, :], oo[:], "sync")
                    else:
                        k.memset(sc1[:, 3:4], 0.0)
                        yield
                        k.act(sq[:], oo[:], AF.Square, accum=sc1[:, 3:4])
                        yield
                        k.act(sc1[:, 3:4], sc1[:, 3:4], AF.Sqrt, bias=epsc[:, 0:1], scale=1.0 / 128)
                        yield
                        k.recip(sc1[:, 3:4], sc1[:, 3:4])
                        yield
                        k.stt(oo[:], oo[:], sc1[:, 3:4], ng[:], ALU.mult, ALU.mult)
                        yield
                        k.tt(oo[:], oo[:], zs[:, h * 128:(h + 1) * 128], ALU.mult)
                        yield
                        k.tr(pB[:, 256:384], oo[:], ident[:])
                        yield
                        k.cp(otb[:], pB[:, 256:384], eng="scalar")
                        yield
                        k.dma(ymix[512 + h * 128:640 + h * 128, r0:r0 + 128], otb[:], "sync")

                for d in range(2):
                    k.memset(Sst[:], 0.0)
                    order = CH_ORDER[d]
                    prep(d, order[0], 0)
                    for ci, c in enumerate(order):
                        par = ci % 2
                        gens = [unit(d, c, par, h) for h in range(4)]
                        first = True
                        while gens:
                            nxt = []
                            for g in gens:
                                try:
                                    next(g)
                                    nxt.append(g)
                                except StopIteration:
                                    pass
                            gens = nxt
                            if first and ci + 1 < len(order):
                                prep(d, order[ci + 1], 1 - par)
                                first = False
                k.S.barrier()

        MIX = {"s5": s5_mixer, "gdn": gdn_mixer, "m2": m2_mixer}

        DBG = os.environ.get("DBGDUMP", "").split(",")

        def dbg_dump(name, ap, shape, dt=F32):
            if name not in DBG:
                return
            o = nc.dram_tensor("dbg_" + name, list(shape), dt, kind="ExternalOutput").ap()
            k.dma(o, ap, "sync")

        def dump_x():
            xo = dbgx.rearrange("(kt p) t -> p kt t", p=128)
            for kt in range(8):
                k.dma(xo[:, kt, :], xT[:, kt, :], "sync")

        skip = os.environ.get("SKIPMIX", "").split(",")
        for l in range(nlayers):
            odd = (l % 2 == 1)
            lastl = (l == nlayers - 1)
            adaln(l)
            rmsnorm_mod(l, n1g[l], 0, 1, odd)
            dbg_dump("h%d" % l, hT[:], [128, 8, NT], BF16)
            dbg_dump("mod%d" % l, mod[:], [128, 2, 48])
            dbg_dump("gs%d" % l, gs[:], [128, 2, 8])
            if "s5" not in skip:
                MIX["s5"](l)
            if "gdn" not in skip:
                MIX["gdn"](l)
            if "m2" not in skip:
                MIX["m2"](l)
            if lastl and stop == "mix":
                break
            out_proj(l, odd)
            if lastl and stop == "oproj":
                dump_x()
                break
            rmsnorm_mod(l, n2g[l], 3, 4, False)
            if l % 2 == 0:
                ffn_dense(l // 2)
            else:
                ffn_moe(l // 2)
            if lastl and stop == "ffn":
                dump_x()
                break
        if stop is None:
            rmsnorm_mod(0, fng, 0, 0, False, final=True)
        k.S.emit()
    return nc


_NC_CACHE = {}


def _prep_shared(inp):
    f = lambda a: np.ascontiguousarray(np.asarray(a, dtype=np.float32))
    g = {}
    g["ada_w"] = f(inp["ada_w"])
    g["ada_b"] = f(np.asarray(inp["ada_b"]).reshape(DEPTH, 48, 128).transpose(0, 2, 1))
    g["n1g"] = f(np.asarray(inp["norm1_g"]).reshape(DEPTH, 8, 128).transpose(0, 2, 1))
    g["n2g"] = f(np.asarray(inp["norm2_g"]).reshape(DEPTH, 8, 128).transpose(0, 2, 1))
    g["fng"] = f(np.asarray(inp["final_norm_g"]).reshape(8, 128).T)
    g["w_in"] = f(inp["w_in"])
    g["w_out"] = f(inp["w_out"])
    g["ffn_wg"] = f(inp["ffn_w_gate"]); g["ffn_wu"] = f(inp["ffn_w_up"]); g["ffn_wd"] = f(inp["ffn_w_down"])
    g["moe_r"] = f(inp["moe_router"])
    g["moe_wg"] = f(inp["moe_w_gate"]); g["moe_wu"] = f(inp["moe_w_up"]); g["moe_wd"] = f(inp["moe_w_down"])
    lam_re = np.asarray(inp["s5_lam_re"]); lam_im = np.asarray(inp["s5_lam_im"]); log_dt = np.asarray(inp["s5_log_dt"])
    b_re = np.asarray(inp["s5_b_re"]); b_im = np.asarray(inp["s5_b_im"])
    c_re = np.asarray(inp["s5_c_re"]); c_im = np.asarray(inp["s5_c_im"])
    s5_lam = np.zeros((DEPTH, 128, 3, 32), np.float32)
    s5_B = np.zeros((DEPTH, 32, 2, 128, 128), np.float32)
    s5_C = np.zeros((DEPTH, 32, 2, 128, 128), np.float32)
    for st in range(16):
        for d in range(2):
            u = st * 2 + d
            for g2 in range(2):
                gi = 2 * st + g2
                ps = slice(g2 * 64, (g2 + 1) * 64)
                s5_lam[:, ps, 0, u] = lam_re[:, d, gi, :]
                s5_lam[:, ps, 1, u] = lam_im[:, d, gi, :]
                s5_lam[:, ps, 2, u] = log_dt[:, d, gi][:, None]
                ch0 = 16 * (2 * (st % 4) + g2)
                s5_B[:, u, 0, ps, ch0:ch0 + 16] = b_re[:, d, gi]
                s5_B[:, u, 1, ps, ch0:ch0 + 16] = b_im[:, d, gi]
                s5_C[:, u, 0, ps, ch0:ch0 + 16] = c_re[:, d, gi].transpose(0, 2, 1)
                s5_C[:, u, 1, ps, ch0:ch0 + 16] = c_im[:, d, gi].transpose(0, 2, 1)
    g["s5_lam"] = s5_lam; g["s5_B"] = s5_B; g["s5_C"] = s5_C
    g["s5_d"] = f(np.asarray(inp["s5_d"]).reshape(DEPTH, 4, 128).transpose(0, 2, 1))
    g["s5_glu"] = f(inp["s5_w_glu"])
    g["gdn_cw"] = f(np.asarray(inp["gdn_conv_w"]).reshape(DEPTH, 5, 12, 128).transpose(0, 3, 2, 1))
    ab = np.concatenate([np.asarray(inp["gdn_a_log"]).reshape(DEPTH, 8), np.asarray(inp["gdn_dt_bias"]).reshape(DEPTH, 8)], 1)
    g["gdn_ab"] = f(np.broadcast_to(ab[:, None, :], (DEPTH, 128, 16)))
    g["gdn_ng"] = f(np.broadcast_to(np.asarray(inp["gdn_norm_g"])[:, None, :], (DEPTH, 128, 128)))
    cw = np.asarray(inp["m2_conv_w"]).reshape(DEPTH, 5, 12, 128).transpose(0, 3, 2, 1)
    cb = np.asarray(inp["m2_conv_b"]).reshape(DEPTH, 12, 128).transpose(0, 2, 1)[..., None]
    g["m2_cw"] = f(np.concatenate([cw, cb], axis=3))
    ab = np.concatenate([np.asarray(inp["m2_a_log"]).reshape(DEPTH, 32), np.asarray(inp["m2_dt_bias"]).reshape(DEPTH, 32),
                         np.asarray(inp["m2_d"]).reshape(DEPTH, 16)], 1)
    g["m2_ab"] = f(np.broadcast_to(ab[:, None, :], (DEPTH, 128, 80)))
    g["m2_ng"] = f(np.broadcast_to(np.asarray(inp["m2_norm_g"])[:, None, :], (DEPTH, 128, 1024)))
    return g


def kernel(**inp):
    n = 8
    x = np.asarray(inp["x"], dtype=np.float32)
    ctx = np.asarray(inp["ctx"], dtype=np.float32)
    c = np.asarray(inp["c"], dtype=np.float32)
    c_ctx = np.asarray(inp["c_ctx"], dtype=np.float32)
    shared = _prep_shared(inp)
    if "nc" not in _NC_CACHE:
        _NC_CACHE["nc"] = build_program()
    nc = _NC_CACHE["nc"]
    in_maps = []
    for b in range(n):
        m = dict(shared)
        m["xT"] = np.ascontiguousarray(np.concatenate([ctx[b], x[b]], axis=0).T)
        cs = np.stack([c[b].reshape(8, 128).T, c_ctx.reshape(8, 128).T], axis=2)
        m["cs"] = np.ascontiguousarray(cs.astype(np.float32))
        in_maps.append(m)
    res = run_bass_kernel_spmd(nc, in_maps, core_ids=list(range(n)))
    out = np.stack([np.asarray(r["outT"], dtype=np.float32).T for r in res.results], axis=0)
    return np.ascontiguousarray(out)
```

```python
import math
import os
from contextlib import ExitStack
import numpy as np
import concourse.bass as bass
import concourse.mybir as mybir
from concourse.bass_utils import run_bass_kernel_spmd

F32 = mybir.dt.float32
BF16 = mybir.dt.bfloat16
I32 = mybir.dt.int32
AF = mybir.ActivationFunctionType
ALU = mybir.AluOpType
AX = mybir.AxisListType

COMPUTE = ["tensor", "vector", "scalar", "gpsimd"]
ENGS = ["sync", "tensor", "vector", "scalar", "gpsimd"]
DMA_RING = 6
SAME_ENGINE_SYNC = True

D = 1024
NT = 2304
NCTX = 256
DEPTH = 4
DIN = 5168
DFF = 2816
NE = 8
TT = [(0, 256), (256, 512), (768, 512), (1280, 512), (1792, 512)]
NCH = 18
EPS = 1e-6
TWO_PI = 2.0 * math.pi


def _key(k):
    if isinstance(k, (str, tuple)):
        return k
    t = getattr(k, "tensor", k)
    return t.name


class Sched:
    def __init__(self, nc):
        self.nc = nc
        self.ops = {e: [] for e in ENGS}
        self.cnt = {e: 0 for e in COMPUTE}
        self.seen = {e: {} for e in ENGS}
        self.last_w = {}
        self.readers = {}
        self.ring_tot = {}
        self.ring_pos = {e: 0 for e in ENGS}
        self.ring_know = {}
        self.sem_names = ["c_" + e for e in COMPUTE]
        for e in ("sync", "scalar", "gpsimd"):
            for i in range(DMA_RING):
                n = "d_%s_%d" % (e, i)
                self.sem_names.append(n)
                self.ring_tot[n] = 0
        self.sems = {}
        self.n_ops = 0

    def _need(self, eng, tok, waits, is_dma=False):
        s, v, know = tok
        if self.seen[eng].get(s, 0) >= v:
            return
        if (not is_dma) and s == "c_" + eng and (eng == "tensor" or not SAME_ENGINE_SYNC):
            return
        waits[s] = max(waits.get(s, 0), v)
        sn = self.seen[eng]
        for ks, kv in know.items():
            if sn.get(ks, 0) < kv:
                sn[ks] = kv
        sn[s] = max(sn.get(s, 0), v)

    def _deps(self, eng, reads, writes, waits, is_dma=False):
        for k in reads:
            k = _key(k)
            t = self.last_w.get(k)
            if t is not None:
                self._need(eng, t, waits, is_dma)
            if isinstance(k, str) and k.startswith("ps"):
                for t in list(self.readers.get(k, {}).values()):
                    if t[0] != "c_" + eng:
                        self._need(eng, t, waits, is_dma)
        for k in writes:
            k = _key(k)
            t = self.last_w.get(k)
            if t is not None:
                self._need(eng, t, waits, is_dma)
            for t in list(self.readers.get(k, {}).values()):
                self._need(eng, t, waits, is_dma)

    def _publish(self, tok, reads, writes):
        for k in reads:
            self.readers.setdefault(_key(k), {})[tok[0]] = tok
        for k in writes:
            k = _key(k)
            self.last_w[k] = tok
            self.readers[k] = {}

    def op(self, eng, fn, reads=(), writes=()):
        waits = {}
        self._deps(eng, reads, writes, waits)
        self.cnt[eng] += 1
        s = "c_" + eng
        know = dict(self.seen[eng])
        know[s] = self.cnt[eng]
        tok = (s, self.cnt[eng], know)
        self.ops[eng].append((waits, fn, s, 1))
        self._publish(tok, reads, writes)
        self.n_ops += 1
        return tok

    def dma(self, eng, fn, reads=(), writes=()):
        waits = {}
        self._deps(eng, reads, writes, waits, True)
        i = self.ring_pos[eng]
        self.ring_pos[eng] = (i + 1) % DMA_RING
        s = "d_%s_%d" % (eng, i)
        if self.ring_tot[s] > 0 and self.seen[eng].get(s, 0) < self.ring_tot[s]:
            waits[s] = self.ring_tot[s]
            self.seen[eng][s] = self.ring_tot[s]
            for ks, kv in self.ring_know.get(s, {}).items():
                if self.seen[eng].get(ks, 0) < kv:
                    self.seen[eng][ks] = kv
        self.ring_tot[s] += 16
        know = dict(self.seen[eng])
        self.ring_know[s] = know
        tok = (s, self.ring_tot[s], know)
        self.ops[eng].append((waits, fn, s, 16))
        self._publish(tok, reads, writes)
        self.n_ops += 1
        return tok

    def barrier(self):
        toks = []
        for e in COMPUTE:
            if self.cnt[e] > 0:
                toks.append(("c_" + e, self.cnt[e], {}))
        for s, v in self.ring_tot.items():
            if v > 0:
                toks.append((s, v, {}))
        for e in ENGS:
            waits = {}
            for t in toks:
                self._need(e, t, waits)
            if waits:
                self.ops[e].append((waits, None, None, 0))

    def emit(self):
        nc = self.nc
        self.barrier()
        with ExitStack() as st:
            for n in self.sem_names:
                self.sems[n] = st.enter_context(nc.semaphore(n))
            block = st.enter_context(nc.Block())
            sems = self.sems

            def run(eng_name):
                def body(eng):
                    for waits, fn, s, inc in self.ops[eng_name]:
                        for ws, wv in waits.items():
                            eng.wait_ge(sems[ws], wv)
                        if fn is not None:
                            ins = fn(eng)
                            ins.then_inc(sems[s], inc)
                return body

            block.sync(run("sync"))
            block.tensor(run("tensor"))
            block.vector(run("vector"))
            block.scalar(run("scalar"))
            block.gpsimd(run("gpsimd"))


def _aps(*xs):
    return [x for x in xs if x is not None and not isinstance(x, (int, float))]


class K:
    def __init__(self, nc):
        self.nc = nc
        self.S = Sched(nc)
        self.rr = 0

    def mm(self, ps, lhsT, rhs, start=True, stop=True):
        rd = [lhsT, rhs] + ([] if start else [ps])
        return self.S.op("tensor", lambda e: e.matmul(ps, lhsT=lhsT, rhs=rhs, start=start, stop=stop), rd, [ps])

    def tr(self, ps, in_, ident):
        return self.S.op("tensor", lambda e: e.transpose(ps, in_, ident), [in_, ident], [ps])

    def act(self, out, in_, func, bias=None, scale=1.0, accum=None, eng="scalar"):
        kw = {}
        if bias is not None:
            kw["bias"] = bias
        if accum is not None:
            kw["accum_out"] = accum
        return self.S.op("scalar", lambda e: e.activation(out=out, in_=in_, func=func, scale=scale, **kw),
                         _aps(in_, bias, scale), _aps(out, accum))

    def tt(self, out, a, b, op, eng="vector"):
        return self.S.op(eng, lambda e: e.tensor_tensor(out=out, in0=a, in1=b, op=op), [a, b], [out])

    def ts(self, out, a, s1, op0, s2=None, op1=None, eng="vector", accum=None):
        def f(e):
            kw = {}
            if op1 is not None:
                kw["op1"] = op1
            if accum is not None:
                kw["accum_out"] = accum
            return e.tensor_scalar(out=out, in0=a, scalar1=s1, scalar2=s2, op0=op0, **kw)
        return self.S.op(eng, f, _aps(a, s1, s2), _aps(out, accum))

    def stt(self, out, a, s, b, op0, op1, eng="vector"):
        eng = "vector"
        return self.S.op(eng, lambda e: e.scalar_tensor_tensor(out=out, in0=a, scalar=s, in1=b, op0=op0, op1=op1),
                         _aps(a, s, b), [out])

    def cp(self, out, in_, eng="vector"):
        if eng == "scalar":
            return self.S.op(eng, lambda e: e.copy(out=out, in_=in_), [in_], [out])
        return self.S.op(eng, lambda e: e.tensor_copy(out=out, in_=in_), [in_], [out])

    def memset(self, out, v, eng="gpsimd"):
        return self.S.op(eng, lambda e: e.memset(out, v), [], [out])

    def red(self, out, in_, op, eng="vector"):
        return self.S.op(eng, lambda e: e.tensor_reduce(out=out, in_=in_, axis=AX.X, op=op), [in_], [out])

    def recip(self, out, in_):
        return self.S.op("vector", lambda e: e.reciprocal(out=out, in_=in_), [in_], [out])

    def scan(self, out, d0, d1, init):
        return self.S.op("vector", lambda e: e.tensor_tensor_scan(out=out, data0=d0, data1=d1, initial=init,
                                                                  op0=ALU.mult, op1=ALU.add),
                         _aps(d0, d1, init), [out])

    def dma(self, out, in_, q="sync"):
        return self.S.dma(q, lambda e: e.dma_start(out=out, in_=in_), [in_], [out])

    def ev(self):
        self.rr ^= 1
        return "vector" if self.rr else "gpsimd"


def perm_ap(t3, kt, lo, n):
    c0 = (lo - NCTX) // 32
    ncol = n // 32
    base = t3[:, kt, NCTX:NT]
    v = base.rearrange("p (r c) -> p c r", c=64)
    return v[:, c0:c0 + ncol, :]


def build_program(nlayers=DEPTH, stop=None):
    nc = bass.Bass("TRN2", target_bir_lowering=False)
    k = K(nc)
    _cnt = [0]

    def sbt(name, shape, dt):
        _cnt[0] += 1
        return nc.sbuf_tensor("%s_%d" % (name, _cnt[0]), shape, dt)

    def din(name, shape, dt=F32):
        return nc.dram_tensor(name, list(shape), dt, kind="ExternalInput").ap()

    def dscr(name, shape, dt=F32):
        return nc.dram_tensor(name, list(shape), dt, kind="Internal").ap()

    xT_in = din("xT", [D, NT])
    cs_in = din("cs", [128, 8, 2])
    ada_w = din("ada_w", [DEPTH, D, 6 * D])
    ada_b = din("ada_b", [DEPTH, 128, 48])
    n1g = din("n1g", [DEPTH, 128, 8])
    n2g = din("n2g", [DEPTH, 128, 8])
    fng = din("fng", [128, 8])
    w_in = din("w_in", [DEPTH, D, DIN])
    w_out = din("w_out", [DEPTH, 2048, D])
    ffn_wg = din("ffn_wg", [2, D, DFF])
    ffn_wu = din("ffn_wu", [2, D, DFF])
    ffn_wd = din("ffn_wd", [2, DFF, D])
    moe_r = din("moe_r", [2, D, NE])
    moe_wg = din("moe_wg", [2, NE, D, DFF])
    moe_wu = din("moe_wu", [2, NE, D, DFF])
    moe_wd = din("moe_wd", [2, NE, DFF, D])
    s5_lam = din("s5_lam", [DEPTH, 128, 3, 32])
    s5_B = din("s5_B", [DEPTH, 32, 2, 128, 128])
    s5_C = din("s5_C", [DEPTH, 32, 2, 128, 128])
    s5_d = din("s5_d", [DEPTH, 128, 4])
    s5_glu = din("s5_glu", [DEPTH, 512, 512])
    gdn_cw = din("gdn_cw", [DEPTH, 128, 12, 5])
    gdn_ab = din("gdn_ab", [DEPTH, 128, 16])
    gdn_ng = din("gdn_ng", [DEPTH, 128, 128])
    m2_cw = din("m2_cw", [DEPTH, 128, 12, 6])
    m2_ab = din("m2_ab", [DEPTH, 128, 80])
    m2_ng = din("m2_ng", [DEPTH, 128, 1024])
    out_T = nc.dram_tensor("outT", [D, 2048], F32, kind="ExternalOutput").ap()

    if stop == "mix":
        ymix = nc.dram_tensor("ymix", [2048, NT], BF16, kind="ExternalOutput").ap()
    else:
        ymix = dscr("ymix", [2048, NT], BF16)
    dbgx = nc.dram_tensor("dbgx", [D, NT], F32, kind="ExternalOutput").ap() if stop in ("oproj", "ffn", "norm1") else None
    yfwd_g = dscr("yfwd_g", [4, NT, 128])
    yfwd_m = dscr("yfwd_m", [NT, 1024])

    with ExitStack() as st:
        def sb(name, shape, dt=F32):
            return st.enter_context(sbt(name, list(shape), dt))

        def psum(name, shape=(128, 512), dt=F32):
            return st.enter_context(nc.psum_tensor(name, list(shape), dt))

        xT = sb("xTs", [128, 8, NT])
        hT = sb("hTs", [128, 8, NT], BF16)
        mod = sb("mod", [128, 2, 48])
        gs = sb("gs", [128, 2, 8])
        ident = sb("ident", [128, 128])
        identb = sb("identb", [128, 128], BF16)
        ones = sb("ones", [128, 128])
        epsc = sb("epsc", [128, 1])
        PS = [psum("ps%d" % i) for i in range(8)]

        DBG = os.environ.get("DBGDUMP", "").split(",")

        def dbg_dump(name, ap, shape, dt=F32):
            if name not in DBG:
                return
            o = nc.dram_tensor("dbg_" + name, list(shape), dt, kind="ExternalOutput").ap()
            k.dma(o, ap, "sync")

        k.memset(ident[:], 0.0)
        k.S.op("gpsimd", lambda e: e.affine_select(out=ident[:], in_=ident[:], pattern=[[-1, 128]],
                                                   compare_op=ALU.not_equal, fill=1.0, base=0,
                                                   channel_multiplier=1), [ident], [ident])
        k.cp(identb[:], ident[:])
        k.memset(ones[:], 1.0)
        k.memset(epsc[:], EPS)

        xv = xT_in.rearrange("(kt p) t -> p kt t", p=128)
        for kt in range(8):
            k.dma(xT[:, kt, :], xv[:, kt, :], "sync")

        csr = sb("csr", [128, 8, 2])
        k.dma(csr[:], cs_in, "sync")
        cs = sb("css", [128, 8, 2])
        k.act(cs[:], csr[:], AF.Silu)

        def adaln(l):
            with ExitStack() as s2:
                wb = [s2.enter_context(sbt("adaw%d" % i, [128, 8, 512], F32)) for i in range(2)]
                bb = s2.enter_context(sbt("adab", [128, 48], F32))
                k.dma(bb[:], ada_b[l], "sync")
                wv = ada_w[l].rearrange("(kt p) n -> p kt n", p=128)
                for blk in range(12):
                    w = wb[blk % 2]
                    k.dma(w[:], wv[:, :, blk * 512:(blk + 1) * 512], "sync" if blk % 2 == 0 else "scalar")
                    for j in range(4):
                        col = blk * 4 + j
                        ps = PS[col % 2]
                        for kt in range(8):
                            k.mm(ps[:, 0:2], w[:, kt, j * 128:(j + 1) * 128], cs[:, kt, :], kt == 0, kt == 7)
                        for jj in range(2):
                            k.ts(mod[:, jj, col:col + 1], ps[:, jj:jj + 1], bb[:, col:col + 1], ALU.add)
                k.S.barrier()

        def rmsnorm_mod(l, gsrc, shift_idx, scale_idx, permute, final=False):
            with ExitStack() as s2:
                g = s2.enter_context(sbt("ng", [128, 8], F32))
                sq = [s2.enter_context(sbt("sq%d" % i, [128, 512], F32)) for i in range(2)]
                rstd = s2.enter_context(sbt("rstd", [128, 512], F32))
                tmp = [s2.enter_context(sbt("nt%d" % i, [128, 512], F32)) for i in range(2)]
                k.dma(g[:], gsrc, "sync")
                if not final:
                    for j in range(2):
                        k.ts(gs[:, j, :], mod[:, j, scale_idx * 8:(scale_idx + 1) * 8], 1.0, ALU.add)
                        k.tt(gs[:, j, :], gs[:, j, :], g[:], ALU.mult)
                for ti, (lo, n) in enumerate(TT):
                    if final and ti == 0:
                        continue
                    ps = PS[2 + ti % 2]
                    for kt in range(8):
                        s = sq[kt % 2]
                        k.act(s[:, :n], xT[:, kt, lo:lo + n], AF.Square)
                        k.mm(ps[:, :n], ones[:], s[:, :n], kt == 0, kt == 7)
                    k.act(rstd[:, :n], ps[:, :n], AF.Sqrt, bias=epsc[:, 0:1], scale=1.0 / D)
                    k.recip(rstd[:, :n], rstd[:, :n])
                    j = 1 if ti == 0 else 0
                    for kt in range(8):
                        t = tmp[kt % 2]
                        k.tt(t[:, :n], xT[:, kt, lo:lo + n], rstd[:, :n], ALU.mult)
                        if final:
                            k.ts(t[:, :n], t[:, :n], g[:, kt:kt + 1], ALU.mult)
                            k.dma(out_T[kt * 128:(kt + 1) * 128, lo - NCTX:lo - NCTX + n], t[:, :n], "sync")
                        else:
                            if permute and ti > 0:
                                r0 = (lo - NCTX) // 64
                                dst = hT[:, kt, NCTX:NT].rearrange("p (c r) -> p r c", r=32)[:, r0:r0 + n // 64, :]
                                src = t[:, :n].rearrange("p (r c) -> p r c", c=64)
                            else:
                                dst = hT[:, kt, lo:lo + n]
                                src = t[:, :n]
                            k.ts(dst, src, gs[:, j, kt:kt + 1], ALU.mult,
                                 mod[:, j, shift_idx * 8 + kt:shift_idx * 8 + kt + 1], ALU.add)
                k.S.barrier()

        def out_proj(l, permute):
            with ExitStack() as s2:
                wo = s2.enter_context(sbt("wo", [128, 16, D], BF16))
                yb = [s2.enter_context(sbt("yb%d" % i, [128, 16, 512], BF16)) for i in range(2)]
                wv = w_out[l].rearrange("(kt p) n -> p kt n", p=128)
                for q4 in range(4):
                    k.dma(wo[:, q4 * 4:(q4 + 1) * 4, :], wv[:, q4 * 4:(q4 + 1) * 4, :], "gpsimd")
                yv = ymix.rearrange("(kt p) t -> p kt t", p=128)
                for ti, (lo, n) in enumerate(TT):
                    y = yb[ti % 2]
                    k.dma(y[:, :, :n], yv[:, :, lo:lo + n], "sync")
                    j = 1 if ti == 0 else 0
                    for nt in range(8):
                        ps = PS[nt % 4]
                        for kt in range(16):
                            k.mm(ps[:, :n], wo[:, kt, nt * 128:(nt + 1) * 128], y[:, kt, :n], kt == 0, kt == 15)
                        if permute and ti > 0:
                            dst = perm_ap(xT, nt, lo, n)
                            src = ps[:, :n].rearrange("p (c r) -> p c r", r=32)
                        else:
                            dst = xT[:, nt, lo:lo + n]
                            src = ps[:, :n]
                        k.stt(dst, src, mod[:, j, 16 + nt:17 + nt], dst, ALU.mult, ALU.add)
                k.S.barrier()

        def ffn_expert(wg, wu, wd, gbc, bufs):
            wgb, wub, wdb, hid, sgs = bufs
            wgv = wg.rearrange("(kt p) f -> p kt f", p=128)
            wuv = wu.rearrange("(kt p) f -> p kt f", p=128)
            wdv = wd.rearrange("(ft p) n -> p ft n", p=128)
            jobs = [(fb, ti) for fb in range(11) for ti in range(5)]

            def stage_a(j):
                fb, ti = jobs[j]
                b = fb % 2
                if ti == 0:
                    k.dma(wgb[b][:], wgv[:, :, fb * 256:(fb + 1) * 256], "gpsimd")
                    k.dma(wub[b][:], wuv[:, :, fb * 256:(fb + 1) * 256], "gpsimd")
                    k.dma(wdb[b][:], wdv[:, fb * 2:(fb + 1) * 2, :], "gpsimd")
                lo, n = TT[ti]
                hb = hid[j % 2]
                for f in range(2):
                    pg = PS[0 + f]
                    pu = PS[2 + f]
                    for kt in range(8):
                        k.mm(pg[:, :n], wgb[b][:, kt, f * 128:(f + 1) * 128], hT[:, kt, lo:lo + n], kt == 0, kt == 7)
                    for kt in range(8):
                        k.mm(pu[:, :n], wub[b][:, kt, f * 128:(f + 1) * 128], hT[:, kt, lo:lo + n], kt == 0, kt == 7)
                    sg = sgs[f]
                    k.act(sg[:, :n], pg[:, :n], AF.Silu)
                    if gbc is not None:
                        k.tt(sg[:, :n], sg[:, :n], gbc[:, lo:lo + n], ALU.mult, eng="gpsimd")
                    k.tt(hb[:, f, :n], sg[:, :n], pu[:, :n], ALU.mult)

            def stage_b(j):
                fb, ti = jobs[j]
                b = fb % 2
                lo, n = TT[ti]
                jj = 1 if ti == 0 else 0
                hb = hid[j % 2]
                for nt in range(8):
                    ps = PS[4 + nt % 4]
                    for f in range(2):
                        k.mm(ps[:, :n], wdb[b][:, f, nt * 128:(nt + 1) * 128], hb[:, f, :n], f == 0, f == 1)
                    k.stt(xT[:, nt, lo:lo + n], ps[:, :n], mod[:, jj, 40 + nt:41 + nt], xT[:, nt, lo:lo + n],
                          ALU.mult, ALU.add)

            stage_a(0)
            for j in range(len(jobs)):
                if j + 1 < len(jobs):
                    stage_a(j + 1)
                stage_b(j)

        def ffn_bufs(s2):
            wgb = [s2.enter_context(sbt("wgb%d" % i, [128, 8, 256], BF16)) for i in range(2)]
            wub = [s2.enter_context(sbt("wub%d" % i, [128, 8, 256], BF16)) for i in range(2)]
            wdb = [s2.enter_context(sbt("wdb%d" % i, [128, 2, D], BF16)) for i in range(2)]
            hid = [s2.enter_context(sbt("hid%d" % i, [128, 2, 512], BF16)) for i in range(2)]
            sg = [s2.enter_context(sbt("sg%d" % i, [128, 512], F32)) for i in range(2)]
            return (wgb, wub, wdb, hid, sg)

        def ffn_dense(j):
            with ExitStack() as s2:
                bufs = ffn_bufs(s2)
                ffn_expert(ffn_wg[j], ffn_wu[j], ffn_wd[j], None, bufs)
                k.S.barrier()

        def ffn_moe(j):
            with ExitStack() as s2:
                bufs = ffn_bufs(s2)
                rw = s2.enter_context(sbt("rw", [128, 8, NE], BF16))
                gT = s2.enter_context(sbt("gT", [NE, NT], F32))
                sel = s2.enter_context(sbt("sel", [NE, NE, 128], F32))
                gbc = s2.enter_context(sbt("gbc", [128, NT], F32))
                lg = s2.enter_context(sbt("lg", [128, NE], F32))
                sm = s2.enter_context(sbt("sm", [128, 8], F32))
                m1 = s2.enter_context(sbt("m1", [128, NE], F32))
                m2 = s2.enter_context(sbt("m2", [128, NE], F32))
                l2 = s2.enter_context(sbt("l2", [128, NE], F32))
                gt = s2.enter_context(sbt("gt", [128, NE], F32))
                k.dma(rw[:], moe_r[j].rearrange("(kt p) e -> p kt e", p=128), "gpsimd")
                for e in range(NE):
                    k.cp(sel[:, e, :], ident[0:NE, e:e + 1].to_broadcast([NE, 128]))
                for c in range(NCH):
                    lo = c * 128
                    ps = PS[c % 2]
                    for kt in range(8):
                        k.mm(ps[:, 0:NE], hT[:, kt, lo:lo + 128], rw[:, kt, :], kt == 0, kt == 7)
                    k.cp(lg[:], ps[:, 0:NE])
                    k.red(sm[:, 0:1], lg[:], ALU.max)
                    k.ts(m1[:], lg[:], sm[:, 0:1], ALU.is_equal)
                    k.stt(l2[:], m1[:], -1e30, lg[:], ALU.mult, ALU.add)
                    k.red(sm[:, 1:2], l2[:], ALU.max)
                    k.ts(m2[:], l2[:], sm[:, 1:2], ALU.is_equal)
                    k.tt(sm[:, 2:3], sm[:, 0:1], sm[:, 1:2], ALU.subtract)
                    k.act(sm[:, 3:4], sm[:, 2:3], AF.Sigmoid)
                    k.ts(sm[:, 4:5], sm[:, 3:4], -1.0, ALU.mult, 1.0, ALU.add)
                    k.ts(gt[:], m1[:], sm[:, 3:4], ALU.mult)
                    k.stt(gt[:], m2[:], sm[:, 4:5], gt[:], ALU.mult, ALU.add)
                    pt = PS[2 + c % 2]
                    k.tr(pt[0:NE, 0:128], gt[:], ident[:])
                    k.cp(gT[:, lo:lo + 128], pt[0:NE, 0:128])
                for e in range(NE):
                    for ti, (lo, n) in enumerate(TT):
                        ps = PS[6 + ti % 2]
                        k.mm(ps[:, :n], sel[:, e, :], gT[:, lo:lo + n])
                        k.cp(gbc[:, lo:lo + n], ps[:, :n], eng="scalar")
                    ffn_expert(moe_wg[j, e], moe_wu[j, e], moe_wd[j, e], gbc, bufs)
                k.S.barrier()

        def load_win(dst, l, c0, ncols, q="gpsimd"):
            wv = w_in[l].rearrange("(kt p) c -> p kt c", p=128)
            k.dma(dst[:, :, :ncols], wv[:, :, c0:c0 + ncols], q)

        def proj_fm(wt, ncols, ti, ps):
            lo, n = TT[ti]
            for kt in range(8):
                k.mm(ps[:ncols, :n], wt[:, kt, :ncols], hT[:, kt, lo:lo + n], kt == 0, kt == 7)

        def proj_tm(wt, ncols, c, ps):
            for kt in range(8):
                k.mm(ps[:, :ncols], hT[:, kt, c * 128:(c + 1) * 128], wt[:, kt, :ncols], kt == 0, kt == 7)

        def sincos(s_out, c_out, ang, ki, tf, shape_slc):
            sl = shape_slc
            k.ts(ki[sl], ang, 1.0 / TWO_PI, ALU.mult)
            k.ts(tf[sl], ki[sl], -TWO_PI, ALU.mult)
            k.tt(tf[sl], tf[sl], ang, ALU.add)
            k.ts(tf[sl], tf[sl], math.pi, ALU.min, -math.pi, ALU.max)
            k.act(s_out, tf[sl], AF.Sin)
            k.ts(ki[sl], ang, 1.0 / TWO_PI, ALU.mult, 0.25, ALU.add)
            k.ts(tf[sl], ki[sl], -TWO_PI, ALU.mult)
            k.stt(tf[sl], ang, math.pi / 2, tf[sl], ALU.add, ALU.add)
            k.ts(tf[sl], tf[sl], math.pi, ALU.min, -math.pi, ALU.max)
            k.act(c_out, tf[sl], AF.Sin)

        s5yg = dscr("s5yg", [512, NT], BF16)

        def s5_mixer(l):
            with ExitStack() as s2:
                def T(name, shape, dt=F32):
                    return s2.enter_context(sbt("s5" + name, list(shape), dt))
                lam = T("lam", [128, 3, 32])
                pa = T("pa", [128, 32]); pdt = T("pdt", [128, 32]); par = T("par", [128, 32])
                pth = T("pth", [128, 32]); pr = T("pr", [128, 32]); psn = T("psn", [128, 32])
                pcs = T("pcs", [128, 32]); pki = T("pki", [128, 32], I32); ptf = T("ptf", [128, 32])
                lbr = T("lbr", [128, 32]); lbi = T("lbi", [128, 32]); den = T("den", [128, 32])
                gre = T("gre", [128, 32]); gim = T("gim", [128, 32]); ngim = T("ngim", [128, 32])
                t32 = T("t32", [128, 32])
                t96i = T("t96i", [128, 96], I32); t96 = T("t96", [128, 96])
                a96 = T("a96", [128, 96]); k96 = T("k96", [128, 96], I32); f96 = T("f96", [128, 96])
                s96 = T("s96", [128, 96]); c96 = T("c96", [128, 96])
                ttmp = [T("ttmp%d" % i, [128, 512]) for i in range(2)]
                ctabL = [T("ctab%d" % i, [128, NT]) for i in range(2)]
                stabL = [T("stab%d" % i, [128, NT]) for i in range(2)]
                SG = []
                for i in range(2):
                    b = {nm: T("%s%d" % (nm, i), [128, 512]) for nm in ("A", "Bb", "T1", "G1", "G2")}
                    b["HR"] = T("HR%d" % i, [128, 512], BF16); b["HI"] = T("HI%d" % i, [128, 512], BF16)
                    SG.append(b)
                car = T("car", [128, 2])
                ubf = T("ubf", [128, NT], BF16)
                wt = T("wt", [128, 8, 128], BF16)
                UB = []
                for i in range(2):
                    b = {nm: T("%s%d" % (nm, i), [128, 128]) for nm in ("Bre", "Bim", "Cre", "Cim", "Btr", "Bti")}
                    for nm in ("BtR", "BtI", "CbR", "CbI"):
                        b[nm] = T("%s%d" % (nm, i), [128, 128], BF16)
                    UB.append(b)
                dsk = T("dsk", [128, 4])
                XT = [T("X%d" % i, [128, 512]) for i in range(4)]
                yy = XT[0]; y2 = XT[1]; ygb = T("ygb", [128, 512], BF16)

                k.dma(lam[:], s5_lam[l], "sync")
                k.dma(dsk[:], s5_d[l], "sync")
                k.S.op("gpsimd", lambda e: e.iota(t96i[:, 0:48], pattern=[[48, 48]], base=0, channel_multiplier=0), [], [t96i])
                k.S.op("gpsimd", lambda e: e.iota(t96i[:, 48:96], pattern=[[1, 48]], base=0, channel_multiplier=0), [t96i], [t96i])
                k.cp(t96[:], t96i[:])
                k.ts(pa[:], lam[:, 0, :], -1e-4, ALU.min)
                k.act(pdt[:], lam[:, 2, :], AF.Exp)
                k.tt(par[:], pa[:], pdt[:], ALU.mult)
                k.tt(pth[:], lam[:, 1, :], pdt[:], ALU.mult)
                k.act(pr[:], par[:], AF.Exp)
                sincos(psn[:], pcs[:], pth[:], pki, ptf, (slice(None), slice(None)))
                k.tt(lbr[:], pr[:], pcs[:], ALU.mult)
                k.tt(lbi[:], pr[:], psn[:], ALU.mult)
                k.ts(lbr[:], lbr[:], -1.0, ALU.add)
                k.tt(den[:], pa[:], pa[:], ALU.mult)
                k.tt(t32[:], lam[:, 1, :], lam[:, 1, :], ALU.mult)
                k.tt(den[:], den[:], t32[:], ALU.add)
                k.recip(den[:], den[:])
                k.tt(gre[:], lbr[:], pa[:], ALU.mult)
                k.tt(t32[:], lbi[:], lam[:, 1, :], ALU.mult)
                k.tt(gre[:], gre[:], t32[:], ALU.add)
                k.tt(gre[:], gre[:], den[:], ALU.mult)
                k.tt(gim[:], lbi[:], pa[:], ALU.mult)
                k.tt(t32[:], lbr[:], lam[:, 1, :], ALU.mult)
                k.tt(gim[:], gim[:], t32[:], ALU.subtract)
                k.tt(gim[:], gim[:], den[:], ALU.mult)
                k.ts(ngim[:], gim[:], -1.0, ALU.mult)

                PSy = PS[0:5]
                UORD = [(0, 0), (1, 0), (2, 0), (3, 0), (0, 1), (1, 1), (2, 1), (3, 1)]

                def setup(ub, ui):
                    stl, d = UORD[ui]
                    u = (ub * 4 + stl) * 2 + d
                    B = UB[ui % 2]
                    ctab = ctabL[ui % 2]; stab = stabL[ui % 2]
                    k.dma(B["Bre"][:], s5_B[l, u, 0], "sync")
                    k.dma(B["Bim"][:], s5_B[l, u, 1], "sync")
                    k.dma(B["Cre"][:], s5_C[l, u, 0], "sync")
                    k.dma(B["Cim"][:], s5_C[l, u, 1], "sync")
                    k.ts(B["Btr"][:], B["Bre"][:], gre[:, u:u + 1], ALU.mult)
                    k.ts(B["Bti"][:], B["Bim"][:], gre[:, u:u + 1], ALU.mult)
                    k.stt(B["Btr"][:], B["Bim"][:], ngim[:, u:u + 1], B["Btr"][:], ALU.mult, ALU.add)
                    k.stt(B["Bti"][:], B["Bre"][:], gim[:, u:u + 1], B["Bti"][:], ALU.mult, ALU.add)
                    k.tr(PS[7][:, 0:128], B["Btr"][:], ident[:])
                    k.tr(PS[7][:, 128:256], B["Bti"][:], ident[:])
                    k.cp(B["BtR"][:], PS[7][:, 0:128], eng="scalar")
                    k.cp(B["BtI"][:], PS[7][:, 128:256], eng="scalar")
                    k.cp(B["CbR"][:], B["Cre"][:], eng="scalar")
                    k.ts(B["CbI"][:], B["Cim"][:], -1.0, ALU.mult)
                    k.ts(a96[:], t96[:], pth[:, u:u + 1], ALU.mult)
                    sincos(s96[:], c96[:], a96[:], k96, f96, (slice(None), slice(None)))
                    for pc in range(5):
                        a0 = pc * 10; na = min(10, 48 - a0)
                        eng = "gpsimd" if pc == 1 else "vector"
                        tm = ttmp[pc % 2]
                        def bc_a(src):
                            return src[:, a0:a0 + na].unsqueeze(2).to_broadcast([128, na, 48])
                        def bc_b(src):
                            return src[:, 48:96].unsqueeze(1).to_broadcast([128, na, 48])
                        cv = ctab[:, a0 * 48:(a0 + na) * 48].rearrange("p (a b) -> p a b", b=48)
                        sv = stab[:, a0 * 48:(a0 + na) * 48].rearrange("p (a b) -> p a b", b=48)
                        tv = tm[:, 0:na * 48].rearrange("p (a b) -> p a b", b=48)
                        k.tt(cv, bc_a(c96), bc_b(c96), ALU.mult, eng=eng)
                        k.tt(tv, bc_a(s96), bc_b(s96), ALU.mult, eng=eng)
                        k.tt(cv, cv, tv, ALU.subtract, eng=eng)
                        k.tt(sv, bc_a(s96), bc_b(c96), ALU.mult, eng=eng)
                        k.tt(tv, bc_a(c96), bc_b(s96), ALU.mult, eng=eng)
                        k.tt(sv, sv, tv, ALU.add, eng=eng)

                segctr = [0]

                def seg_info(ui, si):
                    stl, d = UORD[ui]
                    order = [0, 1, 2, 3, 4] if d == 0 else [0, 4, 3, 2, 1]
                    ti = order[si]
                    lo, n = TT[ti]
                    if d == 0:
                        tsl = slice(lo, lo + n); fw = slice(0, n); last = n - 1
                    else:
                        tsl = slice(255, None, -1) if ti == 0 else slice(2559 - lo, 2559 - lo - n, -1)
                        fw = slice(n - 1, None, -1); last = 0
                    return d, ti, lo, n, tsl, fw, last

                def stageA(ub, ui, si, buf):
                    d, ti, lo, n, tsl, fw, last = seg_info(ui, si)
                    B = UB[ui % 2]; ct = ctabL[ui % 2][:, tsl]; sn = stabL[ui % 2][:, tsl]
                    A, Bb, T1 = buf["A"], buf["Bb"], buf["T1"]
                    k.mm(PS[5][:, :n], B["BtR"][:], ubf[:, lo:lo + n])
                    k.mm(PS[6][:, :n], B["BtI"][:], ubf[:, lo:lo + n])
                    k.tt(A[:, :n], PS[5][:, :n], ct, ALU.mult)
                    k.tt(T1[:, :n], PS[6][:, :n], sn, ALU.mult)
                    k.tt(Bb[:, :n], PS[6][:, :n], ct, ALU.mult)
                    k.tt(A[:, :n], A[:, :n], T1[:, :n], ALU.add, eng="gpsimd")
                    k.tt(T1[:, :n], PS[5][:, :n], sn, ALU.mult)
                    k.tt(Bb[:, :n], Bb[:, :n], T1[:, :n], ALU.subtract, eng="gpsimd")

                def stageB1(ub, ui, si, buf):
                    d, ti, lo, n, tsl, fw, last = seg_info(ui, si)
                    stl = UORD[ui][0]
                    u = (ub * 4 + stl) * 2 + d
                    ct = ctabL[ui % 2][:, tsl]; sn = stabL[ui % 2][:, tsl]
                    A, Bb, G1, G2 = buf["A"], buf["Bb"], buf["G1"], buf["G2"]
                    rb = pr[:, u:u + 1].to_broadcast([128, n])
                    i1 = 0.0 if si == 0 else car[:, 0:1]
                    i2 = 0.0 if si == 0 else car[:, 1:2]
                    k.scan(G1[:, fw], rb, A[:, fw], i1)
                    k.scan(G2[:, fw], rb, Bb[:, fw], i2)
                    if si < 4:
                        k.cp(car[:, 0:1], G1[:, last:last + 1])
                        k.cp(car[:, 1:2], G2[:, last:last + 1])
                    k.tt(XT[0][:, :n], G1[:, :n], ct, ALU.mult, eng="gpsimd")
                    k.tt(XT[1][:, :n], G2[:, :n], sn, ALU.mult, eng="gpsimd")
                    k.tt(XT[2][:, :n], G1[:, :n], sn, ALU.mult, eng="gpsimd")
                    k.tt(XT[3][:, :n], G2[:, :n], ct, ALU.mult, eng="gpsimd")

                def stageB2(ub, ui, si, buf):
                    d, ti, lo, n, tsl, fw, last = seg_info(ui, si)
                    B = UB[ui % 2]
                    HR, HI = buf["HR"], buf["HI"]
                    k.tt(HR[:, :n], XT[0][:, :n], XT[1][:, :n], ALU.subtract)
                    k.tt(HI[:, :n], XT[2][:, :n], XT[3][:, :n], ALU.add)
                    k.mm(PSy[ti][:, :n], B["CbR"][:], HR[:, :n], ui == 0, False)
                    k.mm(PSy[ti][:, :n], B["CbI"][:], HI[:, :n], False, ui == 7)

                for ub in range(4):
                    load_win(wt, l, ub * 128, 128)
                    for ti, (lo, n) in enumerate(TT):
                        proj_fm(wt, 128, ti, PS[5 + ti % 2])
                        k.cp(ubf[:, lo:lo + n], PS[5 + ti % 2][:, :n], eng="scalar")
                    setup(ub, 0)
                    bufs = {}
                    bufs[(0, 0)] = SG[segctr[0] % 2]; segctr[0] += 1
                    stageA(ub, 0, 0, bufs[(0, 0)])
                    pending = None
                    for ui in range(8):
                        for si in range(5):
                            stageB1(ub, ui, si, bufs[(ui, si)]) if pending is None else None
                            if pending is not None:
                                pass
                            if si < 4:
                                nb = SG[segctr[0] % 2]; segctr[0] += 1
                                bufs[(ui, si + 1)] = nb
                                stageA(ub, ui, si + 1, nb)
                            elif ui < 7:
                                setup(ub, ui + 1)
                                nb = SG[segctr[0] % 2]; segctr[0] += 1
                                bufs[(ui + 1, 0)] = nb
                                stageA(ub, ui + 1, 0, nb)
                            stageB2(ub, ui, si, bufs[(ui, si)])
                    for ti, (lo, n) in enumerate(TT):
                        k.stt(yy[:, :n], ubf[:, lo:lo + n], dsk[:, ub:ub + 1], PSy[ti][:, :n], ALU.mult, ALU.add)
                        k.tt(y2[:, :n], yy[:, :n], yy[:, :n], ALU.mult, eng="gpsimd")
                        k.ts(y2[:, :n], y2[:, :n], 0.044715, ALU.mult, 1.0, ALU.add, eng="gpsimd")
                        k.tt(y2[:, :n], y2[:, :n], yy[:, :n], ALU.mult, eng="gpsimd")
                        k.act(y2[:, :n], y2[:, :n], AF.Sigmoid, scale=2.0 * math.sqrt(2.0 / math.pi))
                        k.tt(ygb[:, :n], yy[:, :n], y2[:, :n], ALU.mult)
                        k.dma(s5yg[ub * 128:(ub + 1) * 128, lo:lo + n], ygb[:, :n], "sync")
                k.S.barrier()
            with ExitStack() as s2:
                def T(name, shape, dt=F32):
                    return s2.enter_context(sbt("s5g" + name, list(shape), dt))
                wglu = T("wglu", [128, 4, 512], BF16)
                ygl = T("ygl", [128, 4, 512], BF16)
                yo = T("yo", [128, 512], BF16)
                yy = T("yy", [128, 512])
                k.dma(wglu[:], s5_glu[l].rearrange("(kt p) n -> p kt n", p=128), "gpsimd")
                ygv = s5yg.rearrange("(kt p) t -> p kt t", p=128)
                for ti, (lo, n) in enumerate(TT):
                    k.dma(ygl[:, :, :n], ygv[:, :, lo:lo + n], "sync")
                    for nt in range(4):
                        ps = PS[nt % 4]
                        for kt in range(4):
                            k.mm(ps[:, :n], wglu[:, kt, nt * 128:(nt + 1) * 128], ygl[:, kt, :n], kt == 0, kt == 3)
                        k.act(yy[:, :n], ps[:, :n], AF.Sigmoid)
                        k.tt(yo[:, :n], yy[:, :n], ygl[:, nt, :n], ALU.mult)
                        k.dma(ymix[nt * 128:(nt + 1) * 128, lo:lo + n], yo[:, :n], "sync")
                k.S.barrier()

        maskF = sb("maskF", [128, 128]); maskB = sb("maskB", [128, 128])
        nstrF = sb("nstrF", [128, 128]); nstrB = sb("nstrB", [128, 128])
        selF = sb("selF", [128, 128]); selB = sb("selB", [128, 128])
        k.memset(maskF[:], 1.0); k.memset(maskB[:], 1.0); k.memset(selF[:], 0.0); k.memset(selB[:], 0.0)
        k.S.op("gpsimd", lambda e: e.affine_select(out=maskF[:], in_=maskF[:], pattern=[[1, 128]], compare_op=ALU.is_ge,
                                                   fill=0.0, base=0, channel_multiplier=-1), [maskF], [maskF])
        k.S.op("gpsimd", lambda e: e.affine_select(out=maskB[:], in_=maskB[:], pattern=[[-1, 128]], compare_op=ALU.is_ge,
                                                   fill=0.0, base=0, channel_multiplier=1), [maskB], [maskB])
        k.S.op("gpsimd", lambda e: e.affine_select(out=selF[:], in_=selF[:], pattern=[[0, 128]], compare_op=ALU.not_equal,
                                                   fill=1.0, base=-127, channel_multiplier=1), [selF], [selF])
        k.S.op("gpsimd", lambda e: e.affine_select(out=selB[:], in_=selB[:], pattern=[[0, 128]], compare_op=ALU.not_equal,
                                                   fill=1.0, base=0, channel_multiplier=1), [selB], [selB])
        k.tt(nstrF[:], ident[:], maskF[:], ALU.subtract)
        k.tt(nstrB[:], ident[:], maskB[:], ALU.subtract)
        MASK = [maskF, maskB]; NSTR = [nstrF, nstrB]; SEL = [selF, selB]; ENDI = [127, 0]
        CH_ORDER = [list(range(NCH)), [1, 0] + list(range(NCH - 1, 1, -1))]

        def conv_block(raw, acc, cw, cb, ntap_bias):
            if ntap_bias:
                k.ts(acc[:], raw[:], cw[:, cb, 2:3], ALU.mult, cw[:, cb, 5:6], ALU.add)
            else:
                k.ts(acc[:], raw[:], cw[:, cb, 2:3], ALU.mult)
            for kk in (0, 1, 3, 4):
                dd = kk - 2
                for (s0, L) in ((0, NCTX), (NCTX, NT - NCTX)):
                    o0 = s0 + max(0, -dd); o1 = s0 + L - max(0, dd)
                    i0 = s0 + max(0, dd); i1 = s0 + L - max(0, -dd)
                    k.stt(acc[:, o0:o1], raw[:, i0:i1], cw[:, cb, kk:kk + 1], acc[:, o0:o1], ALU.mult, ALU.add,
                          eng=("vector" if kk < 2 else "gpsimd"))


        PADL = 2 + NCTX + 2 + 2 + (NT - NCTX) + 2

        def pad_off(lo):
            return 2 + lo if lo < NCTX else 2 + NCTX + 2 + 2 + (lo - NCTX)

        def conv_pe(rawp, dg, cw, cb, acc, bias_col):
            for kk in range(5):
                k.ts(dg[:, kk, :], identb[:], cw[:, cb, kk:kk + 1], ALU.mult)
            for ti, (lo, n) in enumerate(TT):
                ps = PS[4 + ti % 4]
                o = pad_off(lo)
                for kk in range(5):
                    k.mm(ps[:, :n], dg[:, kk, :], rawp[:, o + kk - 2:o + kk - 2 + n], kk == 0, kk == 4)
                if bias_col is not None:
                    k.act(acc[:, lo:lo + n], ps[:, :n], AF.Silu, bias=bias_col)
                else:
                    k.act(acc[:, lo:lo + n], ps[:, :n], AF.Silu)

        m2tm = dscr("m2tm", [NT, 1280])
        m2fm = dscr("m2fm", [512, NT])
        M2X = 3600

        def m2_mixer(l):
            with ExitStack() as s2:
                def T(name, shape, dt=F32):
                    return s2.enter_context(sbt("ma" + name, list(shape), dt))
                cw = T("cw", [128, 12, 6])
                wt = [T("wt%d" % i, [128, 8, 128], BF16) for i in range(2)]
                rawp = T("rawp", [128, PADL], BF16); acc = T("acc", [128, NT])
                dg = T("dg", [128, 5, 128], BF16)
                tmall = T("tmall", [128, NCH, 128])
                k.dma(cw[:], m2_cw[l], "sync")
                k.memset(rawp[:], 0.0)
                for cb in range(12):
                    w = wt[cb % 2]
                    load_win(w, l, M2X + cb * 128, 128)
                    for ti, (lo, n) in enumerate(TT):
                        ps = PS[ti % 4]
                        proj_fm(w, 128, ti, ps)
                        o = pad_off(lo)
                        k.cp(rawp[:, o:o + n], ps[:, :n], eng=("scalar" if ti % 2 else "vector"))
                    conv_pe(rawp, dg, cw, cb, acc, cw[:, cb, 5:6])
                    if cb >= 8:
                        k.dma(m2fm[(cb - 8) * 128:(cb - 7) * 128, :], acc[:], "sync")
                    if cb < 10:
                        for c in range(NCH):
                            ps = PS[c % 4]
                            k.tr(ps[:, 0:128], acc[:, c * 128:(c + 1) * 128], ident[:])
                            k.cp(tmall[:, c, :], ps[:, 0:128], eng=("vector" if c % 2 else "scalar"))
                        k.dma(m2tm[:, cb * 128:(cb + 1) * 128].rearrange("(c p) f -> p c f", p=128), tmall[:], "sync")
                k.S.barrier()
            with ExitStack() as s2:
                def T(name, shape, dt=F32):
                    return s2.enter_context(sbt("mb" + name, list(shape), dt))
                wz = T("wz", [128, 8, 1024], BF16)
                wdt = T("wdt", [128, 8, 32], BF16)
                ab = T("ab", [128, 80]); ng = T("ng", [128, 1024])
                nega = T("nega", [128, 32])
                Sst = T("S", [128, 16, 64])
                xtm = T("xtm", [128, 16, 64]); btm = T("btm", [128, 256])
                BT = T("BT", [128, 2, 128]); CT = T("CT", [128, 2, 128])
                dtv = T("dtv", [128, 16]); la = T("la", [128, 16]); cum = T("cum", [128, 16]); cend = T("cend", [128, 16])
                ecum = T("ecum", [128, 16]); edd = T("edd", [128, 16]); ecend = T("ecend", [128, 16])
                DEM = T("DEM", [128, 16, 128])
                Gm = T("Gm", [128, 2, 128])
                xdt = T("xdt", [128, 16, 64]); xdec = T("xdec", [128, 16, 64])
                yacc = T("yacc", [128, 16, 64]); yf = T("yf", [128, 16, 64])
                zs = T("zs", [128, 1024]); sq = T("sq", [128, 512]); ss = T("ss", [128, 2])
                ytb = T("ytb", [128, 8, 128], BF16)
                k.dma(ab[:], m2_ab[l], "sync")
                k.dma(ng[:], m2_ng[l], "sync")
                load_win(wz, l, 2576, 1024)
                load_win(wdt, l, 5136, 32)
                k.act(nega[:], ab[:, 0:32], AF.Exp)
                k.ts(nega[:], nega[:], -1.0, ALU.mult)
                for d in range(2):
                    mk = MASK[d]
                    k.memset(Sst[:], 0.0)
                    for c in CH_ORDER[d]:
                        r0 = c * 128
                        k.dma(xtm[:].rearrange("p h e -> p (h e)"), m2tm[r0:r0 + 128, 0:1024], "sync")
                        k.dma(btm[:], m2tm[r0:r0 + 128, 1024:1280], "sync")
                        k.dma(BT[:], m2fm[0:256, r0:r0 + 128].rearrange("(g p) t -> p g t", p=128), "sync")
                        k.dma(CT[:], m2fm[256:512, r0:r0 + 128].rearrange("(g p) t -> p g t", p=128), "sync")
                        proj_tm(wdt, 32, c, PS[6])
                        k.tt(dtv[:], PS[6][:, d * 16:(d + 1) * 16], ab[:, 32 + d * 16:48 + d * 16], ALU.add)
                        k.act(dtv[:], dtv[:], AF.Exp)
                        k.act(dtv[:], dtv[:], AF.Ln, bias=ones[:, 0:1])
                        k.tt(la[:], dtv[:], nega[:, d * 16:(d + 1) * 16], ALU.mult)
                        k.mm(PS[6][:, 64:80], mk[:], la[:])
                        k.cp(cum[:], PS[6][:, 64:80])
                        k.mm(PS[6][:, 96:112], SEL[d][:], cum[:])
                        k.cp(cend[:], PS[6][:, 96:112])
                        k.act(ecum[:], cum[:], AF.Exp)
                        k.act(ecend[:], cend[:], AF.Exp)
                        k.tt(edd[:], cend[:], cum[:], ALU.subtract)
                        k.act(edd[:], edd[:], AF.Exp)
                        k.tt(xdt[:], xtm[:], dtv[:].unsqueeze(2).to_broadcast([128, 16, 64]), ALU.mult)
                        k.tt(DEM[:], ident[:].unsqueeze(1).to_broadcast([128, 16, 128]),
                             cum[:].unsqueeze(2).to_broadcast([128, 16, 128]), ALU.mult, eng="gpsimd")
                        for q4 in range(4):
                            k.mm(PS[q4][:], ones[:], DEM[:, q4 * 4:(q4 + 1) * 4, :].rearrange("p h t -> p (h t)"))
                        for q4 in range(4):
                            k.tt(DEM[:, q4 * 4:(q4 + 1) * 4, :], PS[q4][:].rearrange("p (h t) -> p h t", t=128),
                                 cum[:, q4 * 4:(q4 + 1) * 4].unsqueeze(2).to_broadcast([128, 4, 128]), ALU.subtract)
                        k.tt(DEM[:], DEM[:], mk[:].unsqueeze(1).to_broadcast([128, 16, 128]), ALU.mult, eng="gpsimd")
                        k.act(DEM[:], DEM[:], AF.Exp)
                        for g in range(2):
                            k.mm(PS[6][:, 128 + g * 128:256 + g * 128], BT[:, g, :], CT[:, g, :])
                        k.tt(Gm[:], PS[6][:, 128:384].rearrange("p (g t) -> p g t", t=128),
                             mk[:].unsqueeze(1).to_broadcast([128, 2, 128]), ALU.mult)
                        for g in range(2):
                            k.tt(DEM[:, g * 8:(g + 1) * 8, :], DEM[:, g * 8:(g + 1) * 8, :],
                                 Gm[:, g, :].unsqueeze(1).to_broadcast([128, 8, 128]), ALU.mult,
                                 eng=("vector" if g == 0 else "gpsimd"))
                        for h in range(16):
                            ps = PS[4 + h // 8]
                            k.mm(ps[:, (h % 8) * 64:(h % 8 + 1) * 64], DEM[:, h, :], xdt[:, h, :])
                        for g in range(2):
                            k.mm(PS[g][:], CT[:, g, :], Sst[:, g * 8:(g + 1) * 8, :].rearrange("p h e -> p (h e)"))
                        for g in range(2):
                            hs = slice(g * 8, (g + 1) * 8)
                            k.tt(yacc[:, hs, :], PS[g][:].rearrange("p (h e) -> p h e", e=64),
                                 ecum[:, hs].unsqueeze(2).to_broadcast([128, 8, 64]), ALU.mult)
                            k.tt(yacc[:, hs, :], yacc[:, hs, :], PS[4 + g][:].rearrange("p (h e) -> p h e", e=64), ALU.add)
                        k.tt(xdec[:], xdt[:], edd[:].unsqueeze(2).to_broadcast([128, 16, 64]), ALU.mult, eng="gpsimd")
                        for g in range(2):
                            k.mm(PS[2 + g][:], btm[:, g * 128:(g + 1) * 128],
                                 xdec[:, g * 8:(g + 1) * 8, :].rearrange("p h e -> p (h e)"))
                        k.tt(Sst[:], Sst[:], ecend[:].unsqueeze(2).to_broadcast([128, 16, 64]), ALU.mult, eng="gpsimd")
                        for g in range(2):
                            hs = slice(g * 8, (g + 1) * 8)
                            k.tt(Sst[:, hs, :], Sst[:, hs, :], PS[2 + g][:].rearrange("p (h e) -> p h e", e=64), ALU.add)
                        if d == 0:
                            k.dma(yfwd_m[r0:r0 + 128, :], yacc[:].rearrange("p h e -> p (h e)"), "sync")
                        else:
                            k.dma(yf[:].rearrange("p h e -> p (h e)"), yfwd_m[r0:r0 + 128, :], "sync")
                            k.tt(yacc[:], yacc[:], yf[:], ALU.add, eng="gpsimd")
                            k.tt(yf[:], xtm[:], ab[:, 64:80].unsqueeze(2).to_broadcast([128, 16, 64]), ALU.mult, eng="gpsimd")
                            k.tt(yacc[:], yacc[:], yf[:], ALU.add, eng="gpsimd")
                            for hf in range(2):
                                proj_tm(wz[:, :, hf * 512:(hf + 1) * 512], 512, c, PS[7])
                                k.act(zs[:, hf * 512:(hf + 1) * 512], PS[7][:], AF.Silu)
                            yv = yacc[:].rearrange("p h e -> p (h e)")
                            k.tt(yv, yv, zs[:], ALU.mult)
                            k.memset(ss[:], 0.0)
                            for g in range(2):
                                k.act(sq[:], yv[:, g * 512:(g + 1) * 512], AF.Square, accum=ss[:, g:g + 1])
                            k.act(ss[:], ss[:], AF.Sqrt, bias=epsc[:, 0:1], scale=1.0 / 512)
                            k.recip(ss[:], ss[:])
                            for g in range(2):
                                k.stt(yv[:, g * 512:(g + 1) * 512], yv[:, g * 512:(g + 1) * 512], ss[:, g:g + 1],
                                      ng[:, g * 512:(g + 1) * 512], ALU.mult, ALU.mult)
                            for cb in range(8):
                                pst = PS[6 + cb % 2]
                                k.tr(pst[:, 0:128], yv[:, cb * 128:(cb + 1) * 128], ident[:])
                                k.cp(ytb[:, cb, :], pst[:, 0:128], eng=("scalar" if cb % 2 else "vector"))
                            k.dma(ymix[1024:2048, r0:r0 + 128].rearrange("(cb p) t -> p cb t", p=128), ytb[:], "sync")
                k.S.barrier()

        gtm = dscr("gtm", [NT, 1024])
        gfm = dscr("gfm", [1024, NT])
        GQ = 512

        GC = os.environ.get("GDNCUT", "")

        def gdn_mixer(l):
            with ExitStack() as s2:
                def T(name, shape, dt=F32):
                    return s2.enter_context(sbt("ga" + name, list(shape), dt))
                cw = T("cw", [128, 12, 5])
                wt = [T("wt%d" % i, [128, 8, 128], BF16) for i in range(2)]
                rawp = T("rawp", [128, PADL], BF16); acc = T("acc", [128, NT])
                dg = T("dg", [128, 5, 128], BF16)
                sq = T("sq", [128, 512]); rs = T("rs", [128, 512])
                tmall = T("tmall", [128, NCH, 128])
                k.dma(cw[:], gdn_cw[l], "sync")
                k.memset(rawp[:], 0.0)
                for cb in range(12):
                    w = wt[cb % 2]
                    load_win(w, l, GQ + cb * 128, 128)
                    for ti, (lo, n) in enumerate(TT):
                        ps = PS[ti % 4]
                        proj_fm(w, 128, ti, ps)
                        o = pad_off(lo)
                        k.cp(rawp[:, o:o + n], ps[:, :n], eng=("scalar" if ti % 2 else "vector"))
                    conv_pe(rawp, dg, cw, cb, acc, None)
                    if cb < 8:
                        for ti, (lo, n) in enumerate(TT):
                            ps = PS[4 + ti % 2]
                            k.tt(sq[:, :n], acc[:, lo:lo + n], acc[:, lo:lo + n], ALU.mult, eng="gpsimd")
                            k.mm(ps[:, :n], ones[:], sq[:, :n])
                            k.act(rs[:, :n], ps[:, :n], AF.Sqrt, bias=epsc[:, 0:1], scale=1.0)
                            k.recip(rs[:, :n], rs[:, :n])
                            if cb < 4:
                                k.stt(acc[:, lo:lo + n], acc[:, lo:lo + n], 128.0 ** -0.5, rs[:, :n], ALU.mult, ALU.mult)
                            else:
                                k.tt(acc[:, lo:lo + n], acc[:, lo:lo + n], rs[:, :n], ALU.mult)
                        k.dma(gfm[cb * 128:(cb + 1) * 128, :], acc[:], "sync")
                    if cb >= 4:
                        for c in range(NCH):
                            ps = PS[6 + c % 2]
                            k.tr(ps[:, 0:128], acc[:, c * 128:(c + 1) * 128], ident[:])
                            k.cp(tmall[:, c, :], ps[:, 0:128], eng=("vector" if c % 2 else "scalar"))
                        k.dma(gtm[:, (cb - 4) * 128:(cb - 3) * 128].rearrange("(c p) f -> p c f", p=128), tmall[:], "sync")
                k.S.barrier()
            if GC == "s1":
                return
            with ExitStack() as s2:
                def T(name, shape, dt=F32):
                    return s2.enter_context(sbt("gb" + name, list(shape), dt))
                wz = T("wz", [128, 8, 512], BF16)
                wab = T("wab", [128, 8, 16], BF16)
                abp = T("abp", [128, 16]); ng = T("ng", [128, 128]); nega = T("nega", [128, 8])
                Sst = T("S", [128, 4, 128])
                ggL = [T("gg%d" % i, [128, 4]) for i in range(2)]
                betaL = [T("beta%d" % i, [128, 4]) for i in range(2)]
                gcL = [T("gc%d" % i, [128, 4]) for i in range(2)]
                egcL = [T("egc%d" % i, [128, 4]) for i in range(2)]
                zsL = [T("zs%d" % i, [128, 512]) for i in range(2)]
                HB = []
                for h in range(4):
                    b = {}
                    for nm in ("knT", "qnT", "ktm", "vtm", "EdT", "AT", "P", "PT", "wT", "vn", "qdT", "eRg", "kdec",
                               "oo", "of", "sq"):
                        b[nm] = T("%s%d" % (nm, h), [128, 128])
                    b["D2"] = T("D2%d" % h, [128, 256]); b["R"] = T("R%d" % h, [128, 256])
                    b["sc1"] = T("sc1%d" % h, [128, 8]); b["otb"] = T("otb%d" % h, [128, 128], BF16)
                    HB.append(b)
                k.dma(abp[:], gdn_ab[l], "sync")
                k.dma(ng[:], gdn_ng[l], "sync")
                load_win(wz, l, 2048, 512)
                load_win(wab, l, 2560, 16)
                k.act(nega[:], abp[:, 0:8], AF.Exp)
                k.ts(nega[:], nega[:], -1.0, ALU.mult)

                def prep(d, c, par):
                    gg, beta, gc, egc, zs = ggL[par], betaL[par], gcL[par], egcL[par], zsL[par]
                    proj_tm(wab, 16, c, PS[7])
                    k.tt(gg[:], PS[7][:, d * 4:(d + 1) * 4], abp[:, 8 + d * 4:12 + d * 4], ALU.add)
                    k.act(gg[:], gg[:], AF.Exp)
                    k.act(gg[:], gg[:], AF.Ln, bias=ones[:, 0:1])
                    k.tt(gg[:], gg[:], nega[:, d * 4:(d + 1) * 4], ALU.mult)
                    k.act(beta[:], PS[7][:, 8 + d * 4:12 + d * 4], AF.Sigmoid)
                    k.mm(PS[7][:, 32:36], MASK[d][:], gg[:])
                    k.cp(gc[:], PS[7][:, 32:36])
                    k.act(egc[:], gc[:], AF.Exp)
                    if d == 1:
                        proj_tm(wz, 512, c, PS[6])
                        k.act(zs[:], PS[6][:], AF.Silu)

                def unit(d, c, par, h):
                    beta, gc, egc, zs = betaL[par], gcL[par], egcL[par], zsL[par]
                    mk = MASK[d]; e_i = ENDI[d]
                    r0 = c * 128
                    B = HB[h]
                    knT, qnT, ktm, vtm, D2, EdT, AT = B["knT"], B["qnT"], B["ktm"], B["vtm"], B["D2"], B["EdT"], B["AT"]
                    P, PT, R, wT, vn, qdT, eRg, kdec = B["P"], B["PT"], B["R"], B["wT"], B["vn"], B["qdT"], B["eRg"], B["kdec"]
                    sc1, oo, of, sq, otb = B["sc1"], B["oo"], B["of"], B["sq"], B["otb"]
                    pA = PS[h]; pB = PS[4 + h]
                    k.dma(qnT[:], gfm[h * 128:(h + 1) * 128, r0:r0 + 128], "sync")
                    k.dma(knT[:], gfm[512 + h * 128:640 + h * 128, r0:r0 + 128], "sync")
                    k.dma(ktm[:], gtm[r0:r0 + 128, h * 128:(h + 1) * 128], "scalar")
                    k.dma(vtm[:], gtm[r0:r0 + 128, 512 + h * 128:640 + h * 128], "scalar")
                    yield
                    k.ts(D2[:, 0:128], ident[:], gc[:, h:h + 1], ALU.mult)
                    k.ts(D2[:, 128:256], ident[:], beta[:, h:h + 1], ALU.mult)
                    yield
                    k.mm(pA[:, 0:256], ones[:], D2[:])
                    Rg = pA[:, 0:128]; Rb = pA[:, 128:256]
                    k.mm(pB[:, 0:128], knT[:], knT[:])
                    k.mm(pB[:, 128:256], knT[:], qnT[:])
                    yield
                    k.ts(EdT[:], Rg, gc[:, h:h + 1], ALU.subtract)
                    yield
                    k.tt(EdT[:], EdT[:], mk[:], ALU.mult)
                    yield
                    k.act(EdT[:], EdT[:], AF.Exp)
                    k.cp(sc1[:, 4:5], pA[:, e_i:e_i + 1])
                    yield
                    k.tt(EdT[:], EdT[:], mk[:], ALU.mult)
                    yield
                    k.tt(AT[:], pB[:, 128:256], EdT[:], ALU.mult)
                    k.tt(PT[:], pB[:, 0:128], EdT[:], ALU.mult)
                    yield
                    k.tt(PT[:], PT[:], Rb, ALU.mult)
                    k.act(eRg[:], Rg, AF.Exp)
                    yield
                    k.tt(PT[:], PT[:], NSTR[d][:], ALU.mult)
                    k.ts(R[:, 0:128], vtm[:], beta[:, h:h + 1], ALU.mult)
                    k.ts(R[:, 128:256], ktm[:], beta[:, h:h + 1], ALU.mult, egc[:, h:h + 1], ALU.mult)
                    yield
                    k.tr(pB[:, 256:384], PT[:], ident[:])
                    yield
                    k.cp(P[:], pB[:, 256:384])
                    k.tt(qdT[:], qnT[:], eRg[:], ALU.mult)
                    yield
                    for lev in range(7):
                        k.mm(pA[:, 256:512], PT[:], R[:])
                        if lev < 6:
                            k.mm(pB[:, 0:128], PT[:], P[:])
                            k.mm(pB[:, 128:256], P[:], PT[:])
                        yield
                        k.tt(R[:], R[:], pA[:, 256:512], ALU.add)
                        if lev < 6:
                            k.cp(P[:], pB[:, 0:128])
                            k.cp(PT[:], pB[:, 128:256])
                        yield
                    k.tr(pB[:, 256:384], R[:, 128:256], ident[:])
                    k.ts(sc1[:, 0:1], gc[:, h:h + 1], -1.0, ALU.mult, sc1[:, 4:5], ALU.add)
                    yield
                    k.cp(wT[:], pB[:, 256:384])
                    k.act(sc1[:, 1:2], sc1[:, 0:1], AF.Exp)
                    k.act(sc1[:, 2:3], sc1[:, 4:5], AF.Exp)
                    yield
                    k.mm(pB[:, 384:512], wT[:], Sst[:, h, :])
                    k.ts(kdec[:], ktm[:], sc1[:, 1:2], ALU.mult)
                    yield
                    k.tt(vn[:], R[:, 0:128], pB[:, 384:512], ALU.subtract)
                    yield
                    k.mm(pB[:, 384:512], qdT[:], Sst[:, h, :], True, False)
                    k.mm(pB[:, 384:512], AT[:], vn[:], False, True)
                    k.mm(pB[:, 256:384], kdec[:], vn[:])
                    yield
                    if d == 0:
                        k.cp(oo[:], pB[:, 384:512])
                    else:
                        k.dma(of[:], yfwd_g[h, r0:r0 + 128, :], "sync")
                        k.tt(oo[:], of[:], pB[:, 384:512], ALU.add)
                    k.stt(Sst[:, h, :], Sst[:, h, :], sc1[:, 2:3], pB[:, 256:384], ALU.mult, ALU.add)
                    yield
                    if d == 0:
                        k.dma(yfwd_g[h, r0:r0 + 128, :], oo[:], "sync")
                    else:
                        k.memset(sc1[:, 3:4], 0.0)
                        yield
                        k.act(sq[:], oo[:], AF.Square, accum=sc1[:, 3:4])
                        yield
                        k.act(sc1[:, 3:4], sc1[:, 3:4], AF.Sqrt, bias=epsc[:, 0:1], scale=1.0 / 128)
                        yield
                        k.recip(sc1[:, 3:4], sc1[:, 3:4])
                        yield
                        k.stt(oo[:], oo[:], sc1[:, 3:4], ng[:], ALU.mult, ALU.mult)
                        yield
                        k.tt(oo[:], oo[:], zs[:, h * 128:(h + 1) * 128], ALU.mult)
                        yield
                        k.tr(pB[:, 256:384], oo[:], ident[:])
                        yield
                        k.cp(otb[:], pB[:, 256:384], eng="scalar")
                        yield
                        k.dma(ymix[512 + h * 128:640 + h * 128, r0:r0 + 128], otb[:], "sync")

                for d in range(2):
                    k.memset(Sst[:], 0.0)
                    order = CH_ORDER[d]
                    prep(d, order[0], 0)
                    for ci, c in enumerate(order):
                        par = ci % 2
                        gens = [unit(d, c, par, h) for h in range(4)]
                        first = True
                        while gens:
                            nxt = []
                            for g in gens:
                                try:
                                    next(g)
                                    nxt.append(g)
                                except StopIteration:
                                    pass
                            gens = nxt
                            if first and ci + 1 < len(order):
                                prep(d, order[ci + 1], 1 - par)
                                first = False
                k.S.barrier()

        MIX = {"s5": s5_mixer, "gdn": gdn_mixer, "m2": m2_mixer}

        DBG = os.environ.get("DBGDUMP", "").split(",")

        def dbg_dump(name, ap, shape, dt=F32):
            if name not in DBG:
                return
            o = nc.dram_tensor("dbg_" + name, list(shape), dt, kind="ExternalOutput").ap()
            k.dma(o, ap, "sync")

        def dump_x():
            xo = dbgx.rearrange("(kt p) t -> p kt t", p=128)
            for kt in range(8):
                k.dma(xo[:, kt, :], xT[:, kt, :], "sync")

        skip = os.environ.get("SKIPMIX", "").split(",")
        for l in range(nlayers):
            odd = (l % 2 == 1)
            lastl = (l == nlayers - 1)
            adaln(l)
            rmsnorm_mod(l, n1g[l], 0, 1, odd)
            dbg_dump("h%d" % l, hT[:], [128, 8, NT], BF16)
            dbg_dump("mod%d" % l, mod[:], [128, 2, 48])
            dbg_dump("gs%d" % l, gs[:], [128, 2, 8])
            if "s5" not in skip:
                MIX["s5"](l)
            if "gdn" not in skip:
                MIX["gdn"](l)
            if "m2" not in skip:
                MIX["m2"](l)
            if lastl and stop == "mix":
                break
            out_proj(l, odd)
            if lastl and stop == "oproj":
                dump_x()
                break
            rmsnorm_mod(l, n2g[l], 3, 4, False)
            if l % 2 == 0:
                ffn_dense(l // 2)
            else:
                ffn_moe(l // 2)
            if lastl and stop == "ffn":
                dump_x()
                break
        if stop is None:
            rmsnorm_mod(0, fng, 0, 0, False, final=True)
        k.S.emit()
    return nc


_NC_CACHE = {}


def _prep_shared(inp):
    f = lambda a: np.ascontiguousarray(np.asarray(a, dtype=np.float32))
    g = {}
    g["ada_w"] = f(inp["ada_w"])
    g["ada_b"] = f(np.asarray(inp["ada_b"]).reshape(DEPTH, 48, 128).transpose(0, 2, 1))
    g["n1g"] = f(np.asarray(inp["norm1_g"]).reshape(DEPTH, 8, 128).transpose(0, 2, 1))
    g["n2g"] = f(np.asarray(inp["norm2_g"]).reshape(DEPTH, 8, 128).transpose(0, 2, 1))
    g["fng"] = f(np.asarray(inp["final_norm_g"]).reshape(8, 128).T)
    g["w_in"] = f(inp["w_in"])
    g["w_out"] = f(inp["w_out"])
    g["ffn_wg"] = f(inp["ffn_w_gate"]); g["ffn_wu"] = f(inp["ffn_w_up"]); g["ffn_wd"] = f(inp["ffn_w_down"])
    g["moe_r"] = f(inp["moe_router"])
    g["moe_wg"] = f(inp["moe_w_gate"]); g["moe_wu"] = f(inp["moe_w_up"]); g["moe_wd"] = f(inp["moe_w_down"])
    lam_re = np.asarray(inp["s5_lam_re"]); lam_im = np.asarray(inp["s5_lam_im"]); log_dt = np.asarray(inp["s5_log_dt"])
    b_re = np.asarray(inp["s5_b_re"]); b_im = np.asarray(inp["s5_b_im"])
    c_re = np.asarray(inp["s5_c_re"]); c_im = np.asarray(inp["s5_c_im"])
    s5_lam = np.zeros((DEPTH, 128, 3, 32), np.float32)
    s5_B = np.zeros((DEPTH, 32, 2, 128, 128), np.float32)
    s5_C = np.zeros((DEPTH, 32, 2, 128, 128), np.float32)
    for st in range(16):
        for d in range(2):
            u = st * 2 + d
            for g2 in range(2):
                gi = 2 * st + g2
                ps = slice(g2 * 64, (g2 + 1) * 64)
                s5_lam[:, ps, 0, u] = lam_re[:, d, gi, :]
                s5_lam[:, ps, 1, u] = lam_im[:, d, gi, :]
                s5_lam[:, ps, 2, u] = log_dt[:, d, gi][:, None]
                ch0 = 16 * (2 * (st % 4) + g2)
                s5_B[:, u, 0, ps, ch0:ch0 + 16] = b_re[:, d, gi]
                s5_B[:, u, 1, ps, ch0:ch0 + 16] = b_im[:, d, gi]
                s5_C[:, u, 0, ps, ch0:ch0 + 16] = c_re[:, d, gi].transpose(0, 2, 1)
                s5_C[:, u, 1, ps, ch0:ch0 + 16] = c_im[:, d, gi].transpose(0, 2, 1)
    g["s5_lam"] = s5_lam; g["s5_B"] = s5_B; g["s5_C"] = s5_C
    g["s5_d"] = f(np.asarray(inp["s5_d"]).reshape(DEPTH, 4, 128).transpose(0, 2, 1))
    g["s5_glu"] = f(inp["s5_w_glu"])
    g["gdn_cw"] = f(np.asarray(inp["gdn_conv_w"]).reshape(DEPTH, 5, 12, 128).transpose(0, 3, 2, 1))
    ab = np.concatenate([np.asarray(inp["gdn_a_log"]).reshape(DEPTH, 8), np.asarray(inp["gdn_dt_bias"]).reshape(DEPTH, 8)], 1)
    g["gdn_ab"] = f(np.broadcast_to(ab[:, None, :], (DEPTH, 128, 16)))
    g["gdn_ng"] = f(np.broadcast_to(np.asarray(inp["gdn_norm_g"])[:, None, :], (DEPTH, 128, 128)))
    cw = np.asarray(inp["m2_conv_w"]).reshape(DEPTH, 5, 12, 128).transpose(0, 3, 2, 1)
    cb = np.asarray(inp["m2_conv_b"]).reshape(DEPTH, 12, 128).transpose(0, 2, 1)[..., None]
    g["m2_cw"] = f(np.concatenate([cw, cb], axis=3))
    ab = np.concatenate([np.asarray(inp["m2_a_log"]).reshape(DEPTH, 32), np.asarray(inp["m2_dt_bias"]).reshape(DEPTH, 32),
                         np.asarray(inp["m2_d"]).reshape(DEPTH, 16)], 1)
    g["m2_ab"] = f(np.broadcast_to(ab[:, None, :], (DEPTH, 128, 80)))
    g["m2_ng"] = f(np.broadcast_to(np.asarray(inp["m2_norm_g"])[:, None, :], (DEPTH, 128, 1024)))
    return g


def kernel(**inp):
    n = 8
    x = np.asarray(inp["x"], dtype=np.float32)
    ctx = np.asarray(inp["ctx"], dtype=np.float32)
    c = np.asarray(inp["c"], dtype=np.float32)
    c_ctx = np.asarray(inp["c_ctx"], dtype=np.float32)
    shared = _prep_shared(inp)
    if "nc" not in _NC_CACHE:
        _NC_CACHE["nc"] = build_program()
    nc = _NC_CACHE["nc"]
    in_maps = []
    for b in range(n):
        m = dict(shared)
        m["xT"] = np.ascontiguousarray(np.concatenate([ctx[b], x[b]], axis=0).T)
        cs = np.stack([c[b].reshape(8, 128).T, c_ctx.reshape(8, 128).T], axis=2)
        m["cs"] = np.ascontiguousarray(cs.astype(np.float32))
        in_maps.append(m)
    res = run_bass_kernel_spmd(nc, in_maps, core_ids=list(range(n)))
    out = np.stack([np.asarray(r["outT"], dtype=np.float32).T for r in res.results], axis=0)
    return np.ascontiguousarray(out)
```

```python
import math
import os
from contextlib import ExitStack
import numpy as np
import concourse.bass as bass
import concourse.mybir as mybir
from concourse.bass_utils import run_bass_kernel_spmd

F32 = mybir.dt.float32
BF16 = mybir.dt.bfloat16
I32 = mybir.dt.int32
AF = mybir.ActivationFunctionType
ALU = mybir.AluOpType
AX = mybir.AxisListType

COMPUTE = ["tensor", "vector", "scalar", "gpsimd"]
ENGS = ["sync", "tensor", "vector", "scalar", "gpsimd"]
DMA_RING = 6
SAME_ENGINE_SYNC = True

D = 1024
NT = 2304
NCTX = 256
DEPTH = 4
DIN = 5168
DFF = 2816
NE = 8
TT = [(0, 256), (256, 512), (768, 512), (1280, 512), (1792, 512)]
NCH = 18
EPS = 1e-6
TWO_PI = 2.0 * math.pi


def _key(k):
    if isinstance(k, (str, tuple)):
        return k
    t = getattr(k, "tensor", k)
    return t.name


class Sched:
    def __init__(self, nc):
        self.nc = nc
        self.ops = {e: [] for e in ENGS}
        self.cnt = {e: 0 for e in COMPUTE}
        self.seen = {e: {} for e in ENGS}
        self.last_w = {}
        self.readers = {}
        self.ring_tot = {}
        self.ring_pos = {e: 0 for e in ENGS}
        self.ring_know = {}
        self.sem_names = ["c_" + e for e in COMPUTE]
        for e in ("sync", "scalar", "gpsimd"):
            for i in range(DMA_RING):
                n = "d_%s_%d" % (e, i)
                self.sem_names.append(n)
                self.ring_tot[n] = 0
        self.sems = {}
        self.n_ops = 0

    def _need(self, eng, tok, waits, is_dma=False):
        s, v, know = tok
        if self.seen[eng].get(s, 0) >= v:
            return
        if (not is_dma) and s == "c_" + eng and (eng == "tensor" or not SAME_ENGINE_SYNC):
            return
        waits[s] = max(waits.get(s, 0), v)
        sn = self.seen[eng]
        for ks, kv in know.items():
            if sn.get(ks, 0) < kv:
                sn[ks] = kv
        sn[s] = max(sn.get(s, 0), v)

    def _deps(self, eng, reads, writes, waits, is_dma=False):
        for k in reads:
            k = _key(k)
            t = self.last_w.get(k)
            if t is not None:
                self._need(eng, t, waits, is_dma)
            if isinstance(k, str) and k.startswith("ps"):
                for t in list(self.readers.get(k, {}).values()):
                    if t[0] != "c_" + eng:
                        self._need(eng, t, waits, is_dma)
        for k in writes:
            k = _key(k)
            t = self.last_w.get(k)
            if t is not None:
                self._need(eng, t, waits, is_dma)
            for t in list(self.readers.get(k, {}).values()):
                self._need(eng, t, waits, is_dma)

    def _publish(self, tok, reads, writes):
        for k in reads:
            self.readers.setdefault(_key(k), {})[tok[0]] = tok
        for k in writes:
            k = _key(k)
            self.last_w[k] = tok
            self.readers[k] = {}

    def op(self, eng, fn, reads=(), writes=()):
        waits = {}
        self._deps(eng, reads, writes, waits)
        self.cnt[eng] += 1
        s = "c_" + eng
        know = dict(self.seen[eng])
        know[s] = self.cnt[eng]
        tok = (s, self.cnt[eng], know)
        self.ops[eng].append((waits, fn, s, 1))
        self._publish(tok, reads, writes)
        self.n_ops += 1
        return tok

    def dma(self, eng, fn, reads=(), writes=()):
        waits = {}
        self._deps(eng, reads, writes, waits, True)
        i = self.ring_pos[eng]
        self.ring_pos[eng] = (i + 1) % DMA_RING
        s = "d_%s_%d" % (eng, i)
        if self.ring_tot[s] > 0 and self.seen[eng].get(s, 0) < self.ring_tot[s]:
            waits[s] = self.ring_tot[s]
            self.seen[eng][s] = self.ring_tot[s]
            for ks, kv in self.ring_know.get(s, {}).items():
                if self.seen[eng].get(ks, 0) < kv:
                    self.seen[eng][ks] = kv
        self.ring_tot[s] += 16
        know = dict(self.seen[eng])
        self.ring_know[s] = know
        tok = (s, self.ring_tot[s], know)
        self.ops[eng].append((waits, fn, s, 16))
        self._publish(tok, reads, writes)
        self.n_ops += 1
        return tok

    def barrier(self):
        toks = []
        for e in COMPUTE:
            if self.cnt[e] > 0:
                toks.append(("c_" + e, self.cnt[e], {}))
        for s, v in self.ring_tot.items():
            if v > 0:
                toks.append((s, v, {}))
        for e in ENGS:
            waits = {}
            for t in toks:
                self._need(e, t, waits)
            if waits:
                self.ops[e].append((waits, None, None, 0))

    def emit(self):
        nc = self.nc
        self.barrier()
        with ExitStack() as st:
            for n in self.sem_names:
                self.sems[n] = st.enter_context(nc.semaphore(n))
            block = st.enter_context(nc.Block())
            sems = self.sems

            def run(eng_name):
                def body(eng):
                    for waits, fn, s, inc in self.ops[eng_name]:
                        for ws, wv in waits.items():
                            eng.wait_ge(sems[ws], wv)
                        if fn is not None:
                            ins = fn(eng)
                            ins.then_inc(sems[s], inc)
                return body

            block.sync(run("sync"))
            block.tensor(run("tensor"))
            block.vector(run("vector"))
            block.scalar(run("scalar"))
            block.gpsimd(run("gpsimd"))


def _aps(*xs):
    return [x for x in xs if x is not None and not isinstance(x, (int, float))]


class K:
    def __init__(self, nc):
        self.nc = nc
        self.S = Sched(nc)
        self.rr = 0

    def mm(self, ps, lhsT, rhs, start=True, stop=True):
        rd = [lhsT, rhs] + ([] if start else [ps])
        return self.S.op("tensor", lambda e: e.matmul(ps, lhsT=lhsT, rhs=rhs, start=start, stop=stop), rd, [ps])

    def tr(self, ps, in_, ident):
        return self.S.op("tensor", lambda e: e.transpose(ps, in_, ident), [in_, ident], [ps])

    def act(self, out, in_, func, bias=None, scale=1.0, accum=None, eng="scalar"):
        kw = {}
        if bias is not None:
            kw["bias"] = bias
        if accum is not None:
            kw["accum_out"] = accum
        return self.S.op("scalar", lambda e: e.activation(out=out, in_=in_, func=func, scale=scale, **kw),
                         _aps(in_, bias, scale), _aps(out, accum))

    def tt(self, out, a, b, op, eng="vector"):
        return self.S.op(eng, lambda e: e.tensor_tensor(out=out, in0=a, in1=b, op=op), [a, b], [out])

    def ts(self, out, a, s1, op0, s2=None, op1=None, eng="vector", accum=None):
        def f(e):
            kw = {}
            if op1 is not None:
                kw["op1"] = op1
            if accum is not None:
                kw["accum_out"] = accum
            return e.tensor_scalar(out=out, in0=a, scalar1=s1, scalar2=s2, op0=op0, **kw)
        return self.S.op(eng, f, _aps(a, s1, s2), _aps(out, accum))

    def stt(self, out, a, s, b, op0, op1, eng="vector"):
        eng = "vector"
        return self.S.op(eng, lambda e: e.scalar_tensor_tensor(out=out, in0=a, scalar=s, in1=b, op0=op0, op1=op1),
                         _aps(a, s, b), [out])

    def cp(self, out, in_, eng="vector"):
        if eng == "scalar":
            return self.S.op(eng, lambda e: e.copy(out=out, in_=in_), [in_], [out])
        return self.S.op(eng, lambda e: e.tensor_copy(out=out, in_=in_), [in_], [out])

    def memset(self, out, v, eng="gpsimd"):
        return self.S.op(eng, lambda e: e.memset(out, v), [], [out])

    def red(self, out, in_, op, eng="vector"):
        return self.S.op(eng, lambda e: e.tensor_reduce(out=out, in_=in_, axis=AX.X, op=op), [in_], [out])

    def recip(self, out, in_):
        return self.S.op("vector", lambda e: e.reciprocal(out=out, in_=in_), [in_], [out])

    def scan(self, out, d0, d1, init):
        return self.S.op("vector", lambda e: e.tensor_tensor_scan(out=out, data0=d0, data1=d1, initial=init,
                                                                  op0=ALU.mult, op1=ALU.add),
                         _aps(d0, d1, init), [out])

    def dma(self, out, in_, q="sync"):
        return self.S.dma(q, lambda e: e.dma_start(out=out, in_=in_), [in_], [out])

    def ev(self):
        self.rr ^= 1
        return "vector" if self.rr else "gpsimd"


def perm_ap(t3, kt, lo, n):
    c0 = (lo - NCTX) // 32
    ncol = n // 32
    base = t3[:, kt, NCTX:NT]
    v = base.rearrange("p (r c) -> p c r", c=64)
    return v[:, c0:c0 + ncol, :]


def build_program(nlayers=DEPTH, stop=None):
    nc = bass.Bass("TRN2", target_bir_lowering=False)
    k = K(nc)
    _cnt = [0]

    def sbt(name, shape, dt):
        _cnt[0] += 1
        return nc.sbuf_tensor("%s_%d" % (name, _cnt[0]), shape, dt)

    def din(name, shape, dt=F32):
        return nc.dram_tensor(name, list(shape), dt, kind="ExternalInput").ap()

    def dscr(name, shape, dt=F32):
        return nc.dram_tensor(name, list(shape), dt, kind="Internal").ap()

    xT_in = din("xT", [D, NT])
    cs_in = din("cs", [128, 8, 2])
    ada_w = din("ada_w", [DEPTH, D, 6 * D])
    ada_b = din("ada_b", [DEPTH, 128, 48])
    n1g = din("n1g", [DEPTH, 128, 8])
    n2g = din("n2g", [DEPTH, 128, 8])
    fng = din("fng", [128, 8])
    w_in = din("w_in", [DEPTH, D, DIN])
    w_out = din("w_out", [DEPTH, 2048, D])
    ffn_wg = din("ffn_wg", [2, D, DFF])
    ffn_wu = din("ffn_wu", [2, D, DFF])
    ffn_wd = din("ffn_wd", [2, DFF, D])
    moe_r = din("moe_r", [2, D, NE])
    moe_wg = din("moe_wg", [2, NE, D, DFF])
    moe_wu = din("moe_wu", [2, NE, D, DFF])
    moe_wd = din("moe_wd", [2, NE, DFF, D])
    s5_lam = din("s5_lam", [DEPTH, 128, 3, 32])
    s5_B = din("s5_B", [DEPTH, 32, 2, 128, 128])
    s5_C = din("s5_C", [DEPTH, 32, 2, 128, 128])
    s5_d = din("s5_d", [DEPTH, 128, 4])
    s5_glu = din("s5_glu", [DEPTH, 512, 512])
    gdn_cw = din("gdn_cw", [DEPTH, 128, 12, 5])
    gdn_ab = din("gdn_ab", [DEPTH, 128, 16])
    gdn_ng = din("gdn_ng", [DEPTH, 128, 128])
    m2_cw = din("m2_cw", [DEPTH, 128, 12, 6])
    m2_ab = din("m2_ab", [DEPTH, 128, 80])
    m2_ng = din("m2_ng", [DEPTH, 128, 1024])
    out_T = nc.dram_tensor("outT", [D, 2048], F32, kind="ExternalOutput").ap()

    if stop == "mix":
        ymix = nc.dram_tensor("ymix", [2048, NT], BF16, kind="ExternalOutput").ap()
    else:
        ymix = dscr("ymix", [2048, NT], BF16)
    dbgx = nc.dram_tensor("dbgx", [D, NT], F32, kind="ExternalOutput").ap() if stop in ("oproj", "ffn", "norm1") else None
    yfwd_g = dscr("yfwd_g", [4, NT, 128])
    yfwd_m = dscr("yfwd_m", [NT, 1024])

    with ExitStack() as st:
        def sb(name, shape, dt=F32):
            return st.enter_context(sbt(name, list(shape), dt))

        def psum(name, shape=(128, 512), dt=F32):
            return st.enter_context(nc.psum_tensor(name, list(shape), dt))

        xT = sb("xTs", [128, 8, NT])
        hT = sb("hTs", [128, 8, NT], BF16)
        mod = sb("mod", [128, 2, 48])
        gs = sb("gs", [128, 2, 8])
        ident = sb("ident", [128, 128])
        identb = sb("identb", [128, 128], BF16)
        ones = sb("ones", [128, 128])
        epsc = sb("epsc", [128, 1])
        PS = [psum("ps%d" % i) for i in range(8)]

        DBG = os.environ.get("DBGDUMP", "").split(",")

        def dbg_dump(name, ap, shape, dt=F32):
            if name not in DBG:
                return
            o = nc.dram_tensor("dbg_" + name, list(shape), dt, kind="ExternalOutput").ap()
            k.dma(o, ap, "sync")

        k.memset(ident[:], 0.0)
        k.S.op("gpsimd", lambda e: e.affine_select(out=ident[:], in_=ident[:], pattern=[[-1, 128]],
                                                   compare_op=ALU.not_equal, fill=1.0, base=0,
                                                   channel_multiplier=1), [ident], [ident])
        k.cp(identb[:], ident[:])
        k.memset(ones[:], 1.0)
        k.memset(epsc[:], EPS)

        xv = xT_in.rearrange("(kt p) t -> p kt t", p=128)
        for kt in range(8):
            k.dma(xT[:, kt, :], xv[:, kt, :], "sync")

        csr = sb("csr", [128, 8, 2])
        k.dma(csr[:], cs_in, "sync")
        cs = sb("css", [128, 8, 2])
        k.act(cs[:], csr[:], AF.Silu)

        def adaln(l):
            with ExitStack() as s2:
                wb = [s2.enter_context(sbt("adaw%d" % i, [128, 8, 512], F32)) for i in range(2)]
                bb = s2.enter_context(sbt("adab", [128, 48], F32))
                k.dma(bb[:], ada_b[l], "sync")
                wv = ada_w[l].rearrange("(kt p) n -> p kt n", p=128)
                for blk in range(12):
                    w = wb[blk % 2]
                    k.dma(w[:], wv[:, :, blk * 512:(blk + 1) * 512], "sync" if blk % 2 == 0 else "scalar")
                    for j in range(4):
                        col = blk * 4 + j
                        ps = PS[col % 2]
                        for kt in range(8):
                            k.mm(ps[:, 0:2], w[:, kt, j * 128:(j + 1) * 128], cs[:, kt, :], kt == 0, kt == 7)
                        for jj in range(2):
                            k.ts(mod[:, jj, col:col + 1], ps[:, jj:jj + 1], bb[:, col:col + 1], ALU.add)
                k.S.barrier()

        def rmsnorm_mod(l, gsrc, shift_idx, scale_idx, permute, final=False):
            with ExitStack() as s2:
                g = s2.enter_context(sbt("ng", [128, 8], F32))
                sq = [s2.enter_context(sbt("sq%d" % i, [128, 512], F32)) for i in range(2)]
                rstd = s2.enter_context(sbt("rstd", [128, 512], F32))
                tmp = [s2.enter_context(sbt("nt%d" % i, [128, 512], F32)) for i in range(2)]
                k.dma(g[:], gsrc, "sync")
                if not final:
                    for j in range(2):
                        k.ts(gs[:, j, :], mod[:, j, scale_idx * 8:(scale_idx + 1) * 8], 1.0, ALU.add)
                        k.tt(gs[:, j, :], gs[:, j, :], g[:], ALU.mult)
                for ti, (lo, n) in enumerate(TT):
                    if final and ti == 0:
                        continue
                    ps = PS[2 + ti % 2]
                    for kt in range(8):
                        s = sq[kt % 2]
                        k.act(s[:, :n], xT[:, kt, lo:lo + n], AF.Square)
                        k.mm(ps[:, :n], ones[:], s[:, :n], kt == 0, kt == 7)
                    k.act(rstd[:, :n], ps[:, :n], AF.Sqrt, bias=epsc[:, 0:1], scale=1.0 / D)
                    k.recip(rstd[:, :n], rstd[:, :n])
                    j = 1 if ti == 0 else 0
                    for kt in range(8):
                        t = tmp[kt % 2]
                        k.tt(t[:, :n], xT[:, kt, lo:lo + n], rstd[:, :n], ALU.mult)
                        if final:
                            k.ts(t[:, :n], t[:, :n], g[:, kt:kt + 1], ALU.mult)
                            k.dma(out_T[kt * 128:(kt + 1) * 128, lo - NCTX:lo - NCTX + n], t[:, :n], "sync")
                        else:
                            if permute and ti > 0:
                                r0 = (lo - NCTX) // 64
                                dst = hT[:, kt, NCTX:NT].rearrange("p (c r) -> p r c", r=32)[:, r0:r0 + n // 64, :]
                                src = t[:, :n].rearrange("p (r c) -> p r c", c=64)
                            else:
                                dst = hT[:, kt, lo:lo + n]
                                src = t[:, :n]
                            k.ts(dst, src, gs[:, j, kt:kt + 1], ALU.mult,
                                 mod[:, j, shift_idx * 8 + kt:shift_idx * 8 + kt + 1], ALU.add)
                k.S.barrier()

        def out_proj(l, permute):
            with ExitStack() as s2:
                wo = s2.enter_context(sbt("wo", [128, 16, D], BF16))
                yb = [s2.enter_context(sbt("yb%d" % i, [128, 16, 512], BF16)) for i in range(2)]
                wv = w_out[l].rearrange("(kt p) n -> p kt n", p=128)
                for q4 in range(4):
                    k.dma(wo[:, q4 * 4:(q4 + 1) * 4, :], wv[:, q4 * 4:(q4 + 1) * 4, :], "gpsimd")
                yv = ymix.rearrange("(kt p) t -> p kt t", p=128)
                for ti, (lo, n) in enumerate(TT):
                    y = yb[ti % 2]
                    k.dma(y[:, :, :n], yv[:, :, lo:lo + n], "sync")
                    j = 1 if ti == 0 else 0
                    for nt in range(8):
                        ps = PS[nt % 4]
                        for kt in range(16):
                            k.mm(ps[:, :n], wo[:, kt, nt * 128:(nt + 1) * 128], y[:, kt, :n], kt == 0, kt == 15)
                        if permute and ti > 0:
                            dst = perm_ap(xT, nt, lo, n)
                            src = ps[:, :n].rearrange("p (c r) -> p c r", r=32)
                        else:
                            dst = xT[:, nt, lo:lo + n]
                            src = ps[:, :n]
                        k.stt(dst, src, mod[:, j, 16 + nt:17 + nt], dst, ALU.mult, ALU.add)
                k.S.barrier()

        def ffn_expert(wg, wu, wd, gbc, bufs):
            wgb, wub, wdb, hid, sgs = bufs
            wgv = wg.rearrange("(kt p) f -> p kt f", p=128)
            wuv = wu.rearrange("(kt p) f -> p kt f", p=128)
            wdv = wd.rearrange("(ft p) n -> p ft n", p=128)
            jobs = [(fb, ti) for fb in range(11) for ti in range(5)]

            def stage_a(j):
                fb, ti = jobs[j]
                b = fb % 2
                if ti == 0:
                    k.dma(wgb[b][:], wgv[:, :, fb * 256:(fb + 1) * 256], "gpsimd")
                    k.dma(wub[b][:], wuv[:, :, fb * 256:(fb + 1) * 256], "gpsimd")
                    k.dma(wdb[b][:], wdv[:, fb * 2:(fb + 1) * 2, :], "gpsimd")
                lo, n = TT[ti]
                hb = hid[j % 2]
                for f in range(2):
                    pg = PS[0 + f]
                    pu = PS[2 + f]
                    for kt in range(8):
                        k.mm(pg[:, :n], wgb[b][:, kt, f * 128:(f + 1) * 128], hT[:, kt, lo:lo + n], kt == 0, kt == 7)
                    for kt in range(8):
                        k.mm(pu[:, :n], wub[b][:, kt, f * 128:(f + 1) * 128], hT[:, kt, lo:lo + n], kt == 0, kt == 7)
                    sg = sgs[f]
                    k.act(sg[:, :n], pg[:, :n], AF.Silu)
                    if gbc is not None:
                        k.tt(sg[:, :n], sg[:, :n], gbc[:, lo:lo + n], ALU.mult, eng="gpsimd")
                    k.tt(hb[:, f, :n], sg[:, :n], pu[:, :n], ALU.mult)

            def stage_b(j):
                fb, ti = jobs[j]
                b = fb % 2
                lo, n = TT[ti]
                jj = 1 if ti == 0 else 0
                hb = hid[j % 2]
                for nt in range(8):
                    ps = PS[4 + nt % 4]
                    for f in range(2):
                        k.mm(ps[:, :n], wdb[b][:, f, nt * 128:(nt + 1) * 128], hb[:, f, :n], f == 0, f == 1)
                    k.stt(xT[:, nt, lo:lo + n], ps[:, :n], mod[:, jj, 40 + nt:41 + nt], xT[:, nt, lo:lo + n],
                          ALU.mult, ALU.add)

            stage_a(0)
            for j in range(len(jobs)):
                if j + 1 < len(jobs):
                    stage_a(j + 1)
                stage_b(j)

        def ffn_bufs(s2):
            wgb = [s2.enter_context(sbt("wgb%d" % i, [128, 8, 256], BF16)) for i in range(2)]
            wub = [s2.enter_context(sbt("wub%d" % i, [128, 8, 256], BF16)) for i in range(2)]
            wdb = [s2.enter_context(sbt("wdb%d" % i, [128, 2, D], BF16)) for i in range(2)]
            hid = [s2.enter_context(sbt("hid%d" % i, [128, 2, 512], BF16)) for i in range(2)]
            sg = [s2.enter_context(sbt("sg%d" % i, [128, 512], F32)) for i in range(2)]
            return (wgb, wub, wdb, hid, sg)

        def ffn_dense(j):
            with ExitStack() as s2:
                bufs = ffn_bufs(s2)
                ffn_expert(ffn_wg[j], ffn_wu[j], ffn_wd[j], None, bufs)
                k.S.barrier()

        def ffn_moe(j):
            with ExitStack() as s2:
                bufs = ffn_bufs(s2)
                rw = s2.enter_context(sbt("rw", [128, 8, NE], BF16))
                gT = s2.enter_context(sbt("gT", [NE, NT], F32))
                sel = s2.enter_context(sbt("sel", [NE, NE, 128], F32))
                gbc = s2.enter_context(sbt("gbc", [128, NT], F32))
                lg = s2.enter_context(sbt("lg", [128, NE], F32))
                sm = s2.enter_context(sbt("sm", [128, 8], F32))
                m1 = s2.enter_context(sbt("m1", [128, NE], F32))
                m2 = s2.enter_context(sbt("m2", [128, NE], F32))
                l2 = s2.enter_context(sbt("l2", [128, NE], F32))
                gt = s2.enter_context(sbt("gt", [128, NE], F32))
                k.dma(rw[:], moe_r[j].rearrange("(kt p) e -> p kt e", p=128), "gpsimd")
                for e in range(NE):
                    k.cp(sel[:, e, :], ident[0:NE, e:e + 1].to_broadcast([NE, 128]))
                for c in range(NCH):
                    lo = c * 128
                    ps = PS[c % 2]
                    for kt in range(8):
                        k.mm(ps[:, 0:NE], hT[:, kt, lo:lo + 128], rw[:, kt, :], kt == 0, kt == 7)
                    k.cp(lg[:], ps[:, 0:NE])
                    k.red(sm[:, 0:1], lg[:], ALU.max)
                    k.ts(m1[:], lg[:], sm[:, 0:1], ALU.is_equal)
                    k.stt(l2[:], m1[:], -1e30, lg[:], ALU.mult, ALU.add)
                    k.red(sm[:, 1:2], l2[:], ALU.max)
                    k.ts(m2[:], l2[:], sm[:, 1:2], ALU.is_equal)
                    k.tt(sm[:, 2:3], sm[:, 0:1], sm[:, 1:2], ALU.subtract)
                    k.act(sm[:, 3:4], sm[:, 2:3], AF.Sigmoid)
                    k.ts(sm[:, 4:5], sm[:, 3:4], -1.0, ALU.mult, 1.0, ALU.add)
                    k.ts(gt[:], m1[:], sm[:, 3:4], ALU.mult)
                    k.stt(gt[:], m2[:], sm[:, 4:5], gt[:], ALU.mult, ALU.add)
                    pt = PS[2 + c % 2]
                    k.tr(pt[0:NE, 0:128], gt[:], ident[:])
                    k.cp(gT[:, lo:lo + 128], pt[0:NE, 0:128])
                for e in range(NE):
                    for ti, (lo, n) in enumerate(TT):
                        ps = PS[6 + ti % 2]
                        k.mm(ps[:, :n], sel[:, e, :], gT[:, lo:lo + n])
                        k.cp(gbc[:, lo:lo + n], ps[:, :n], eng="scalar")
                    ffn_expert(moe_wg[j, e], moe_wu[j, e], moe_wd[j, e], gbc, bufs)
                k.S.barrier()

        def load_win(dst, l, c0, ncols, q="gpsimd"):
            wv = w_in[l].rearrange("(kt p) c -> p kt c", p=128)
            k.dma(dst[:, :, :ncols], wv[:, :, c0:c0 + ncols], q)

        def proj_fm(wt, ncols, ti, ps):
            lo, n = TT[ti]
            for kt in range(8):
                k.mm(ps[:ncols, :n], wt[:, kt, :ncols], hT[:, kt, lo:lo + n], kt == 0, kt == 7)

        def proj_tm(wt, ncols, c, ps):
            for kt in range(8):
                k.mm(ps[:, :ncols], hT[:, kt, c * 128:(c + 1) * 128], wt[:, kt, :ncols], kt == 0, kt == 7)

        def sincos(s_out, c_out, ang, ki, tf, shape_slc):
            sl = shape_slc
            k.ts(ki[sl], ang, 1.0 / TWO_PI, ALU.mult)
            k.ts(tf[sl], ki[sl], -TWO_PI, ALU.mult)
            k.tt(tf[sl], tf[sl], ang, ALU.add)
            k.ts(tf[sl], tf[sl], math.pi, ALU.min, -math.pi, ALU.max)
            k.act(s_out, tf[sl], AF.Sin)
            k.ts(ki[sl], ang, 1.0 / TWO_PI, ALU.mult, 0.25, ALU.add)
            k.ts(tf[sl], ki[sl], -TWO_PI, ALU.mult)
            k.stt(tf[sl], ang, math.pi / 2, tf[sl], ALU.add, ALU.add)
            k.ts(tf[sl], tf[sl], math.pi, ALU.min, -math.pi, ALU.max)
            k.act(c_out, tf[sl], AF.Sin)

        s5yg = dscr("s5yg", [512, NT], BF16)

        def s5_mixer(l):
            with ExitStack() as s2:
                def T(name, shape, dt=F32):
                    return s2.enter_context(sbt("s5" + name, list(shape), dt))
                lam = T("lam", [128, 3, 32])
                pa = T("pa", [128, 32]); pdt = T("pdt", [128, 32]); par = T("par", [128, 32])
                pth = T("pth", [128, 32]); pr = T("pr", [128, 32]); psn = T("psn", [128, 32])
                pcs = T("pcs", [128, 32]); pki = T("pki", [128, 32], I32); ptf = T("ptf", [128, 32])
                lbr = T("lbr", [128, 32]); lbi = T("lbi", [128, 32]); den = T("den", [128, 32])
                gre = T("gre", [128, 32]); gim = T("gim", [128, 32]); ngim = T("ngim", [128, 32])
                t32 = T("t32", [128, 32])
                t96i = T("t96i", [128, 96], I32); t96 = T("t96", [128, 96])
                a96 = T("a96", [128, 96]); k96 = T("k96", [128, 96], I32); f96 = T("f96", [128, 96])
                s96 = T("s96", [128, 96]); c96 = T("c96", [128, 96])
                ttmp = [T("ttmp%d" % i, [128, 512]) for i in range(2)]
                ctabL = [T("ctab%d" % i, [128, NT]) for i in range(2)]
                stabL = [T("stab%d" % i, [128, NT]) for i in range(2)]
                SG = []
                for i in range(2):
                    b = {nm: T("%s%d" % (nm, i), [128, 512]) for nm in ("A", "Bb", "T1", "G1", "G2")}
                    b["HR"] = T("HR%d" % i, [128, 512], BF16); b["HI"] = T("HI%d" % i, [128, 512], BF16)
                    SG.append(b)
                car = T("car", [128, 2])
                ubf = T("ubf", [128, NT], BF16)
                wt = T("wt", [128, 8, 128], BF16)
                UB = []
                for i in range(2):
                    b = {nm: T("%s%d" % (nm, i), [128, 128]) for nm in ("Bre", "Bim", "Cre", "Cim", "Btr", "Bti")}
                    for nm in ("BtR", "BtI", "CbR", "CbI"):
                        b[nm] = T("%s%d" % (nm, i), [128, 128], BF16)
                    UB.append(b)
                dsk = T("dsk", [128, 4])
                XT = [T("X%d" % i, [128, 512]) for i in range(4)]
                yy = XT[0]; y2 = XT[1]; ygb = T("ygb", [128, 512], BF16)

                k.dma(lam[:], s5_lam[l], "sync")
                k.dma(dsk[:], s5_d[l], "sync")
                k.S.op("gpsimd", lambda e: e.iota(t96i[:, 0:48], pattern=[[48, 48]], base=0, channel_multiplier=0), [], [t96i])
                k.S.op("gpsimd", lambda e: e.iota(t96i[:, 48:96], pattern=[[1, 48]], base=0, channel_multiplier=0), [t96i], [t96i])
                k.cp(t96[:], t96i[:])
                k.ts(pa[:], lam[:, 0, :], -1e-4, ALU.min)
                k.act(pdt[:], lam[:, 2, :], AF.Exp)
                k.tt(par[:], pa[:], pdt[:], ALU.mult)
                k.tt(pth[:], lam[:, 1, :], pdt[:], ALU.mult)
                k.act(pr[:], par[:], AF.Exp)
                sincos(psn[:], pcs[:], pth[:], pki, ptf, (slice(None), slice(None)))
                k.tt(lbr[:], pr[:], pcs[:], ALU.mult)
                k.tt(lbi[:], pr[:], psn[:], ALU.mult)
                k.ts(lbr[:], lbr[:], -1.0, ALU.add)
                k.tt(den[:], pa[:], pa[:], ALU.mult)
                k.tt(t32[:], lam[:, 1, :], lam[:, 1, :], ALU.mult)
                k.tt(den[:], den[:], t32[:], ALU.add)
                k.recip(den[:], den[:])
                k.tt(gre[:], lbr[:], pa[:], ALU.mult)
                k.tt(t32[:], lbi[:], lam[:, 1, :], ALU.mult)
                k.tt(gre[:], gre[:], t32[:], ALU.add)
                k.tt(gre[:], gre[:], den[:], ALU.mult)
                k.tt(gim[:], lbi[:], pa[:], ALU.mult)
                k.tt(t32[:], lbr[:], lam[:, 1, :], ALU.mult)
                k.tt(gim[:], gim[:], t32[:], ALU.subtract)
                k.tt(gim[:], gim[:], den[:], ALU.mult)
                k.ts(ngim[:], gim[:], -1.0, ALU.mult)

                PSy = PS[0:5]
                UORD = [(0, 0), (1, 0), (2, 0), (3, 0), (0, 1), (1, 1), (2, 1), (3, 1)]

                def setup(ub, ui):
                    stl, d = UORD[ui]
                    u = (ub * 4 + stl) * 2 + d
                    B = UB[ui % 2]
                    ctab = ctabL[ui % 2]; stab = stabL[ui % 2]
                    k.dma(B["Bre"][:], s5_B[l, u, 0], "sync")
                    k.dma(B["Bim"][:], s5_B[l, u, 1], "sync")
                    k.dma(B["Cre"][:], s5_C[l, u, 0], "sync")
                    k.dma(B["Cim"][:], s5_C[l, u, 1], "sync")
                    k.ts(B["Btr"][:], B["Bre"][:], gre[:, u:u + 1], ALU.mult)
                    k.ts(B["Bti"][:], B["Bim"][:], gre[:, u:u + 1], ALU.mult)
                    k.stt(B["Btr"][:], B["Bim"][:], ngim[:, u:u + 1], B["Btr"][:], ALU.mult, ALU.add)
                    k.stt(B["Bti"][:], B["Bre"][:], gim[:, u:u + 1], B["Bti"][:], ALU.mult, ALU.add)
                    k.tr(PS[7][:, 0:128], B["Btr"][:], ident[:])
                    k.tr(PS[7][:, 128:256], B["Bti"][:], ident[:])
                    k.cp(B["BtR"][:], PS[7][:, 0:128], eng="scalar")
                    k.cp(B["BtI"][:], PS[7][:, 128:256], eng="scalar")
                    k.cp(B["CbR"][:], B["Cre"][:], eng="scalar")
                    k.ts(B["CbI"][:], B["Cim"][:], -1.0, ALU.mult)
                    k.ts(a96[:], t96[:], pth[:, u:u + 1], ALU.mult)
                    sincos(s96[:], c96[:], a96[:], k96, f96, (slice(None), slice(None)))
                    for pc in range(5):
                        a0 = pc * 10; na = min(10, 48 - a0)
                        eng = "gpsimd" if pc == 1 else "vector"
                        tm = ttmp[pc % 2]
                        def bc_a(src):
                            return src[:, a0:a0 + na].unsqueeze(2).to_broadcast([128, na, 48])
                        def bc_b(src):
                            return src[:, 48:96].unsqueeze(1).to_broadcast([128, na, 48])
                        cv = ctab[:, a0 * 48:(a0 + na) * 48].rearrange("p (a b) -> p a b", b=48)
                        sv = stab[:, a0 * 48:(a0 + na) * 48].rearrange("p (a b) -> p a b", b=48)
                        tv = tm[:, 0:na * 48].rearrange("p (a b) -> p a b", b=48)
                        k.tt(cv, bc_a(c96), bc_b(c96), ALU.mult, eng=eng)
                        k.tt(tv, bc_a(s96), bc_b(s96), ALU.mult, eng=eng)
                        k.tt(cv, cv, tv, ALU.subtract, eng=eng)
                        k.tt(sv, bc_a(s96), bc_b(c96), ALU.mult, eng=eng)
                        k.tt(tv, bc_a(c96), bc_b(s96), ALU.mult, eng=eng)
                        k.tt(sv, sv, tv, ALU.add, eng=eng)

                segctr = [0]

                def seg_info(ui, si):
                    stl, d = UORD[ui]
                    order = [0, 1, 2, 3, 4] if d == 0 else [0, 4, 3, 2, 1]
                    ti = order[si]
                    lo, n = TT[ti]
                    if d == 0:
                        tsl = slice(lo, lo + n); fw = slice(0, n); last = n - 1
                    else:
                        tsl = slice(255, None, -1) if ti == 0 else slice(2559 - lo, 2559 - lo - n, -1)
                        fw = slice(n - 1, None, -1); last = 0
                    return d, ti, lo, n, tsl, fw, last

                def stageA(ub, ui, si, buf):
                    d, ti, lo, n, tsl, fw, last = seg_info(ui, si)
                    B = UB[ui % 2]; ct = ctabL[ui % 2][:, tsl]; sn = stabL[ui % 2][:, tsl]
                    A, Bb, T1 = buf["A"], buf["Bb"], buf["T1"]
                    k.mm(PS[5][:, :n], B["BtR"][:], ubf[:, lo:lo + n])
                    k.mm(PS[6][:, :n], B["BtI"][:], ubf[:, lo:lo + n])
                    k.tt(A[:, :n], PS[5][:, :n], ct, ALU.mult)
                    k.tt(T1[:, :n], PS[6][:, :n], sn, ALU.mult)
                    k.tt(Bb[:, :n], PS[6][:, :n], ct, ALU.mult)
                    k.tt(A[:, :n], A[:, :n], T1[:, :n], ALU.add, eng="gpsimd")
                    k.tt(T1[:, :n], PS[5][:, :n], sn, ALU.mult)
                    k.tt(Bb[:, :n], Bb[:, :n], T1[:, :n], ALU.subtract, eng="gpsimd")

                def stageB1(ub, ui, si, buf):
                    d, ti, lo, n, tsl, fw, last = seg_info(ui, si)
                    stl = UORD[ui][0]
                    u = (ub * 4 + stl) * 2 + d
                    ct = ctabL[ui % 2][:, tsl]; sn = stabL[ui % 2][:, tsl]
                    A, Bb, G1, G2 = buf["A"], buf["Bb"], buf["G1"], buf["G2"]
                    rb = pr[:, u:u + 1].to_broadcast([128, n])
                    i1 = 0.0 if si == 0 else car[:, 0:1]
                    i2 = 0.0 if si == 0 else car[:, 1:2]
                    k.scan(G1[:, fw], rb, A[:, fw], i1)
                    k.scan(G2[:, fw], rb, Bb[:, fw], i2)
                    if si < 4:
                        k.cp(car[:, 0:1], G1[:, last:last + 1])
                        k.cp(car[:, 1:2], G2[:, last:last + 1])
                    k.tt(XT[0][:, :n], G1[:, :n], ct, ALU.mult, eng="gpsimd")
                    k.tt(XT[1][:, :n], G2[:, :n], sn, ALU.mult, eng="gpsimd")
                    k.tt(XT[2][:, :n], G1[:, :n], sn, ALU.mult, eng="gpsimd")
                    k.tt(XT[3][:, :n], G2[:, :n], ct, ALU.mult, eng="gpsimd")

                def stageB2(ub, ui, si, buf):
                    d, ti, lo, n, tsl, fw, last = seg_info(ui, si)
                    B = UB[ui % 2]
                    HR, HI = buf["HR"], buf["HI"]
                    k.tt(HR[:, :n], XT[0][:, :n], XT[1][:, :n], ALU.subtract)
                    k.tt(HI[:, :n], XT[2][:, :n], XT[3][:, :n], ALU.add)
                    k.mm(PSy[ti][:, :n], B["CbR"][:], HR[:, :n], ui == 0, False)
                    k.mm(PSy[ti][:, :n], B["CbI"][:], HI[:, :n], False, ui == 7)

                for ub in range(4):
                    load_win(wt, l, ub * 128, 128)
                    for ti, (lo, n) in enumerate(TT):
                        proj_fm(wt, 128, ti, PS[5 + ti % 2])
                        k.cp(ubf[:, lo:lo + n], PS[5 + ti % 2][:, :n], eng="scalar")
                    setup(ub, 0)
                    bufs = {}
                    bufs[(0, 0)] = SG[segctr[0] % 2]; segctr[0] += 1
                    stageA(ub, 0, 0, bufs[(0, 0)])
                    pending = None
                    for ui in range(8):
                        for si in range(5):
                            stageB1(ub, ui, si, bufs[(ui, si)]) if pending is None else None
                            if pending is not None:
                                pass
                            if si < 4:
                                nb = SG[segctr[0] % 2]; segctr[0] += 1
                                bufs[(ui, si + 1)] = nb
                                stageA(ub, ui, si + 1, nb)
                            elif ui < 7:
                                setup(ub, ui + 1)
                                nb = SG[segctr[0] % 2]; segctr[0] += 1
                                bufs[(ui + 1, 0)] = nb
                                stageA(ub, ui + 1, 0, nb)
                            stageB2(ub, ui, si, bufs[(ui, si)])
                    for ti, (lo, n) in enumerate(TT):
                        k.stt(yy[:, :n], ubf[:, lo:lo + n], dsk[:, ub:ub + 1], PSy[ti][:, :n], ALU.mult, ALU.add)
                        k.tt(y2[:, :n], yy[:, :n], yy[:, :n], ALU.mult, eng="gpsimd")
                        k.ts(y2[:, :n], y2[:, :n], 0.044715, ALU.mult, 1.0, ALU.add, eng="gpsimd")
                        k.tt(y2[:, :n], y2[:, :n], yy[:, :n], ALU.mult, eng="gpsimd")
                        k.act(y2[:, :n], y2[:, :n], AF.Sigmoid, scale=2.0 * math.sqrt(2.0 / math.pi))
                        k.tt(ygb[:, :n], yy[:, :n], y2[:, :n], ALU.mult)
                        k.dma(s5yg[ub * 128:(ub + 1) * 128, lo:lo + n], ygb[:, :n], "sync")
                k.S.barrier()
            with ExitStack() as s2:
                def T(name, shape, dt=F32):
                    return s2.enter_context(sbt("s5g" + name, list(shape), dt))
                wglu = T("wglu", [128, 4, 512], BF16)
                ygl = T("ygl", [128, 4, 512], BF16)
                yo = T("yo", [128, 512], BF16)
                yy = T("yy", [128, 512])
                k.dma(wglu[:], s5_glu[l].rearrange("(kt p) n -> p kt n", p=128), "gpsimd")
                ygv = s5yg.rearrange("(kt p) t -> p kt t", p=128)
                for ti, (lo, n) in enumerate(TT):
                    k.dma(ygl[:, :, :n], ygv[:, :, lo:lo + n], "sync")
                    for nt in range(4):
                        ps = PS[nt % 4]
                        for kt in range(4):
                            k.mm(ps[:, :n], wglu[:, kt, nt * 128:(nt + 1) * 128], ygl[:, kt, :n], kt == 0, kt == 3)
                        k.act(yy[:, :n], ps[:, :n], AF.Sigmoid)
                        k.tt(yo[:, :n], yy[:, :n], ygl[:, nt, :n], ALU.mult)
                        k.dma(ymix[nt * 128:(nt + 1) * 128, lo:lo + n], yo[:, :n], "sync")
                k.S.barrier()

        maskF = sb("maskF", [128, 128]); maskB = sb("maskB", [128, 128])
        nstrF = sb("nstrF", [128, 128]); nstrB = sb("nstrB", [128, 128])
        selF = sb("selF", [128, 128]); selB = sb("selB", [128, 128])
        k.memset(maskF[:], 1.0); k.memset(maskB[:], 1.0); k.memset(selF[:], 0.0); k.memset(selB[:], 0.0)
        k.S.op("gpsimd", lambda e: e.affine_select(out=maskF[:], in_=maskF[:], pattern=[[1, 128]], compare_op=ALU.is_ge,
                                                   fill=0.0, base=0, channel_multiplier=-1), [maskF], [maskF])
        k.S.op("gpsimd", lambda e: e.affine_select(out=maskB[:], in_=maskB[:], pattern=[[-1, 128]], compare_op=ALU.is_ge,
                                                   fill=0.0, base=0, channel_multiplier=1), [maskB], [maskB])
        k.S.op("gpsimd", lambda e: e.affine_select(out=selF[:], in_=selF[:], pattern=[[0, 128]], compare_op=ALU.not_equal,
                                                   fill=1.0, base=-127, channel_multiplier=1), [selF], [selF])
        k.S.op("gpsimd", lambda e: e.affine_select(out=selB[:], in_=selB[:], pattern=[[0, 128]], compare_op=ALU.not_equal,
                                                   fill=1.0, base=0, channel_multiplier=1), [selB], [selB])
        k.tt(nstrF[:], ident[:], maskF[:], ALU.subtract)
        k.tt(nstrB[:], ident[:], maskB[:], ALU.subtract)
        MASK = [maskF, maskB]; NSTR = [nstrF, nstrB]; SEL = [selF, selB]; ENDI = [127, 0]
        CH_ORDER = [list(range(NCH)), [1, 0] + list(range(NCH - 1, 1, -1))]

        def conv_block(raw, acc, cw, cb, ntap_bias):
            if ntap_bias:
                k.ts(acc[:], raw[:], cw[:, cb, 2:3], ALU.mult, cw[:, cb, 5:6], ALU.add)
            else:
                k.ts(acc[:], raw[:], cw[:, cb, 2:3], ALU.mult)
            for kk in (0, 1, 3, 4):
                dd = kk - 2
                for (s0, L) in ((0, NCTX), (NCTX, NT - NCTX)):
                    o0 = s0 + max(0, -dd); o1 = s0 + L - max(0, dd)
                    i0 = s0 + max(0, dd); i1 = s0 + L - max(0, -dd)
                    k.stt(acc[:, o0:o1], raw[:, i0:i1], cw[:, cb, kk:kk + 1], acc[:, o0:o1], ALU.mult, ALU.add,
                          eng=("vector" if kk < 2 else "gpsimd"))


        PADL = 2 + NCTX + 2 + 2 + (NT - NCTX) + 2

        def pad_off(lo):
            return 2 + lo if lo < NCTX else 2 + NCTX + 2 + 2 + (lo - NCTX)

        def conv_pe(rawp, dg, cw, cb, acc, bias_col):
            for kk in range(5):
                k.ts(dg[:, kk, :], identb[:], cw[:, cb, kk:kk + 1], ALU.mult)
            for ti, (lo, n) in enumerate(TT):
                ps = PS[4 + ti % 4]
                o = pad_off(lo)
                for kk in range(5):
                    k.mm(ps[:, :n], dg[:, kk, :], rawp[:, o + kk - 2:o + kk - 2 + n], kk == 0, kk == 4)
                if bias_col is not None:
                    k.act(acc[:, lo:lo + n], ps[:, :n], AF.Silu, bias=bias_col)
                else:
                    k.act(acc[:, lo:lo + n], ps[:, :n], AF.Silu)

        m2tm = dscr("m2tm", [NT, 1280])
        m2fm = dscr("m2fm", [512, NT])
        M2X = 3600

        def m2_mixer(l):
            with ExitStack() as s2:
                def T(name, shape, dt=F32):
                    return s2.enter_context(sbt("ma" + name, list(shape), dt))
                cw = T("cw", [128, 12, 6])
                wt = [T("wt%d" % i, [128, 8, 128], BF16) for i in range(2)]
                rawp = T("rawp", [128, PADL], BF16); acc = T("acc", [128, NT])
                dg = T("dg", [128, 5, 128], BF16)
                tmall = T("tmall", [128, NCH, 128])
                k.dma(cw[:], m2_cw[l], "sync")
                k.memset(rawp[:], 0.0)
                for cb in range(12):
                    w = wt[cb % 2]
                    load_win(w, l, M2X + cb * 128, 128)
                    for ti, (lo, n) in enumerate(TT):
                        ps = PS[ti % 4]
                        proj_fm(w, 128, ti, ps)
                        o = pad_off(lo)
                        k.cp(rawp[:, o:o + n], ps[:, :n], eng=("scalar" if ti % 2 else "vector"))
                    conv_pe(rawp, dg, cw, cb, acc, cw[:, cb, 5:6])
                    if cb >= 8:
                        k.dma(m2fm[(cb - 8) * 128:(cb - 7) * 128, :], acc[:], "sync")
                    if cb < 10:
                        for c in range(NCH):
                            ps = PS[c % 4]
                            k.tr(ps[:, 0:128], acc[:, c * 128:(c + 1) * 128], ident[:])
                            k.cp(tmall[:, c, :], ps[:, 0:128], eng=("vector" if c % 2 else "scalar"))
                        k.dma(m2tm[:, cb * 128:(cb + 1) * 128].rearrange("(c p) f -> p c f", p=128), tmall[:], "sync")
                k.S.barrier()
            with ExitStack() as s2:
                def T(name, shape, dt=F32):
                    return s2.enter_context(sbt("mb" + name, list(shape), dt))
                wz = T("wz", [128, 8, 1024], BF16)
                wdt = T("wdt", [128, 8, 32], BF16)
                ab = T("ab", [128, 80]); ng = T("ng", [128, 1024])
                nega = T("nega", [128, 32])
                Sst = T("S", [128, 16, 64])
                xtm = T("xtm", [128, 16, 64]); btm = T("btm", [128, 256])
                BT = T("BT", [128, 2, 128]); CT = T("CT", [128, 2, 128])
                dtv = T("dtv", [128, 16]); la = T("la", [128, 16]); cum = T("cum", [128, 16]); cend = T("cend", [128, 16])
                ecum = T("ecum", [128, 16]); edd = T("edd", [128, 16]); ecend = T("ecend", [128, 16])
                DEM = T("DEM", [128, 16, 128])
                DEMb = T("DEMb", [128, 16, 128], BF16); xdtb = T("xdtb", [128, 16, 64], BF16)
                Gm = T("Gm", [128, 2, 128])
                xdt = T("xdt", [128, 16, 64]); xdec = T("xdec", [128, 16, 64])
                yacc = T("yacc", [128, 16, 64]); yf = T("yf", [128, 16, 64])
                zs = T("zs", [128, 1024]); sq = T("sq", [128, 512]); ss = T("ss", [128, 2])
                ytb = T("ytb", [128, 8, 128], BF16)
                k.dma(ab[:], m2_ab[l], "sync")
                k.dma(ng[:], m2_ng[l], "sync")
                load_win(wz, l, 2576, 1024)
                load_win(wdt, l, 5136, 32)
                k.act(nega[:], ab[:, 0:32], AF.Exp)
                k.ts(nega[:], nega[:], -1.0, ALU.mult)
                for d in range(2):
                    mk = MASK[d]
                    k.memset(Sst[:], 0.0)
                    for c in CH_ORDER[d]:
                        r0 = c * 128
                        k.dma(xtm[:].rearrange("p h e -> p (h e)"), m2tm[r0:r0 + 128, 0:1024], "sync")
                        k.dma(btm[:], m2tm[r0:r0 + 128, 1024:1280], "sync")
                        k.dma(BT[:], m2fm[0:256, r0:r0 + 128].rearrange("(g p) t -> p g t", p=128), "sync")
                        k.dma(CT[:], m2fm[256:512, r0:r0 + 128].rearrange("(g p) t -> p g t", p=128), "sync")
                        proj_tm(wdt, 32, c, PS[6])
                        k.tt(dtv[:], PS[6][:, d * 16:(d + 1) * 16], ab[:, 32 + d * 16:48 + d * 16], ALU.add)
                        k.act(dtv[:], dtv[:], AF.Exp)
                        k.act(dtv[:], dtv[:], AF.Ln, bias=ones[:, 0:1])
                        k.tt(la[:], dtv[:], nega[:, d * 16:(d + 1) * 16], ALU.mult)
                        k.mm(PS[6][:, 64:80], mk[:], la[:])
                        k.cp(cum[:], PS[6][:, 64:80])
                        k.mm(PS[6][:, 96:112], SEL[d][:], cum[:])
                        k.cp(cend[:], PS[6][:, 96:112])
                        k.act(ecum[:], cum[:], AF.Exp)
                        k.act(ecend[:], cend[:], AF.Exp)
                        k.tt(edd[:], cend[:], cum[:], ALU.subtract)
                        k.act(edd[:], edd[:], AF.Exp)
                        k.tt(xdt[:], xtm[:], dtv[:].unsqueeze(2).to_broadcast([128, 16, 64]), ALU.mult)
                        k.tt(DEM[:], ident[:].unsqueeze(1).to_broadcast([128, 16, 128]),
                             cum[:].unsqueeze(2).to_broadcast([128, 16, 128]), ALU.mult, eng="gpsimd")
                        for q4 in range(4):
                            k.mm(PS[q4][:], ones[:], DEM[:, q4 * 4:(q4 + 1) * 4, :].rearrange("p h t -> p (h t)"))
                        for q4 in range(4):
                            k.tt(DEM[:, q4 * 4:(q4 + 1) * 4, :], PS[q4][:].rearrange("p (h t) -> p h t", t=128),
                                 cum[:, q4 * 4:(q4 + 1) * 4].unsqueeze(2).to_broadcast([128, 4, 128]), ALU.subtract)
                        k.tt(DEM[:], DEM[:], mk[:].unsqueeze(1).to_broadcast([128, 16, 128]), ALU.mult, eng="gpsimd")
                        k.act(DEM[:], DEM[:], AF.Exp)
                        for g in range(2):
                            k.mm(PS[6][:, 128 + g * 128:256 + g * 128], BT[:, g, :], CT[:, g, :])
                        k.tt(Gm[:], PS[6][:, 128:384].rearrange("p (g t) -> p g t", t=128),
                             mk[:].unsqueeze(1).to_broadcast([128, 2, 128]), ALU.mult)
                        for g in range(2):
                            k.tt(DEMb[:, g * 8:(g + 1) * 8, :], DEM[:, g * 8:(g + 1) * 8, :],
                                 Gm[:, g, :].unsqueeze(1).to_broadcast([128, 8, 128]), ALU.mult,
                                 eng=("vector" if g == 0 else "gpsimd"))
                        k.cp(xdtb[:], xdt[:], eng="scalar")
                        for h in range(16):
                            ps = PS[4 + h // 8]
                            k.mm(ps[:, (h % 8) * 64:(h % 8 + 1) * 64], DEMb[:, h, :], xdtb[:, h, :])
                        for g in range(2):
                            k.mm(PS[g][:], CT[:, g, :], Sst[:, g * 8:(g + 1) * 8, :].rearrange("p h e -> p (h e)"))
                        for g in range(2):
                            hs = slice(g * 8, (g + 1) * 8)
                            k.tt(yacc[:, hs, :], PS[g][:].rearrange("p (h e) -> p h e", e=64),
                                 ecum[:, hs].unsqueeze(2).to_broadcast([128, 8, 64]), ALU.mult)
                            k.tt(yacc[:, hs, :], yacc[:, hs, :], PS[4 + g][:].rearrange("p (h e) -> p h e", e=64), ALU.add)
                        k.tt(xdec[:], xdt[:], edd[:].unsqueeze(2).to_broadcast([128, 16, 64]), ALU.mult, eng="gpsimd")
                        for g in range(2):
                            k.mm(PS[2 + g][:], btm[:, g * 128:(g + 1) * 128],
                                 xdec[:, g * 8:(g + 1) * 8, :].rearrange("p h e -> p (h e)"))
                        k.tt(Sst[:], Sst[:], ecend[:].unsqueeze(2).to_broadcast([128, 16, 64]), ALU.mult, eng="gpsimd")
                        for g in range(2):
                            hs = slice(g * 8, (g + 1) * 8)
                            k.tt(Sst[:, hs, :], Sst[:, hs, :], PS[2 + g][:].rearrange("p (h e) -> p h e", e=64), ALU.add)
                        if d == 0:
                            k.dma(yfwd_m[r0:r0 + 128, :], yacc[:].rearrange("p h e -> p (h e)"), "sync")
                        else:
                            k.dma(yf[:].rearrange("p h e -> p (h e)"), yfwd_m[r0:r0 + 128, :], "sync")
                            k.tt(yacc[:], yacc[:], yf[:], ALU.add, eng="gpsimd")
                            k.tt(yf[:], xtm[:], ab[:, 64:80].unsqueeze(2).to_broadcast([128, 16, 64]), ALU.mult, eng="gpsimd")
                            k.tt(yacc[:], yacc[:], yf[:], ALU.add, eng="gpsimd")
                            for hf in range(2):
                                proj_tm(wz[:, :, hf * 512:(hf + 1) * 512], 512, c, PS[7])
                                k.act(zs[:, hf * 512:(hf + 1) * 512], PS[7][:], AF.Silu)
                            yv = yacc[:].rearrange("p h e -> p (h e)")
                            k.tt(yv, yv, zs[:], ALU.mult)
                            k.memset(ss[:], 0.0)
                            for g in range(2):
                                k.act(sq[:], yv[:, g * 512:(g + 1) * 512], AF.Square, accum=ss[:, g:g + 1])
                            k.act(ss[:], ss[:], AF.Sqrt, bias=epsc[:, 0:1], scale=1.0 / 512)
                            k.recip(ss[:], ss[:])
                            for g in range(2):
                                k.stt(yv[:, g * 512:(g + 1) * 512], yv[:, g * 512:(g + 1) * 512], ss[:, g:g + 1],
                                      ng[:, g * 512:(g + 1) * 512], ALU.mult, ALU.mult)
                            for cb in range(8):
                                pst = PS[6 + cb % 2]
                                k.tr(pst[:, 0:128], yv[:, cb * 128:(cb + 1) * 128], ident[:])
                                k.cp(ytb[:, cb, :], pst[:, 0:128], eng=("scalar" if cb % 2 else "vector"))
                            k.dma(ymix[1024:2048, r0:r0 + 128].rearrange("(cb p) t -> p cb t", p=128), ytb[:], "sync")
                k.S.barrier()

        gtm = dscr("gtm", [NT, 1024])
        gfm = dscr("gfm", [1024, NT])
        GQ = 512

        GC = os.environ.get("GDNCUT", "")

        def gdn_mixer(l):
            with ExitStack() as s2:
                def T(name, shape, dt=F32):
                    return s2.enter_context(sbt("ga" + name, list(shape), dt))
                cw = T("cw", [128, 12, 5])
                wt = [T("wt%d" % i, [128, 8, 128], BF16) for i in range(2)]
                rawp = T("rawp", [128, PADL], BF16); acc = T("acc", [128, NT])
                dg = T("dg", [128, 5, 128], BF16)
                sq = T("sq", [128, 512]); rs = T("rs", [128, 512])
                tmall = T("tmall", [128, NCH, 128])
                k.dma(cw[:], gdn_cw[l], "sync")
                k.memset(rawp[:], 0.0)
                for cb in range(12):
                    w = wt[cb % 2]
                    load_win(w, l, GQ + cb * 128, 128)
                    for ti, (lo, n) in enumerate(TT):
                        ps = PS[ti % 4]
                        proj_fm(w, 128, ti, ps)
                        o = pad_off(lo)
                        k.cp(rawp[:, o:o + n], ps[:, :n], eng=("scalar" if ti % 2 else "vector"))
                    conv_pe(rawp, dg, cw, cb, acc, None)
                    if cb < 8:
                        for ti, (lo, n) in enumerate(TT):
                            ps = PS[4 + ti % 2]
                            k.tt(sq[:, :n], acc[:, lo:lo + n], acc[:, lo:lo + n], ALU.mult, eng="gpsimd")
                            k.mm(ps[:, :n], ones[:], sq[:, :n])
                            k.act(rs[:, :n], ps[:, :n], AF.Sqrt, bias=epsc[:, 0:1], scale=1.0)
                            k.recip(rs[:, :n], rs[:, :n])
                            if cb < 4:
                                k.stt(acc[:, lo:lo + n], acc[:, lo:lo + n], 128.0 ** -0.5, rs[:, :n], ALU.mult, ALU.mult)
                            else:
                                k.tt(acc[:, lo:lo + n], acc[:, lo:lo + n], rs[:, :n], ALU.mult)
                        k.dma(gfm[cb * 128:(cb + 1) * 128, :], acc[:], "sync")
                    if cb >= 4:
                        for c in range(NCH):
                            ps = PS[6 + c % 2]
                            k.tr(ps[:, 0:128], acc[:, c * 128:(c + 1) * 128], ident[:])
                            k.cp(tmall[:, c, :], ps[:, 0:128], eng=("vector" if c % 2 else "scalar"))
                        k.dma(gtm[:, (cb - 4) * 128:(cb - 3) * 128].rearrange("(c p) f -> p c f", p=128), tmall[:], "sync")
                k.S.barrier()
            if GC == "s1":
                return
            with ExitStack() as s2:
                def T(name, shape, dt=F32):
                    return s2.enter_context(sbt("gb" + name, list(shape), dt))
                wz = T("wz", [128, 8, 512], BF16)
                wab = T("wab", [128, 8, 16], BF16)
                abp = T("abp", [128, 16]); ng = T("ng", [128, 128]); nega = T("nega", [128, 8])
                Sst = T("S", [128, 4, 128])
                ggL = [T("gg%d" % i, [128, 4]) for i in range(2)]
                betaL = [T("beta%d" % i, [128, 4]) for i in range(2)]
                gcL = [T("gc%d" % i, [128, 4]) for i in range(2)]
                egcL = [T("egc%d" % i, [128, 4]) for i in range(2)]
                zsL = [T("zs%d" % i, [128, 512]) for i in range(2)]
                HB = []
                for h in range(4):
                    b = {}
                    for nm in ("knT", "qnT", "ktm", "vtm", "EdT", "AT", "P", "PT", "wT", "vn", "qdT", "eRg", "kdec",
                               "oo", "of", "sq"):
                        b[nm] = T("%s%d" % (nm, h), [128, 128])
                    b["D2"] = T("D2%d" % h, [128, 256]); b["R"] = T("R%d" % h, [128, 256])
                    b["sc1"] = T("sc1%d" % h, [128, 8]); b["otb"] = T("otb%d" % h, [128, 128], BF16)
                    HB.append(b)
                k.dma(abp[:], gdn_ab[l], "sync")
                k.dma(ng[:], gdn_ng[l], "sync")
                load_win(wz, l, 2048, 512)
                load_win(wab, l, 2560, 16)
                k.act(nega[:], abp[:, 0:8], AF.Exp)
                k.ts(nega[:], nega[:], -1.0, ALU.mult)

                def prep(d, c, par):
                    gg, beta, gc, egc, zs = ggL[par], betaL[par], gcL[par], egcL[par], zsL[par]
                    proj_tm(wab, 16, c, PS[7])
                    k.tt(gg[:], PS[7][:, d * 4:(d + 1) * 4], abp[:, 8 + d * 4:12 + d * 4], ALU.add)
                    k.act(gg[:], gg[:], AF.Exp)
                    k.act(gg[:], gg[:], AF.Ln, bias=ones[:, 0:1])
                    k.tt(gg[:], gg[:], nega[:, d * 4:(d + 1) * 4], ALU.mult)
                    k.act(beta[:], PS[7][:, 8 + d * 4:12 + d * 4], AF.Sigmoid)
                    k.mm(PS[7][:, 32:36], MASK[d][:], gg[:])
                    k.cp(gc[:], PS[7][:, 32:36])
                    k.act(egc[:], gc[:], AF.Exp)
                    if d == 1:
                        proj_tm(wz, 512, c, PS[6])
                        k.act(zs[:], PS[6][:], AF.Silu)

                def unit(d, c, par, h):
                    beta, gc, egc, zs = betaL[par], gcL[par], egcL[par], zsL[par]
                    mk = MASK[d]; e_i = ENDI[d]
                    r0 = c * 128
                    B = HB[h]
                    knT, qnT, ktm, vtm, D2, EdT, AT = B["knT"], B["qnT"], B["ktm"], B["vtm"], B["D2"], B["EdT"], B["AT"]
                    P, PT, R, wT, vn, qdT, eRg, kdec = B["P"], B["PT"], B["R"], B["wT"], B["vn"], B["qdT"], B["eRg"], B["kdec"]
                    sc1, oo, of, sq, otb = B["sc1"], B["oo"], B["of"], B["sq"], B["otb"]
                    pA = PS[h]; pB = PS[4 + h]
                    k.dma(qnT[:], gfm[h * 128:(h + 1) * 128, r0:r0 + 128], "sync")
                    k.dma(knT[:], gfm[512 + h * 128:640 + h * 128, r0:r0 + 128], "sync")
                    k.dma(ktm[:], gtm[r0:r0 + 128, h * 128:(h + 1) * 128], "scalar")
                    k.dma(vtm[:], gtm[r0:r0 + 128, 512 + h * 128:640 + h * 128], "scalar")
                    yield
                    k.ts(D2[:, 0:128], ident[:], gc[:, h:h + 1], ALU.mult)
                    k.ts(D2[:, 128:256], ident[:], beta[:, h:h + 1], ALU.mult)
                    yield
                    k.mm(pA[:, 0:256], ones[:], D2[:])
                    Rg = pA[:, 0:128]; Rb = pA[:, 128:256]
                    k.mm(pB[:, 0:128], knT[:], knT[:])
                    k.mm(pB[:, 128:256], knT[:], qnT[:])
                    yield
                    k.ts(EdT[:], Rg, gc[:, h:h + 1], ALU.subtract)
                    yield
                    k.tt(EdT[:], EdT[:], mk[:], ALU.mult)
                    yield
                    k.act(EdT[:], EdT[:], AF.Exp)
                    k.cp(sc1[:, 4:5], pA[:, e_i:e_i + 1])
                    yield
                    k.tt(EdT[:], EdT[:], mk[:], ALU.mult)
                    yield
                    k.tt(AT[:], pB[:, 128:256], EdT[:], ALU.mult)
                    k.tt(PT[:], pB[:, 0:128], EdT[:], ALU.mult)
                    yield
                    k.tt(PT[:], PT[:], Rb, ALU.mult)
                    k.act(eRg[:], Rg, AF.Exp)
                    yield
                    k.tt(PT[:], PT[:], NSTR[d][:], ALU.mult)
                    k.ts(R[:, 0:128], vtm[:], beta[:, h:h + 1], ALU.mult)
                    k.ts(R[:, 128:256], ktm[:], beta[:, h:h + 1], ALU.mult, egc[:, h:h + 1], ALU.mult)
                    yield
                    k.tr(pB[:, 256:384], PT[:], ident[:])
                    yield
                    k.cp(P[:], pB[:, 256:384])
                    k.tt(qdT[:], qnT[:], eRg[:], ALU.mult)
                    yield
                    for lev in range(7):
                        k.mm(pA[:, 256:512], PT[:], R[:])
                        if lev < 6:
                            k.mm(pB[:, 0:128], PT[:], P[:])
                            k.mm(pB[:, 128:256], P[:], PT[:])
                        yield
                        k.tt(R[:], R[:], pA[:, 256:512], ALU.add)
                        if lev < 6:
                            k.cp(P[:], pB[:, 0:128])
                            k.cp(PT[:], pB[:, 128:256])
                        yield
                    k.tr(pB[:, 256:384], R[:, 128:256], ident[:])
                    k.ts(sc1[:, 0:1], gc[:, h:h + 1], -1.0, ALU.mult, sc1[:, 4:5], ALU.add)
                    yield
                    k.cp(wT[:], pB[:, 256:384])
                    k.act(sc1[:, 1:2], sc1[:, 0:1], AF.Exp)
                    k.act(sc1[:, 2:3], sc1[:, 4:5], AF.Exp)
                    yield
                    k.mm(pB[:, 384:512], wT[:], Sst[:, h, :])
                    k.ts(kdec[:], ktm[:], sc1[:, 1:2], ALU.mult)
                    yield
                    k.tt(vn[:], R[:, 0:128], pB[:, 384:512], ALU.subtract)
                    yield
                    k.mm(pB[:, 384:512], qdT[:], Sst[:, h, :], True, False)
                    k.mm(pB[:, 384:512], AT[:], vn[:], False, True)
                    k.mm(pB[:, 256:384], kdec[:], vn[:])
                    yield
                    if d == 0:
                        k.cp(oo[:], pB[:, 384:512])
                    else:
                        k.dma(of[:], yfwd_g[h, r0:r0 + 128, :], "sync")
                        k.tt(oo[:], of[:], pB[:, 384:512], ALU.add)
                    k.stt(Sst[:, h, :], Sst[:, h, :], sc1[:, 2:3], pB[:, 256:384], ALU.mult, ALU.add)
                    yield
                    if d == 0:
                        k.dma(yfwd_g[h, r0:r0 + 128, :], oo[:], "sync")
                    else:
                        k.memset(sc1[:, 3:4], 0.0)
                        yield
                        k.act(sq[:], oo[:], AF.Square, accum=sc1[:, 3:4])
                        yield
                        k.act(sc1[:, 3:4], sc1[:, 3:4], AF.Sqrt, bias=epsc[:, 0:1], scale=1.0 / 128)
                        yield
                        k.recip(sc1[:, 3:4], sc1[:, 3:4])
                        yield
                        k.stt(oo[:], oo[:], sc1[:, 3:4], ng[:], ALU.mult, ALU.mult)
                        yield
                        k.tt(oo[:], oo[:], zs[:, h * 128:(h + 1) * 128], ALU.mult)
                        yield
                        k.tr(pB[:, 256:384], oo[:], ident[:])
                        yield
                        k.cp(otb[:], pB[:, 256:384], eng="scalar")
                        yield
                        k.dma(ymix[512 + h * 128:640 + h * 128, r0:r0 + 128], otb[:], "sync")

                for d in range(2):
                    k.memset(Sst[:], 0.0)
                    order = CH_ORDER[d]
                    prep(d, order[0], 0)
                    for ci, c in enumerate(order):
                        par = ci % 2
                        gens = [unit(d, c, par, h) for h in range(4)]
                        first = True
                        while gens:
                            nxt = []
                            for g in gens:
                                try:
                                    next(g)
                                    nxt.append(g)
                                except StopIteration:
                                    pass
                            gens = nxt
                            if first and ci + 1 < len(order):
                                prep(d, order[ci + 1], 1 - par)
                                first = False
                k.S.barrier()

        MIX = {"s5": s5_mixer, "gdn": gdn_mixer, "m2": m2_mixer}

        DBG = os.environ.get("DBGDUMP", "").split(",")

        def dbg_dump(name, ap, shape, dt=F32):
            if name not in DBG:
                return
            o = nc.dram_tensor("dbg_" + name, list(shape), dt, kind="ExternalOutput").ap()
            k.dma(o, ap, "sync")

        def dump_x():
            xo = dbgx.rearrange("(kt p) t -> p kt t", p=128)
            for kt in range(8):
                k.dma(xo[:, kt, :], xT[:, kt, :], "sync")

        skip = os.environ.get("SKIPMIX", "").split(",")
        for l in range(nlayers):
            odd = (l % 2 == 1)
            lastl = (l == nlayers - 1)
            adaln(l)
            rmsnorm_mod(l, n1g[l], 0, 1, odd)
            dbg_dump("h%d" % l, hT[:], [128, 8, NT], BF16)
            dbg_dump("mod%d" % l, mod[:], [128, 2, 48])
            dbg_dump("gs%d" % l, gs[:], [128, 2, 8])
            if "s5" not in skip:
                MIX["s5"](l)
            if "gdn" not in skip:
                MIX["gdn"](l)
            if "m2" not in skip:
                MIX["m2"](l)
            if lastl and stop == "mix":
                break
            out_proj(l, odd)
            if lastl and stop == "oproj":
                dump_x()
                break
            rmsnorm_mod(l, n2g[l], 3, 4, False)
            if l % 2 == 0:
                ffn_dense(l // 2)
            else:
                ffn_moe(l // 2)
            if lastl and stop == "ffn":
                dump_x()
                break
        if stop is None:
            rmsnorm_mod(0, fng, 0, 0, False, final=True)
        k.S.emit()
    return nc


_NC_CACHE = {}


def _prep_shared(inp):
    f = lambda a: np.ascontiguousarray(np.asarray(a, dtype=np.float32))
    g = {}
    g["ada_w"] = f(inp["ada_w"])
    g["ada_b"] = f(np.asarray(inp["ada_b"]).reshape(DEPTH, 48, 128).transpose(0, 2, 1))
    g["n1g"] = f(np.asarray(inp["norm1_g"]).reshape(DEPTH, 8, 128).transpose(0, 2, 1))
    g["n2g"] = f(np.asarray(inp["norm2_g"]).reshape(DEPTH, 8, 128).transpose(0, 2, 1))
    g["fng"] = f(np.asarray(inp["final_norm_g"]).reshape(8, 128).T)
    g["w_in"] = f(inp["w_in"])
    g["w_out"] = f(inp["w_out"])
    g["ffn_wg"] = f(inp["ffn_w_gate"]); g["ffn_wu"] = f(inp["ffn_w_up"]); g["ffn_wd"] = f(inp["ffn_w_down"])
    g["moe_r"] = f(inp["moe_router"])
    g["moe_wg"] = f(inp["moe_w_gate"]); g["moe_wu"] = f(inp["moe_w_up"]); g["moe_wd"] = f(inp["moe_w_down"])
    lam_re = np.asarray(inp["s5_lam_re"]); lam_im = np.asarray(inp["s5_lam_im"]); log_dt = np.asarray(inp["s5_log_dt"])
    b_re = np.asarray(inp["s5_b_re"]); b_im = np.asarray(inp["s5_b_im"])
    c_re = np.asarray(inp["s5_c_re"]); c_im = np.asarray(inp["s5_c_im"])
    s5_lam = np.zeros((DEPTH, 128, 3, 32), np.float32)
    s5_B = np.zeros((DEPTH, 32, 2, 128, 128), np.float32)
    s5_C = np.zeros((DEPTH, 32, 2, 128, 128), np.float32)
    for st in range(16):
        for d in range(2):
            u = st * 2 + d
            for g2 in range(2):
                gi = 2 * st + g2
                ps = slice(g2 * 64, (g2 + 1) * 64)
                s5_lam[:, ps, 0, u] = lam_re[:, d, gi, :]
                s5_lam[:, ps, 1, u] = lam_im[:, d, gi, :]
                s5_lam[:, ps, 2, u] = log_dt[:, d, gi][:, None]
                ch0 = 16 * (2 * (st % 4) + g2)
                s5_B[:, u, 0, ps, ch0:ch0 + 16] = b_re[:, d, gi]
                s5_B[:, u, 1, ps, ch0:ch0 + 16] = b_im[:, d, gi]
                s5_C[:, u, 0, ps, ch0:ch0 + 16] = c_re[:, d, gi].transpose(0, 2, 1)
                s5_C[:, u, 1, ps, ch0:ch0 + 16] = c_im[:, d, gi].transpose(0, 2, 1)
    g["s5_lam"] = s5_lam; g["s5_B"] = s5_B; g["s5_C"] = s5_C
    g["s5_d"] = f(np.asarray(inp["s5_d"]).reshape(DEPTH, 4, 128).transpose(0, 2, 1))
    g["s5_glu"] = f(inp["s5_w_glu"])
    g["gdn_cw"] = f(np.asarray(inp["gdn_conv_w"]).reshape(DEPTH, 5, 12, 128).transpose(0, 3, 2, 1))
    ab = np.concatenate([np.asarray(inp["gdn_a_log"]).reshape(DEPTH, 8), np.asarray(inp["gdn_dt_bias"]).reshape(DEPTH, 8)], 1)
    g["gdn_ab"] = f(np.broadcast_to(ab[:, None, :], (DEPTH, 128, 16)))
    g["gdn_ng"] = f(np.broadcast_to(np.asarray(inp["gdn_norm_g"])[:, None, :], (DEPTH, 128, 128)))
    cw = np.asarray(inp["m2_conv_w"]).reshape(DEPTH, 5, 12, 128).transpose(0, 3, 2, 1)
    cb = np.asarray(inp["m2_conv_b"]).reshape(DEPTH, 12, 128).transpose(0, 2, 1)[..., None]
    g["m2_cw"] = f(np.concatenate([cw, cb], axis=3))
    ab = np.concatenate([np.asarray(inp["m2_a_log"]).reshape(DEPTH, 32), np.asarray(inp["m2_dt_bias"]).reshape(DEPTH, 32),
                         np.asarray(inp["m2_d"]).reshape(DEPTH, 16)], 1)
    g["m2_ab"] = f(np.broadcast_to(ab[:, None, :], (DEPTH, 128, 80)))
    g["m2_ng"] = f(np.broadcast_to(np.asarray(inp["m2_norm_g"])[:, None, :], (DEPTH, 128, 1024)))
    return g


def kernel(**inp):
    n = 8
    x = np.asarray(inp["x"], dtype=np.float32)
    ctx = np.asarray(inp["ctx"], dtype=np.float32)
    c = np.asarray(inp["c"], dtype=np.float32)
    c_ctx = np.asarray(inp["c_ctx"], dtype=np.float32)
    shared = _prep_shared(inp)
    if "nc" not in _NC_CACHE:
        _NC_CACHE["nc"] = build_program()
    nc = _NC_CACHE["nc"]
    in_maps = []
    for b in range(n):
        m = dict(shared)
        m["xT"] = np.ascontiguousarray(np.concatenate([ctx[b], x[b]], axis=0).T)
        cs = np.stack([c[b].reshape(8, 128).T, c_ctx.reshape(8, 128).T], axis=2)
        m["cs"] = np.ascontiguousarray(cs.astype(np.float32))
        in_maps.append(m)
    res = run_bass_kernel_spmd(nc, in_maps, core_ids=list(range(n)))
    out = np.stack([np.asarray(r["outT"], dtype=np.float32).T for r in res.results], axis=0)
    return np.ascontiguousarray(out)
```
